# Optimizing a Trainium2 kernel written in Bass

```python
import math
import jax, jax.numpy as jnp
from jax import lax
import numpy as np

D_MODEL = 1024
BATCH = 4
SEQ = 4096
DEPTH = 4

D_FF = 2816
NSA_HEADS = 8
NSA_GROUPS = 2
NSA_HPG = NSA_HEADS // NSA_GROUPS
NSA_DH = 64
CMP_BLOCK = 32
CMP_STRIDE = 16
CMP_HIDDEN = 256
SEL_BLOCK = 64
SEL_TOPN = 16
WINDOW = 512
Q_BLOCK = 64
GLA_HEADS = 4
GLA_DK = 128
GLA_DV = 256
GLA_RANK = 16
GLA_TAU = 16.0
GLA_CHUNK = 64
REL_BUCKETS = 32
REL_MAX_DIST = 1024
EPS = 1e-6
NEG = -1e30

W_IN_SPLITS = (
    NSA_HEADS * NSA_DH,
    6 * NSA_GROUPS * NSA_DH,
    3 * NSA_HEADS,
    GLA_HEADS * GLA_DK,
    GLA_HEADS * GLA_DK,
    GLA_HEADS * GLA_DV,
    GLA_RANK,
    GLA_HEADS * GLA_DV,
    2 * D_MODEL,
)
D_IN = sum(W_IN_SPLITS)

kernel_name = "hybrid_nsa_gla_macaron_t5bias"


def rmsnorm(x, g):
    xf = x.astype(jnp.float32)
    y = xf * lax.rsqrt(jnp.mean(xf * xf, axis=-1, keepdims=True) + EPS)
    return (y * g.astype(jnp.float32)).astype(x.dtype)


def swiglu(x, w_gate, w_up, w_down):
    return (jax.nn.silu(x @ w_gate) * (x @ w_up)) @ w_down


def rel_bucket(dist):
    n = jnp.maximum(dist, 0)
    exact = REL_BUCKETS // 2
    nf = jnp.maximum(n, 1).astype(jnp.float32)
    log_b = exact + (jnp.log(nf / exact) / math.log(REL_MAX_DIST / exact)
                     * (REL_BUCKETS - exact)).astype(jnp.int32)
    return jnp.where(n < exact, n, jnp.minimum(log_b, REL_BUCKETS - 1))


def masked_softmax(logits, mask):
    p = jax.nn.softmax(jnp.where(mask, logits, NEG), axis=-1)
    return jnp.where(mask, p, 0.0)


def nsa_compress(k, pos, w1, w2):
    B, S, G, dh = k.shape
    nc = (S - CMP_BLOCK) // CMP_STRIDE + 1
    idx = jnp.arange(nc)[:, None] * CMP_STRIDE + jnp.arange(CMP_BLOCK)[None, :]
    blocks = k[:, idx] + pos[None, None, :, None, :]
    blocks = blocks.transpose(0, 1, 3, 2, 4).reshape(B, nc, G, CMP_BLOCK * dh)
    return jax.nn.silu(blocks @ w1) @ w2


def nsa_attention(q, k_cmp, v_cmp, k_slc, v_slc, k_win, v_win, gates, rel_table):
    B, S, H, dh = q.shape
    G, HPG = NSA_GROUPS, NSA_HPG
    nqb = S // Q_BLOCK
    ns = S // SEL_BLOCK
    nc = k_cmp.shape[1]
    n_sel = min(SEL_TOPN, ns)
    kw_len = WINDOW + Q_BLOCK

    c_start = jnp.arange(nc) * CMP_STRIDE
    cmp_end = c_start + CMP_BLOCK - 1
    sb_start = jnp.arange(ns) * SEL_BLOCK
    overlap = ((c_start[:, None] < sb_start[None, :] + SEL_BLOCK)
               & (c_start[:, None] + CMP_BLOCK > sb_start[None, :])).astype(jnp.float32)
    js = jnp.arange(ns)
    table_g = rel_table.reshape(REL_BUCKETS, G, HPG).transpose(1, 0, 2)

    kb = k_slc.reshape(B, ns, SEL_BLOCK, G, dh).transpose(0, 3, 1, 2, 4)
    vb = v_slc.reshape(B, ns, SEL_BLOCK, G, dh).transpose(0, 3, 1, 2, 4)
    pad = ((0, 0), (WINDOW, 0), (0, 0), (0, 0))
    kw = jnp.pad(k_win, pad)
    vw = jnp.pad(v_win, pad)

    def to_blocks(a):
        return a.reshape(B, nqb, Q_BLOCK, *a.shape[2:]).swapaxes(0, 1)

    qs = to_blocks(q * (dh ** -0.5))
    gs = to_blocks(gates)
    bi = jnp.arange(B)[:, None, None, None]
    gi = jnp.arange(G)[None, :, None, None]

    def block_fn(args):
        c, qc, gc = args
        t = c * Q_BLOCK + jnp.arange(Q_BLOCK)
        qg = qc.reshape(B, Q_BLOCK, G, HPG, dh)

        lc = jnp.einsum('bqghd,bcgd->bghqc', qg, k_cmp).astype(jnp.float32)
        bias_c = rel_table[rel_bucket(t[:, None] - cmp_end[None, :])]
        lc = lc + bias_c.reshape(Q_BLOCK, nc, G, HPG).transpose(2, 3, 0, 1)
        p_cmp = masked_softmax(lc, cmp_end[None, :] <= t[:, None])
        o_cmp = jnp.einsum('bghqc,bcgd->bqghd', p_cmp.astype(v_cmp.dtype), v_cmp)

        imp = jnp.einsum('bghqc,cs->bgqs', p_cmp, overlap)
        jcur = t // SEL_BLOCK
        forced = (js[None, :] == 0) | (js[None, :] == jcur[:, None]) | (js[None, :] == jcur[:, None] - 1)
        valid = js[None, :] <= jcur[:, None]
        score = jnp.where(forced, 1e6, jnp.where(valid, imp, -1e6))
        _, sel = lax.top_k(score, n_sel)
        ks = kb[bi, gi, sel]
        vs = vb[bi, gi, sel]
        s_pos = sel[..., None] * SEL_BLOCK + jnp.arange(SEL_BLOCK)
        ls = jnp.einsum('bqghd,bgqnrd->bghqnr', qg, ks).astype(jnp.float32)
        bias_s = table_g[gi[..., None], rel_bucket(t[:, None, None] - s_pos)]
        ls = ls + bias_s.transpose(0, 1, 5, 2, 3, 4)
        ksel = n_sel * SEL_BLOCK
        ls = ls.reshape(B, G, HPG, Q_BLOCK, ksel)
        ms = (s_pos <= t[:, None, None]).reshape(B, G, Q_BLOCK, ksel)[:, :, None]
        p_s = masked_softmax(ls, ms)
        o_slc = jnp.einsum('bghqk,bgqkd->bqghd', p_s.astype(vs.dtype),
                           vs.reshape(B, G, Q_BLOCK, ksel, dh))

        kwc = lax.dynamic_slice_in_dim(kw, c * Q_BLOCK, kw_len, axis=1)
        vwc = lax.dynamic_slice_in_dim(vw, c * Q_BLOCK, kw_len, axis=1)
        s_w = c * Q_BLOCK - WINDOW + jnp.arange(kw_len)
        dist = t[:, None] - s_w[None, :]
        mw = (s_w[None, :] >= 0) & (dist >= 0) & (dist < WINDOW)
        lw = jnp.einsum('bqghd,bkgd->bghqk', qg, kwc).astype(jnp.float32)
        lw = lw + rel_table[rel_bucket(dist)].reshape(Q_BLOCK, kw_len, G, HPG).transpose(2, 3, 0, 1)
        p_w = masked_softmax(lw, mw)
        o_win = jnp.einsum('bghqk,bkgd->bqghd', p_w.astype(vwc.dtype), vwc)

        o = jnp.stack([o_cmp, o_slc, o_win], axis=-1)
        g = gc.reshape(B, Q_BLOCK, G, HPG, 3)[..., None, :]
        return jnp.sum(o * g, axis=-1).reshape(B, Q_BLOCK, H * dh).astype(q.dtype)

    out = lax.map(block_fn, (jnp.arange(nqb), qs, gs))
    return out.swapaxes(0, 1).reshape(B, S, H * dh)


def gla_attention(q, k, v, log_a):
    B, S, Hb, dk = q.shape
    dv = v.shape[-1]
    C = GLA_CHUNK
    n = S // C

    def chunks(a):
        return a.reshape(B, n, C, *a.shape[2:])

    qc, kc, vc, la = chunks(q * (dk ** -0.5)), chunks(k), chunks(v), chunks(log_a)
    b = jnp.cumsum(la.astype(jnp.float32), axis=2)
    b_last = b[:, :, -1:]
    q_dec = qc * jnp.exp(b)
    k_intra = kc * jnp.exp(-b)
    k_state = kc * jnp.exp(b_last - b)
    causal = jnp.tril(jnp.ones((C, C), dtype=bool))
    A = jnp.where(causal, jnp.einsum('bnihd,bnjhd->bnhij', q_dec, k_intra), 0.0)
    o_intra = jnp.einsum('bnhij,bnjhv->bnihv', A, vc.astype(jnp.float32))
    decay_chunk = jnp.exp(b_last[:, :, 0])

    def step(state, inp):
        qd, ksd, vcc, dch = inp
        o = jnp.einsum('bihd,bhdv->bihv', qd, state)
        state = dch[..., None] * state + jnp.einsum('bjhd,bjhv->bhdv', ksd, vcc.astype(jnp.float32))
        return state, o

    s0 = jnp.zeros((B, Hb, dk, dv), jnp.float32)
    _, o_inter = lax.scan(step, s0, (q_dec.swapaxes(0, 1), k_state.swapaxes(0, 1),
                                     vc.swapaxes(0, 1), decay_chunk.swapaxes(0, 1)))
    o = o_intra + o_inter.swapaxes(0, 1)
    return o.reshape(B, S, Hb, dv).astype(v.dtype)


def setup_inputs(seed: int = 0) -> dict:
    key = jax.random.key(seed)
    ks = jax.random.split(key, 32)
    f32 = jnp.float32
    L, D = DEPTH, D_MODEL

    def w(k, shape, fan_in):
        return jax.random.normal(k, shape, f32) * fan_in ** -0.5

    def gain(k, shape):
        return 1.0 + 0.05 * jax.random.normal(k, shape, f32)

    return {
        "x": jax.random.normal(ks[0], (BATCH, SEQ, D), f32),
        "rel_table": 0.2 * jax.random.normal(ks[1], (REL_BUCKETS, NSA_HEADS), f32),
        "ffn1_norm": gain(ks[2], (L, D)),
        "ffn1_w_gate": w(ks[3], (L, D, D_FF), D),
        "ffn1_w_up": w(ks[4], (L, D, D_FF), D),
        "ffn1_w_down": w(ks[5], (L, D_FF, D), D_FF),
        "mix_norm": gain(ks[6], (L, D)),
        "w_in": w(ks[7], (L, D, D_IN), D),
        "cmp_pos_k": 0.5 * jax.random.normal(ks[8], (L, CMP_BLOCK, NSA_DH), f32),
        "cmp_pos_v": 0.5 * jax.random.normal(ks[9], (L, CMP_BLOCK, NSA_DH), f32),
        "cmp_k_w1": w(ks[10], (L, CMP_BLOCK * NSA_DH, CMP_HIDDEN), CMP_BLOCK * NSA_DH),
        "cmp_k_w2": w(ks[11], (L, CMP_HIDDEN, NSA_DH), CMP_HIDDEN),
        "cmp_v_w1": w(ks[12], (L, CMP_BLOCK * NSA_DH, CMP_HIDDEN), CMP_BLOCK * NSA_DH),
        "cmp_v_w2": w(ks[13], (L, CMP_HIDDEN, NSA_DH), CMP_HIDDEN),
        "gla_a_w2": w(ks[14], (L, GLA_RANK, GLA_HEADS * GLA_DK), GLA_RANK),
        "gla_a_b": 0.1 * jax.random.normal(ks[15], (L, GLA_HEADS * GLA_DK), f32),
        "gla_out_norm": gain(ks[16], (L, GLA_HEADS * GLA_DV)),
        "w_branch_nsa": w(ks[17], (L, NSA_HEADS * NSA_DH, D), NSA_HEADS * NSA_DH),
        "w_branch_gla": w(ks[18], (L, GLA_HEADS * GLA_DV, D), GLA_HEADS * GLA_DV),
        "w_out": w(ks[19], (L, D, D), D),
        "ffn2_norm": gain(ks[20], (L, D)),
        "ffn2_w_gate": w(ks[21], (L, D, D_FF), D),
        "ffn2_w_up": w(ks[22], (L, D, D_FF), D),
        "ffn2_w_down": w(ks[23], (L, D_FF, D), D_FF),
        "final_norm": gain(ks[24], (D,)),
    }


def reference(x, rel_table, ffn1_norm, ffn1_w_gate, ffn1_w_up, ffn1_w_down, mix_norm, w_in,
              cmp_pos_k, cmp_pos_v, cmp_k_w1, cmp_k_w2, cmp_v_w1, cmp_v_w2, gla_a_w2, gla_a_b,
              gla_out_norm, w_branch_nsa, w_branch_gla, w_out, ffn2_norm, ffn2_w_gate, ffn2_w_up,
              ffn2_w_down, final_norm):
    B, S, D = x.shape
    offsets = np.cumsum(W_IN_SPLITS)[:-1].tolist()
    for l in range(DEPTH):
        x = x + 0.5 * swiglu(rmsnorm(x, ffn1_norm[l]), ffn1_w_gate[l], ffn1_w_up[l], ffn1_w_down[l])

        h = rmsnorm(x, mix_norm[l])
        proj = h @ w_in[l]
        q_a, kv_a, g_a, q_b, k_b, v_b, a_lr, r_b, g_merge = jnp.split(proj, offsets, axis=-1)

        q_a = q_a.reshape(B, S, NSA_HEADS, NSA_DH)
        kv_a = kv_a.reshape(B, S, 6, NSA_GROUPS, NSA_DH)
        k_cmp = nsa_compress(kv_a[:, :, 0], cmp_pos_k[l], cmp_k_w1[l], cmp_k_w2[l])
        v_cmp = nsa_compress(kv_a[:, :, 1], cmp_pos_v[l], cmp_v_w1[l], cmp_v_w2[l])
        gates_a = jax.nn.sigmoid(g_a).reshape(B, S, NSA_HEADS, 3)
        o_a = nsa_attention(q_a, k_cmp, v_cmp, kv_a[:, :, 2], kv_a[:, :, 3],
                            kv_a[:, :, 4], kv_a[:, :, 5], gates_a, rel_table)

        log_a = jax.nn.log_sigmoid((a_lr @ gla_a_w2[l] + gla_a_b[l]).astype(jnp.float32)) / GLA_TAU
        o_b = gla_attention(q_b.reshape(B, S, GLA_HEADS, GLA_DK),
                            k_b.reshape(B, S, GLA_HEADS, GLA_DK),
                            v_b.reshape(B, S, GLA_HEADS, GLA_DV),
                            log_a.reshape(B, S, GLA_HEADS, GLA_DK))
        o_b = rmsnorm(o_b, gla_out_norm[l].reshape(GLA_HEADS, GLA_DV))
        o_b = o_b.reshape(B, S, GLA_HEADS * GLA_DV) * jax.nn.silu(r_b)

        gm = jax.nn.sigmoid(g_merge).reshape(B, S, 2, D)
        y = gm[:, :, 0] * (o_a @ w_branch_nsa[l]) + gm[:, :, 1] * (o_b @ w_branch_gla[l])
        x = x + y @ w_out[l]

        x = x + 0.5 * swiglu(rmsnorm(x, ffn2_norm[l]), ffn2_w_gate[l], ffn2_w_up[l], ffn2_w_down[l])
    return rmsnorm(x, final_norm)
```

```python
import numpy as np
from contextlib import ExitStack
import concourse.bass as bass
import concourse.mybir as mybir
from concourse.bass_utils import run_bass_kernel_spmd

F32 = mybir.dt.float32
BF16 = mybir.dt.bfloat16
AF = mybir.ActivationFunctionType
ALU = mybir.AluOpType
AX = mybir.AxisListType

D = 1024
DFF = 2816
S = 4096
NB = 4
DEPTH = 4
NTOK = 2048
KD = D // 128
KF = DFF // 128
EPS = 1e-6
D_IN = 6440
OFF_QA = 0
OFF_KVA = 512
OFF_GA = 512 + 768
OFF_QB = OFF_GA + 24
OFF_KB = OFF_QB + 512
OFF_VB = OFF_KB + 512
OFF_ALR = OFF_VB + 1024
OFF_RB = OFF_ALR + 16
OFF_GM = OFF_RB + 1024
assert OFF_GM + 2048 == D_IN

ENGS = ("pe", "act", "dve", "pool", "sp")


class Op:
    __slots__ = ("eng", "fn", "waits", "pos", "marked", "dma_key", "dma_cnt", "dma_inc")

    def __init__(self, eng, fn):
        self.eng = eng
        self.fn = fn
        self.waits = []
        self.pos = None
        self.marked = False
        self.dma_key = None
        self.dma_cnt = 0
        self.dma_inc = 16


class Prog:
    def __init__(self, nc):
        self.nc = nc
        self.ops = {e: [] for e in ENGS}
        self.last_writer = {}
        self.readers = {}
        self.waited = {e: {} for e in ENGS}
        self.dma_counts = {}
        self.same_engine_sync = {"act": True, "dve": True, "pool": True, "pe": False, "sp": False}

    def _add_wait(self, op, prod):
        e = op.eng
        if prod.dma_key is not None:
            k = ("dma", prod.dma_key)
            if self.waited[e].get(k, 0) >= prod.dma_cnt:
                return
            self.waited[e][k] = prod.dma_cnt
            op.waits.append(("dma", prod.dma_key, prod.dma_cnt))
        else:
            if prod.eng == e and not self.same_engine_sync[e]:
                return
            k = ("eng", prod.eng)
            if self.waited[e].get(k, -1) >= prod.pos:
                return
            self.waited[e][k] = prod.pos
            prod.marked = True
            op.waits.append(("eng", prod.eng, prod.pos))

    def op(self, eng, fn, reads=(), writes=(), dma_key=None, dma_inc=16):
        o = Op(eng, fn)
        o.pos = len(self.ops[eng])
        if dma_key is not None:
            o.dma_key = dma_key
            o.dma_inc = dma_inc
            self.dma_counts[dma_key] = self.dma_counts.get(dma_key, 0) + dma_inc
            o.dma_cnt = self.dma_counts[dma_key]
        for k in reads:
            w = self.last_writer.get(k)
            if w is not None:
                self._add_wait(o, w)
        for k in writes:
            w = self.last_writer.get(k)
            if w is not None:
                self._add_wait(o, w)
            for r in self.readers.get(k, ()):
                self._add_wait(o, r)
        for k in reads:
            self.readers.setdefault(k, []).append(o)
        for k in writes:
            self.last_writer[k] = o
            self.readers[k] = []
        self.ops[eng].append(o)
        return o

    def wait_all(self, eng, keys):
        o = Op(eng, None)
        o.pos = len(self.ops[eng])
        for k in keys:
            w = self.last_writer.get(k)
            if w is not None:
                self._add_wait(o, w)
        self.ops[eng].append(o)
        return o

    def barrier(self):
        lasts = {e: (self.ops[e][-1] if self.ops[e] else None) for e in ENGS}
        last_comp = {}
        for e in ENGS:
            for o in reversed(self.ops[e]):
                if o.fn is not None and o.dma_key is None:
                    last_comp[e] = o
                    break
        dma_now = dict(self.dma_counts)
        for e in ENGS:
            o = Op(e, None)
            o.pos = len(self.ops[e])
            for pe_, lo in last_comp.items():
                if pe_ == e and e in ("pe", "sp"):
                    continue
                k = ("eng", pe_)
                if self.waited[e].get(k, -1) >= lo.pos:
                    continue
                self.waited[e][k] = lo.pos
                lo.marked = True
                o.waits.append(("eng", pe_, lo.pos))
            for dk, c in dma_now.items():
                k = ("dma", dk)
                if self.waited[e].get(k, 0) >= c:
                    continue
                self.waited[e][k] = c
                o.waits.append(("dma", dk, c))
            self.ops[e].append(o)

    def emit(self, stack):
        nc = self.nc
        sems = {e: stack.enter_context(nc.semaphore("s_" + e)) for e in ENGS}
        dsems = {k: stack.enter_context(nc.semaphore("d_" + str(i))) for i, k in enumerate(self.dma_counts)}
        cnt = {}
        for e in ENGS:
            c = 0
            for o in self.ops[e]:
                if o.marked:
                    c += 1
                cnt[(e, o.pos)] = c
        block = stack.enter_context(nc.Block())

        def run(e, engobj):
            for o in self.ops[e]:
                for w in o.waits:
                    if w[0] == "dma":
                        engobj.wait_ge(dsems[w[1]], w[2])
                    else:
                        engobj.wait_ge(sems[w[1]], cnt[(w[1], w[2])])
                if o.fn is None:
                    continue
                ins = o.fn(engobj)
                if o.dma_key is not None:
                    if o.dma_inc == 1:
                        ins.then_inc(dsems[o.dma_key])
                    else:
                        ins.then_inc(dsems[o.dma_key], o.dma_inc)
                elif o.marked:
                    ins.then_inc(sems[e], 1)

        @block.tensor
        def _(eng):
            run("pe", eng)

        @block.scalar
        def _(eng):
            run("act", eng)

        @block.vector
        def _(eng):
            run("dve", eng)

        @block.gpsimd
        def _(eng):
            run("pool", eng)

        @block.sync
        def _(eng):
            run("sp", eng)


class Ctx:
    def __init__(self, nc, stack):
        self.nc = nc
        self.stack = stack
        self.P = Prog(nc)
        self.ps_i = 0
        self.uid = 0

    def sb(self, name, shape, dt, stack=None):
        self.uid += 1
        return (stack or self.stack).enter_context(self.nc.sbuf_tensor("sb%d_%s" % (self.uid, name), list(shape), dt))

    def psum_banks(self):
        self.ps = [self.stack.enter_context(self.nc.psum_tensor("ps%d" % i, [128, 512], F32)) for i in range(8)]

    def next_ps(self):
        banks = getattr(self, "gen_banks", list(range(8)))
        held = getattr(self, "held", ())
        while True:
            i = banks[self.ps_i % len(banks)]
            self.ps_i += 1
            if i not in held:
                return i


def dram_w(nc, name, shape, dt=F32):
    return nc.dram_tensor(name, list(shape), dt, kind="ExternalInput").ap()


TS = 1024
TT = 512


def emit_rmsnorm_tile(C, xT, t0, g_col, hT, hkey, ones_bf, tmp, xkeys, gkey="gains"):
    P = C.P
    sq, rstd, eps_col = tmp["sq"], tmp["rstd"], tmp["epsc"]
    P.op("act", lambda e: e.activation(out=sq[:, :, :], in_=xT[:, :, t0:t0 + TT], func=AF.Square),
         reads=xkeys, writes=["sq"])
    pi = C.next_ps()
    ps = C.ps[pi]
    for k in range(KD):
        P.op("pe", lambda e, k=k: e.matmul(ps[:, :], lhsT=ones_bf[:, :], rhs=sq[:, k, :], start=(k == 0), stop=(k == KD - 1)),
             reads=["sq", "ones"], writes=[("ps", pi)])
    P.op("act", lambda e: e.activation(out=rstd[:, :], in_=ps[:, :], func=AF.Sqrt, bias=eps_col[:, 0:1], scale=1.0 / D),
         reads=[("ps", pi), "epsc"], writes=["rstd"])
    P.op("dve", lambda e: e.reciprocal(out=rstd[:, :], in_=rstd[:, :]),
         reads=["rstd"], writes=["rstd"])
    for k in range(KD):
        P.op("dve", lambda e, k=k: e.scalar_tensor_tensor(out=hT[:, k, :], in0=xT[:, k, t0:t0 + TT], scalar=g_col[:, k:k + 1],
                                                          in1=rstd[:, :], op0=ALU.mult, op1=ALU.mult),
             reads=xkeys + ["rstd", "gains"], writes=[hkey])


def emit_ffn(C, xT, g_col, wg, wu, wd, tiles, lname):
    P = C.P
    nc = C.nc
    hT, act = tiles["hT"], tiles["act"]
    wgu, wdn = tiles["wgu"], tiles["wdn"]
    ones_bf = tiles["ones"]
    sgs = tiles["sg"]
    wg_v = wg.rearrange("(k p) f -> p k f", p=128)
    wu_v = wu.rearrange("(k p) f -> p k f", p=128)
    wd_v = wd.rearrange("(f p) d -> p f d", p=128)
    NG = KF // 2
    st = tiles["state"]
    for ts in range(NTOK // TS):
        xk = [("xT", ts)]
        for tt in range(TS // TT):
            emit_rmsnorm_tile(C, xT, ts * TS + tt * TT, g_col, hT[tt], ("hT", tt), ones_bf, tiles, xk)
        for g in range(NG):
            slot = st["gu"] % 2
            st["gu"] += 1
            w = wgu[slot]
            P.op("pool", lambda e, w=w, g=g: e.dma_start(out=w[:, 0, :, :], in_=wg_v[:, :, g * 256:(g + 1) * 256]),
                 writes=[("wgu", slot)], dma_key=("wgu", slot))
            P.op("pool", lambda e, w=w, g=g: e.dma_start(out=w[:, 1, :, :], in_=wu_v[:, :, g * 256:(g + 1) * 256]),
                 writes=[("wgu", slot)], dma_key=("wgu", slot))
            for fi in range(2):
                f = g * 2 + fi
                for tt in range(TS // TT):
                    pg, pu = C.next_ps(), C.next_ps()
                    for k in range(KD):
                        P.op("pe", lambda e, k=k, pg=pg, w=w, fi=fi, tt=tt: e.matmul(
                            C.ps[pg][:, :], lhsT=w[:, 0, k, fi * 128:(fi + 1) * 128], rhs=hT[tt][:, k, :],
                            start=(k == 0), stop=(k == KD - 1)),
                            reads=[("wgu", slot), ("hT", tt)], writes=[("ps", pg)])
                    for k in range(KD):
                        P.op("pe", lambda e, k=k, pu=pu, w=w, fi=fi, tt=tt: e.matmul(
                            C.ps[pu][:, :], lhsT=w[:, 1, k, fi * 128:(fi + 1) * 128], rhs=hT[tt][:, k, :],
                            start=(k == 0), stop=(k == KD - 1)),
                            reads=[("wgu", slot), ("hT", tt)], writes=[("ps", pu)])
                    si = st["sg"] % len(sgs)
                    st["sg"] += 1
                    sg = sgs[si]
                    P.op("act", lambda e, sg=sg, pg=pg: e.activation(out=sg[:, :], in_=C.ps[pg][:, :], func=AF.Silu),
                         reads=[("ps", pg)], writes=[("sg", si)])
                    P.op("dve", lambda e, sg=sg, pu=pu, f=f, tt=tt: e.tensor_tensor(
                        out=act[:, f, tt * TT:(tt + 1) * TT], in0=C.ps[pu][:, :], in1=sg[:, :], op=ALU.mult),
                        reads=[("ps", pu), ("sg", si)], writes=[("act", f, tt)])
        for dg in range(4):
            slot = st["dn"] % 2
            st["dn"] += 1
            w = wdn[slot]
            P.op("pool", lambda e, w=w, dg=dg: e.dma_start(out=w[:, :, :], in_=wd_v[:, :, dg * 256:(dg + 1) * 256]),
                 writes=[("wdn", slot)], dma_key=("wdn", slot))
            for di in range(2):
                dch = dg * 2 + di
                for tt in range(TS // TT):
                    pd = C.next_ps()
                    for f in range(KF):
                        P.op("pe", lambda e, f=f, pd=pd, w=w, di=di, tt=tt: e.matmul(
                            C.ps[pd][:, :], lhsT=w[:, f, di * 128:(di + 1) * 128], rhs=act[:, f, tt * TT:(tt + 1) * TT],
                            start=(f == 0), stop=(f == KF - 1)),
                            reads=[("wdn", slot), ("act", f, tt)], writes=[("ps", pd)])
                    t0 = ts * TS + tt * TT
                    P.op("dve", lambda e, pd=pd, dch=dch, t0=t0: e.scalar_tensor_tensor(
                        out=xT[:, dch, t0:t0 + TT], in0=C.ps[pd][:, :], scalar=0.5, in1=xT[:, dch, t0:t0 + TT],
                        op0=ALU.mult, op1=ALU.add),
                        reads=[("ps", pd), ("xT", ts)], writes=[("xT", ts)])


def alloc_ffn_tiles(C, stack):
    t = {}
    t["hT"] = [C.sb("hT%d" % i, [128, KD, TT], BF16, stack) for i in range(TS // TT)]
    t["act"] = C.sb("act", [128, KF, TS], BF16, stack)
    t["wgu"] = [C.sb("wgu%d" % i, [128, 2, KD, 256], BF16, stack) for i in range(2)]
    t["wdn"] = [C.sb("wdn%d" % i, [128, KF, 256], BF16, stack) for i in range(2)]
    t["sg"] = [C.sb("sg%d" % i, [128, TT], BF16, stack) for i in range(3)]
    t["state"] = {"gu": 0, "dn": 0, "sg": 0}
    return t


def build_ffn_test():
    nc = bass.Bass("TRN2", target_bir_lowering=False)
    x_in = dram_w(nc, "xT_in", [128, KD, NTOK])
    gain = dram_w(nc, "gain", [128, KD])
    wg = dram_w(nc, "wg", [D, DFF])
    wu = dram_w(nc, "wu", [D, DFF])
    wd = dram_w(nc, "wd", [DFF, D])
    y = nc.dram_tensor("xT_out", [128, KD, NTOK], F32, kind="ExternalOutput").ap()
    with ExitStack() as stack:
        C = Ctx(nc, stack)
        C.psum_banks()
        xT = C.sb("xT", [128, KD, NTOK], F32)
        g_col = C.sb("g_col", [128, KD], F32)
        ones = C.sb("ones", [128, 128], BF16)
        P = C.P
        P.op("pool", lambda e: e.memset(ones[:, :], 1.0), writes=["ones"])
        for ts in range(NTOK // TS):
            P.op("sp", lambda e, ts=ts: e.dma_start(out=xT[:, :, ts * TS:(ts + 1) * TS], in_=x_in[:, :, ts * TS:(ts + 1) * TS]),
                 writes=[("xT", ts)], dma_key=("xT", ts))
        P.op("sp", lambda e: e.dma_start(out=g_col[:, :], in_=gain[:, :]), writes=["gains"], dma_key="gains")
        tiles = alloc_ffn_tiles(C, stack)
        tiles["sq"] = C.sb("sq", [128, KD, TT], BF16)
        tiles["rstd"] = C.sb("rstd", [128, TT], F32)
        tiles["ones"] = ones
        epsc = C.sb("epsc", [128, 1], F32)
        P.op("pool", lambda e: e.memset(epsc[:, :], EPS), writes=["epsc"])
        tiles["epsc"] = epsc
        emit_ffn(C, xT, g_col, wg, wu, wd, tiles, "ffn1")
        for ts in range(NTOK // TS):
            P.op("sp", lambda e, ts=ts: e.dma_start(out=y[:, :, ts * TS:(ts + 1) * TS], in_=xT[:, :, ts * TS:(ts + 1) * TS]),
                 reads=[("xT", ts)], writes=[("yout", ts)], dma_key=("yout", ts))
        P.wait_all("sp", [("yout", ts) for ts in range(NTOK // TS)])
        P.emit(stack)
    return nc


def to_feature_major(x2d):
    n = x2d.shape[0]
    return np.ascontiguousarray(x2d.T.reshape(KD, 128, n).transpose(1, 0, 2))


def from_feature_major(xT):
    n = xT.shape[2]
    return np.ascontiguousarray(xT.transpose(1, 0, 2).reshape(D, n).T)


def gain_cols(g):
    return np.ascontiguousarray(g.reshape(KD, 128).T)


QT128 = 128
NQT = S // 128
BLK = 512
NBLK = S // BLK
NEG = -30000.0
LC = 6656
OFFD = 2064
LW = 1024
OFFW = 128
CQ, CKC, CVC, CKS, CKW, CALR, CQB, CKBT = 0, 256, 384, 512, 576, 640, 656, 912
CFM_END = 1168
CVS, CVW, CGA, CKB, CVB, CRB = 1168, 1232, 1296, 1308, 1564, 2076
NCOLS = 2588


def rel_bucket_np(d):
    n = np.maximum(d, 0)
    nf = np.maximum(n, 1).astype(np.float32)
    log_b = 16 + (np.log(nf / np.float32(16)) / np.float32(np.log(1024 / 16)) * np.float32(16)).astype(np.int32)
    return np.where(n < 16, n, np.minimum(log_b, 31))


_CONSTS = {}


def mixer_consts():
    if _CONSTS:
        return _CONSTS
    c = {}
    dd = np.arange(LC) - OFFD
    b = rel_bucket_np(dd)
    oh = np.zeros((33, LC), np.float32)
    for i in range(LC):
        if dd[i] >= 0:
            oh[b[i], i] = 1.0
        else:
            oh[32, i] = 1.0
    c["oh_c"] = oh
    dd = np.arange(LW) - OFFW
    b = rel_bucket_np(dd)
    oh = np.zeros((33, LW), np.float32)
    for i in range(LW):
        if 0 <= dd[i] < 512:
            oh[b[i], i] = 1.0
        else:
            oh[32, i] = 1.0
    c["oh_w"] = oh
    o31 = np.zeros((33, 128), np.float32)
    o31[31, :] = 1.0
    c["oh31"] = o31
    ov = np.zeros((256, 64), np.float32)
    for p in range(1, 256):
        cc = p - 1
        for j in range(64):
            if (16 * cc < 64 * j + 64) and (16 * cc + 32 > 64 * j):
                ov[p, j] = 1.0
    c["ov"] = np.ascontiguousarray(ov.reshape(2, 128, 64).transpose(1, 0, 2))
    ex = np.zeros((64, S), np.float32)
    for s_ in range(S):
        ex[s_ // 64, s_] = 1.0
    c["exall"] = ex
    jj = np.arange(128)
    same = (jj[:, None] // 64) == (jj[None, :] // 64)
    c["tri_b"] = (np.where(same & (jj[:, None] <= jj[None, :]), -1.0 / 16.0, 0.0)).astype(np.float32)
    c["tri_u"] = (np.where(same & (jj[:, None] > jj[None, :]), -1.0 / 16.0, 0.0)).astype(np.float32)
    c["amask"] = (same & (jj[:, None] <= jj[None, :])).astype(np.float32)
    t = np.arange(S)
    jcur = t // 64
    js = np.arange(64)
    forced = (js[None, :] == 0) | (js[None, :] == jcur[:, None]) | (js[None, :] == jcur[:, None] - 1)
    valid = js[None, :] <= jcur[:, None]
    aadd = np.where(forced, 1e6 + 1000.0 * js[None, :], np.where(valid, 0.0, -1e6 - 1000.0 * js[None, :])).astype(np.float32)
    c["aadd"] = np.ascontiguousarray(aadd.reshape(NQT, 128, 64).transpose(1, 0, 2))
    _CONSTS.update(c)
    return c


MIX_CONST_SHAPES = {"oh_c": [33, LC], "oh_w": [33, LW], "oh31": [33, 128], "ov": [128, 2, 64], "exall": [64, S],
                    "tri_b": [128, 128], "tri_u": [128, 128], "amask": [128, 128], "aadd": [128, NQT, 64]}
MIX_W_SHAPES = {"wmix": [D, NCOLS], "w2a": [16, 256], "ba": [1, 256], "gnorm": [1, 512], "posk": [128, 16], "posv": [128, 16],
                "w1k": [128, 16, 256], "w1v": [128, 16, 256], "w2k": [128, 2, 64], "w2v": [128, 2, 64], "rel": [32, 4]}


def mixer_weight_inputs(inp, l, g):
    w = inp["w_in"][l]
    cols = []
    cols.append(w[:, OFF_QA + g * 256: OFF_QA + (g + 1) * 256])
    for i in (0, 0, 1, 1, 2, 4):
        cols.append(w[:, OFF_KVA + i * 128 + g * 64: OFF_KVA + i * 128 + (g + 1) * 64])
    cols.append(w[:, OFF_ALR:OFF_ALR + 16])
    cols.append(w[:, OFF_QB + g * 256: OFF_QB + (g + 1) * 256])
    cols.append(w[:, OFF_KB + g * 256: OFF_KB + (g + 1) * 256])
    for i in (3, 5):
        cols.append(w[:, OFF_KVA + i * 128 + g * 64: OFF_KVA + i * 128 + (g + 1) * 64])
    cols.append(w[:, OFF_GA + g * 12: OFF_GA + (g + 1) * 12])
    cols.append(w[:, OFF_KB + g * 256: OFF_KB + (g + 1) * 256])
    cols.append(w[:, OFF_VB + g * 512: OFF_VB + (g + 1) * 512])
    cols.append(w[:, OFF_RB + g * 512: OFF_RB + (g + 1) * 512])
    wmix = np.ascontiguousarray(np.concatenate(cols, axis=1))
    assert wmix.shape == (D, NCOLS)
    r = {"wmix": wmix}
    r["w2a"] = np.ascontiguousarray(inp["gla_a_w2"][l][:, g * 256:(g + 1) * 256])
    r["ba"] = np.ascontiguousarray(inp["gla_a_b"][l][None, g * 256:(g + 1) * 256])
    r["gnorm"] = np.ascontiguousarray(inp["gla_out_norm"][l][None, g * 512:(g + 1) * 512])
    r["posk"] = np.ascontiguousarray(inp["cmp_pos_k"][l].reshape(2, 16, 64).transpose(0, 2, 1).reshape(128, 16))
    r["posv"] = np.ascontiguousarray(inp["cmp_pos_v"][l].reshape(2, 16, 64).transpose(0, 2, 1).reshape(128, 16))
    r["w1k"] = np.ascontiguousarray(inp["cmp_k_w1"][l].reshape(2, 16, 64, 256).transpose(0, 2, 1, 3).reshape(128, 16, 256))
    r["w1v"] = np.ascontiguousarray(inp["cmp_v_w1"][l].reshape(2, 16, 64, 256).transpose(0, 2, 1, 3).reshape(128, 16, 256))
    r["w2k"] = np.ascontiguousarray(inp["cmp_k_w2"][l].reshape(2, 128, 64).transpose(1, 0, 2))
    r["w2v"] = np.ascontiguousarray(inp["cmp_v_w2"][l].reshape(2, 128, 64).transpose(1, 0, 2))
    r["rel"] = np.ascontiguousarray(inp["rel_table"][:, g * 4:(g + 1) * 4])
    return r


class MixerTiles:
    pass


def mixer_setup(C, cin, stack):
    nc, P = C.nc, C.P
    T = MixerTiles()
    sb = lambda n, s, d: C.sb(n, s, d, stack)
    T.ident_f = sb("ident_f", [128, 128], F32)
    T.ident_b = sb("ident_b", [128, 128], BF16)
    T.exall = sb("exall", [64, S], BF16)
    T.tri_b = sb("tri_b", [128, 128], F32)
    T.tri_u = sb("tri_u", [128, 128], F32)
    T.amask = sb("amask", [128, 128], F32)
    T.aadd = [sb("aadd%d" % i, [128, 64], F32) for i in range(2)]
    T.aadd_d = cin["aadd"]
    T.one_col = sb("one_col", [128, 1], F32)
    T.eps_col = sb("eps_col", [128, 1], F32)
    T.tiny_col = sb("tiny_col", [128, 1], F32)
    T.ones_f = sb("ones_f", [1, 128], F32)
    T.efar = sb("efar", [128, 4], F32)
    T.bsel = sb("bsel", [128, 8, 512], BF16)
    T.bwin = sb("bwin", [128, 5, 512], BF16)
    T.voc = sb("voc", [128, 2, 129], BF16)
    T.kcT = sb("kcT", [64, 256], BF16)
    P.op("pool", lambda e: e.memset(T.ident_f[:, :], 0.0), writes=["ident_f"])
    P.op("pool", lambda e: e.affine_select(out=T.ident_f[:, :], in_=T.ident_f[:, :], pattern=[[-1, 128]], compare_op=ALU.not_equal,
                                           fill=1.0, base=0, channel_multiplier=1), reads=["ident_f"], writes=["ident_f"])
    P.op("pool", lambda e: e.tensor_copy(out=T.ident_b[:, :], in_=T.ident_f[:, :]), reads=["ident_f"], writes=["ident_b"])
    P.op("pool", lambda e: e.memset(T.one_col[:, :], 1.0), writes=["one_col"])
    P.op("pool", lambda e: e.memset(T.eps_col[:, :], EPS), writes=["eps_col"])
    P.op("pool", lambda e: e.memset(T.tiny_col[:, :], 1e-30), writes=["tiny_col"])
    P.op("pool", lambda e: e.memset(T.ones_f[:, :], 1.0), writes=["ones_f"])
    P.op("pool", lambda e: e.memset(T.voc[:, :, :], 0.0), writes=["voc"])
    P.op("pool", lambda e: e.memset(T.kcT[:, :], 0.0), writes=["kcT"])
    P.op("pool", lambda e: e.memset(T.voc[:, :, 64:65], 1.0), reads=["voc"], writes=["voc"])
    P.op("pool", lambda e: e.memset(T.voc[0:1, 0, 64:65], 0.0), reads=["voc"], writes=["voc"])
    P.op("pool", lambda e: e.dma_start(out=T.voc[:, :, 65:129], in_=cin["ov"][:, :, :]), reads=["voc"], writes=["voc"], dma_key="voc_ov")
    P.op("pool", lambda e: e.dma_start(out=T.exall[:, :], in_=cin["exall"][:, :]), writes=["exall"], dma_key="exall")
    for nm in ("tri_b", "tri_u", "amask"):
        t = getattr(T, nm)
        P.op("sp", lambda e, t=t, nm=nm: e.dma_start(out=t[:, :], in_=cin[nm][:, :]), writes=[nm], dma_key=nm)
    relx = sb("relx", [33, 4], F32)
    ones33 = sb("ones33", [33, 128], F32)
    relb = sb("relb", [33, 4, 128], F32)
    ohb = [sb("ohb%d" % i, [33, 512], F32) for i in range(1)]
    oh31 = sb("oh31", [33, 128], F32)
    fsb = [sb("fsb%d" % i, [128, 512], BF16) for i in range(2)]
    P.op("sp", lambda e: e.dma_start(out=relx[0:32, :], in_=cin["rel"][:, :]), writes=["relx"], dma_key="relx")
    P.op("pool", lambda e: e.memset(relx[32:33, :], NEG), writes=["relx32"])
    P.op("pool", lambda e: e.memset(ones33[:, :], 1.0), writes=["ones33"])
    P.op("sp", lambda e: e.dma_start(out=oh31[:, :], in_=cin["oh31"][:, :]), writes=["oh31"], dma_key="oh31")
    pi = C.next_ps()
    P.op("pe", lambda e: e.matmul(C.ps[pi][:, 0:4], lhsT=oh31[:, :], rhs=relx[:, :], start=True, stop=True),
         reads=["oh31", "relx", "relx32"], writes=[("ps", pi)])
    P.op("act", lambda e: e.activation(out=T.efar[:, :], in_=C.ps[pi][:, 0:4], func=AF.Exp), reads=[("ps", pi)], writes=["efar"])
    for h in range(4):
        P.op("dve", lambda e, h=h: e.tensor_scalar(out=relb[:, h, :], in0=ones33[:, :], scalar1=relx[:, h:h + 1], scalar2=None, op0=ALU.mult),
             reads=["ones33", "relx", "relx32"], writes=["relb"])
    it = 0
    for tab, src_oh, L_, scr in (("c", cin["oh_c"], LC, cin["fc"]), ("w", cin["oh_w"], LW, cin["fw"])):
        for blk in range(L_ // 512):
            ob = ohb[0]
            okey = ("ohb", 0)
            P.op("sp", lambda e, ob=ob, blk=blk, src_oh=src_oh: e.dma_start(out=ob[:, :], in_=src_oh[:, blk * 512:(blk + 1) * 512]),
                 writes=[okey], dma_key=okey)
            for h in range(4):
                pi = C.next_ps()
                fb = fsb[it % 2]
                fkey = ("fsb", it % 2)
                par = it % 2
                it += 1
                P.op("pe", lambda e, pi=pi, ob=ob, h=h: e.matmul(C.ps[pi][:, :], lhsT=relb[:, h, :], rhs=ob[:, :], start=True, stop=True),
                     reads=["relb", okey], writes=[("ps", pi)])
                P.op("act", lambda e, pi=pi, fb=fb: e.activation(out=fb[:, :], in_=C.ps[pi][:, :], func=AF.Copy),
                     reads=[("ps", pi)], writes=[fkey])
                P.op("sp", lambda e, h=h, scr=scr, fb=fb, blk=blk: e.dma_start(out=scr[h, :, blk * 512:(blk + 1) * 512], in_=fb[:, :]),
                     reads=[fkey], writes=[("F", tab, par)], dma_key=("Fst", par))
    for dl in range(8):
        src = bass.AP(cin["fc"].tensor, OFFD + 128 * dl, [[LC - 1, 128], [128 * LC, 4], [1, 128]])
        P.op("sp", lambda e, dl=dl, src=src: e.dma_start(out=T.bsel[:, dl, :].rearrange("p (h t) -> p h t", h=4), in_=src),
             reads=[("F", "c", 0), ("F", "c", 1)], writes=["bsel"], dma_key="bsel")
    for dl in range(5):
        src = bass.AP(cin["fw"].tensor, OFFW + 128 * dl, [[LW - 1, 128], [128 * LW, 4], [1, 128]])
        P.op("sp", lambda e, dl=dl, src=src: e.dma_start(out=T.bwin[:, dl, :].rearrange("p (h t) -> p h t", h=4), in_=src),
             reads=[("F", "w", 0), ("F", "w", 1)], writes=["bwin"], dma_key="bwin")
    return T


def alloc_mixer_layer_tiles(C, stack):
    sb = lambda n, s, d: C.sb(n, s, d, stack)
    L = MixerTiles()
    L.wmix = sb("wmix", [128, KD, NCOLS], BF16)
    L.w2a = sb("w2a", [16, 256], F32)
    L.ba = sb("ba", [1, 256], F32)
    L.gnb = sb("gnb", [128, 512], F32)
    L.posk = sb("posk", [128, 16], BF16)
    L.posv = sb("posv", [128, 16], BF16)
    L.w1k = sb("w1k", [128, 16, 256], BF16)
    L.w1v = sb("w1v", [128, 16, 256], BF16)
    L.w2k = sb("w2k", [128, 2, 64], BF16)
    L.w2v = sb("w2v", [128, 2, 64], BF16)
    L.pbk = sb("pbk", [128, 2], F32)
    L.pbv = sb("pbv", [128, 2], F32)
    L.hTb = [sb("hTb%d" % i, [128, KD, BLK], BF16) for i in range(2)]
    L.ksT = sb("ksT", [64, S], BF16)
    L.kwT = sb("kwT", [64, S], BF16)
    L.kcrT = sb("kcrT", [128, 16 + S], BF16)
    L.vcrT = sb("vcrT", [128, 16 + S], BF16)
    L.vs = sb("vs", [128, NQT, 65], BF16)
    L.vw = sb("vw", [128, NQT, 65], BF16)
    L.qT = [sb("qT%d" % i, [64, 4, 4, 128], BF16) for i in range(2)]
    L.alrT = sb("alrT", [16, BLK], F32)
    L.qbT = sb("qbT", [128, 2, BLK], BF16)
    L.kbT = sb("kbT", [128, 2, BLK], BF16)
    L.hidk = sb("hidk", [128, 2, 32], BF16)
    L.hidv = sb("hidv", [128, 2, 32], BF16)
    L.vstage = [sb("vstage%d" % i, [32, 64], BF16) for i in range(2)]
    L.bcmp = [sb("bcmp%d" % i, [128, 2, 512], BF16) for i in range(2)]
    L.pt = [sb("pt%d" % i, [128, 512], BF16) for i in range(5)]
    L.msel = [sb("msel%d" % i, [64, 4, 128], BF16) for i in range(2)]
    L.gates = [sb("gates%d" % i, [128, 12], F32) for i in range(2)]
    L.kbtok = [sb("kbtok%d" % i, [128, 256], BF16) for i in range(2)]
    L.vb = [sb("vb%d" % i, [128, 512], BF16) for i in range(2)]
    L.gsr = [sb("gsr%d" % i, [128, 512], F32) for i in range(2)]
    L.ez = sb("ez", [128, 256], F32)
    L.sp = sb("sp", [128, 256], F32)
    L.eb = [sb("eb%d" % i, [128, 128], F32) for i in range(2)]
    L.enb = [sb("enb%d" % i, [128, 128], F32) for i in range(2)]
    L.erb = sb("erb", [128, 256], F32)
    L.kstate = sb("kstate", [128, 256], BF16)
    L.qdec = [sb("qdec%d" % i, [128, 128], BF16) for i in range(2)]
    L.kint = [sb("kint%d" % i, [128, 128], BF16) for i in range(2)]
    L.at = [sb("at%d" % i, [128, 128], BF16) for i in range(2)]
    L.state = sb("state", [128, 2, 256], F32)
    L.sbf = [[sb("sbf%d_%d" % (h, i), [128, 256], BF16) for i in range(2)] for h in range(2)]
    L.junk = sb("junk", [128, 256], F32)
    L.ssq = sb("ssq", [128, 2], F32)
    L.ob = sb("ob", [128, 512], F32)
    L.obT = [sb("obT%d" % i, [128, 4, 128], BF16) for i in range(2)]
    L.oacc = sb("oacc", [128, 4, 64], F32)
    L.oaT = [sb("oaT%d" % i, [128, 2, 128], BF16) for i in range(2)]
    L.onsb = sb("onsb", [128, 4, 65], F32)
    L.osel = sb("osel", [128, 4, 65], F32)
    L.small = sb("small", [128, 64], F32)
    L.imp = sb("imp", [128, 64], F32)
    L.sc = sb("sc", [128, 64], F32)
    L.sc2 = sb("sc2", [128, 64], F32)
    L.m8 = sb("m8", [128, 8], F32)
    L.nsel = sb("nsel", [128, 64], F32)
    L.cnt = {"pt": 0, "sbf": [0, 0]}
    return L


B_OF, B_ON, B_OW, B_OC, B_IMP = 3, 4, 5, 3, 4
MIX_GEN_BANKS = [0, 1, 2, 6, 7]


def emit_mixer_layer(C, T, L, din, hT_d, oT_d, lidx=0, dbg=None):
    nc, P = C.nc, C.P
    C.gen_banks = MIX_GEN_BANKS
    ps = C.ps
    lk = lambda name: (name, lidx)
    P.op("pool", lambda e: e.dma_start(out=L.wmix[:, :, :], in_=din["wmix"].rearrange("(k p) c -> p k c", p=128)),
         writes=["wmix"], dma_key="wmix")
    for nm, t in (("w1k", L.w1k), ("w1v", L.w1v)):
        P.op("pool", lambda e, nm=nm, t=t: e.dma_start(out=t[:, :, :], in_=din[nm][:, :, :]), writes=[nm], dma_key=nm)
    for nm, t in (("w2k", L.w2k), ("w2v", L.w2v)):
        P.op("pool", lambda e, nm=nm, t=t: e.dma_start(out=t[:, :, :], in_=din[nm][:, :, :]), writes=[nm], dma_key=nm)
    for nm, t in (("posk", L.posk), ("posv", L.posv)):
        P.op("pool", lambda e, nm=nm, t=t: e.dma_start(out=t[:, :], in_=din[nm][:, :]), writes=[nm], dma_key=nm)
    P.op("sp", lambda e: e.dma_start(out=L.w2a[:, :], in_=din["w2a"][:, :]), writes=["w2a"], dma_key="w2a")
    P.op("sp", lambda e: e.dma_start(out=L.ba[:, :], in_=din["ba"][:, :]), writes=["ba"], dma_key="ba")
    P.op("sp", lambda e: e.dma_start(out=L.gnb[:, :], in_=bass.AP(din["gnorm"].tensor, 0, [[0, 128], [1, 512]])), writes=["gnb"], dma_key="gnb")
    P.op("pool", lambda e: e.memset(L.state[:, :, :], 0.0), writes=["state0", "state1"])
    for h in range(2):
        P.op("pool", lambda e, h=h: e.memset(L.sbf[h][0][:, :], 0.0), writes=[("sbf", h, 0)])
    P.op("pool", lambda e: e.memset(L.kcrT[0:64, 0:16], 0.0), writes=["kcr_pad"])
    P.op("pool", lambda e: e.memset(L.vcrT[0:64, 0:16], 0.0), writes=["vcr_pad"])
    P.op("pool", lambda e: e.memset(L.vs[:, :, 64:65], 1.0), writes=["vs_one"])
    P.op("pool", lambda e: e.memset(L.vw[:, :, 64:65], 1.0), writes=["vw_one"])
    for (w1, pos, pb, nm) in ((L.w1k, L.posk, L.pbk, "k"), (L.w1v, L.posv, L.pbv, "v")):
        pi = C.next_ps()
        for hc in range(2):
            for l in range(16):
                P.op("pe", lambda e, w1=w1, pos=pos, hc=hc, l=l, pi=pi: e.matmul(
                    ps[pi][:, hc:hc + 1], lhsT=w1[:, l, hc * 128:(hc + 1) * 128], rhs=pos[:, l:l + 1],
                    start=(l == 0 and hc == 0), stop=(l == 15), skip_group_check=True),
                    reads=["w1" + nm, "pos" + nm], writes=[("ps", pi)])
        P.op("dve", lambda e, pb=pb, pi=pi: e.tensor_copy(out=pb[:, :], in_=ps[pi][:, 0:2]), reads=[("ps", pi)], writes=["pb" + nm])

    def proj_fm(dst_fn, col0, ncol, hb, hkey, wkeys, evac):
        pi = C.next_ps()
        for k in range(KD):
            P.op("pe", lambda e, k=k, pi=pi: e.matmul(ps[pi][0:ncol, :], lhsT=L.wmix[:, k, col0:col0 + ncol], rhs=hb[:, k, :],
                                                      start=(k == 0), stop=(k == KD - 1)),
                 reads=["wmix", hkey], writes=[("ps", pi)])
        evac(pi)

    for B in range(NBLK):
        T0 = B * BLK
        hb = L.hTb[B % 2]
        hkey = ("hTb", B % 2)
        qT = L.qT[B % 2]
        qkey = ("qT", B % 2)
        h_src = hT_d(B) if callable(hT_d) else hT_d[:, :, T0:T0 + BLK]
        P.op("sp", lambda e, hb=hb, h_src=h_src: e.dma_start(out=hb[:, :, :], in_=h_src), writes=[hkey], dma_key=hkey)
        for h in range(4):
            def ev(pi, h=h, qT=qT):
                P.op("dve", lambda e: e.tensor_scalar(out=qT[:, :, h, :], in0=ps[pi][0:64, :].rearrange("p (q t) -> p q t", q=4),
                                                      scalar1=0.125, scalar2=None, op0=ALU.mult),
                     reads=[("ps", pi)], writes=[qkey])
            proj_fm(None, CQ + 64 * h, 64, hb, hkey, None, ev)
        for (col, dst, key) in ((CKC, L.kcrT, "kcrT"), (CVC, L.vcrT, "vcrT")):
            def ev(pi, dst=dst, key=key, T0=T0):
                P.op("act", lambda e: e.activation(out=dst[0:64, 16 + T0: 16 + T0 + BLK], in_=ps[pi][0:64, :], func=AF.Copy),
                     reads=[("ps", pi)], writes=[(key, B)])
                P.op("dve", lambda e: e.tensor_copy(out=dst[64:128, T0: T0 + BLK], in_=ps[pi][64:128, :]),
                     reads=[("ps", pi)], writes=[(key + "u", B)])
            proj_fm(None, col, 128, hb, hkey, None, ev)
        for (col, dst, key) in ((CKS, L.ksT, "ksT"), (CKW, L.kwT, "kwT")):
            def ev(pi, dst=dst, key=key, T0=T0):
                P.op("act", lambda e: e.activation(out=dst[:, T0: T0 + BLK], in_=ps[pi][0:64, :], func=AF.Copy),
                     reads=[("ps", pi)], writes=[(key, B)])
            proj_fm(None, col, 64, hb, hkey, None, ev)

        def ev(pi):
            P.op("dve", lambda e: e.tensor_copy(out=L.alrT[:, :], in_=ps[pi][0:16, :]), reads=[("ps", pi)], writes=["alrT"])
        proj_fm(None, CALR, 16, hb, hkey, None, ev)
        for h in range(2):
            def ev(pi, h=h):
                P.op("act", lambda e: e.activation(out=L.qbT[:, h, :], in_=ps[pi][:, :], func=AF.Copy), reads=[("ps", pi)], writes=[("qbT", h)])
            proj_fm(None, CQB + 128 * h, 128, hb, hkey, None, ev)

            def ev2(pi, h=h):
                P.op("dve", lambda e: e.tensor_copy(out=L.kbT[:, h, :], in_=ps[pi][:, :]), reads=[("ps", pi)], writes=[("kbT", h)])
            proj_fm(None, CKBT + 128 * h, 128, hb, hkey, None, ev2)
        p0 = 32 * B
        for (w1, w2, raw, rkey, pb, hid, nm) in ((L.w1k, L.w2k, L.kcrT, "kcrT", L.pbk, L.hidk, "k"), (L.w1v, L.w2v, L.vcrT, "vcrT", L.pbv, L.hidv, "v")):
            rk = [(rkey, B), (rkey + "u", B), rkey[0:3] + "_pad"] + ([(rkey, B - 1), (rkey + "u", B - 1)] if B > 0 else [])
            for hc in range(2):
                pi = C.next_ps()
                for l in range(16):
                    rhs = raw[:, 16 * p0 + l: 16 * p0 + l + 16 * 31 + 1: 16]
                    P.op("pe", lambda e, pi=pi, l=l, hc=hc, rhs=rhs, w1=w1: e.matmul(
                        ps[pi][:, 0:32], lhsT=w1[:, l, hc * 128:(hc + 1) * 128], rhs=rhs, start=(l == 0), stop=(l == 15)),
                        reads=rk + ["w1" + nm], writes=[("ps", pi)])
                P.op("act", lambda e, pi=pi, hc=hc, hid=hid, pb=pb: e.activation(out=hid[:, hc, :], in_=ps[pi][:, 0:32], func=AF.Silu,
                                                                                 bias=pb[:, hc:hc + 1]),
                     reads=[("ps", pi), "pb" + nm], writes=[("hid" + nm, hc)])
            pi = C.next_ps()
            if nm == "k":
                for hc in range(2):
                    P.op("pe", lambda e, pi=pi, hc=hc: e.matmul(ps[pi][0:64, 0:32], lhsT=L.w2k[:, hc, :], rhs=L.hidk[:, hc, :],
                                                                start=(hc == 0), stop=(hc == 1)),
                         reads=[("hidk", hc), "w2k"], writes=[("ps", pi)])
                P.op("dve", lambda e, pi=pi, p0=p0: e.tensor_copy(out=T.kcT[:, p0:p0 + 32], in_=ps[pi][0:64, 0:32]), reads=[("ps", pi)], writes=["kcT"])
            else:
                ct, r0 = p0 // 128, p0 % 128
                vst = L.vstage[B % 2]
                for hc in range(2):
                    P.op("pe", lambda e, pi=pi, hc=hc: e.matmul(ps[pi][0:32, 0:64], lhsT=L.hidv[:, hc, :], rhs=L.w2v[:, hc, :],
                                                                start=(hc == 0), stop=(hc == 1)),
                         reads=[("hidv", hc), "w2v"], writes=[("ps", pi)])
                P.op("dve", lambda e, pi=pi, vst=vst: e.tensor_copy(out=vst[:, :], in_=ps[pi][0:32, 0:64]),
                     reads=[("ps", pi)], writes=[("vstage", B % 2)])
                P.op("sp", lambda e, ct=ct, r0=r0, vst=vst: e.dma_start(out=T.voc[r0:r0 + 32, ct, 0:64], in_=vst[:, :]),
                     reads=[("vstage", B % 2)], writes=["voc"], dma_key=("vocst", B % 2))
                if B == 0:
                    P.op("pool", lambda e: e.memset(T.voc[0:1, 0, 0:64], 0.0), reads=["voc"], writes=["voc"])
        for tq in range(4):
            qi = 4 * B + tq
            t0 = qi * 128
            par = qi % 2
            pA, pB_, pC = C.next_ps(), C.next_ps(), C.next_ps()
            for (pi, col, ncol) in ((pA, CVS, CVB - CVS), (pB_, CVB, 512), (pC, CRB, 512)):
                for k in range(KD):
                    P.op("pe", lambda e, k=k, pi=pi, col=col, ncol=ncol, hb=hb, tq=tq: e.matmul(
                        ps[pi][:, 0:ncol], lhsT=hb[:, k, tq * 128:(tq + 1) * 128], rhs=L.wmix[:, k, col:col + ncol],
                        start=(k == 0), stop=(k == KD - 1)),
                        reads=["wmix", hkey], writes=[("ps", pi)])
            P.op("dve", lambda e, pA=pA, qi=qi: e.tensor_copy(out=L.vs[:, qi, 0:64], in_=ps[pA][:, 0:64]), reads=[("ps", pA)], writes=[("vs", qi)])
            P.op("dve", lambda e, pA=pA, qi=qi: e.tensor_copy(out=L.vw[:, qi, 0:64], in_=ps[pA][:, 64:128]), reads=[("ps", pA)], writes=[("vw", qi)])
            P.op("act", lambda e, pA=pA, par=par: e.activation(out=L.gates[par][:, :], in_=ps[pA][:, 128:140], func=AF.Sigmoid),
                 reads=[("ps", pA)], writes=[("gates", par)])
            P.op("dve", lambda e, pA=pA, par=par: e.tensor_copy(out=L.kbtok[par][:, :], in_=ps[pA][:, 140:396]), reads=[("ps", pA)],
                 writes=[("kbtok", par)])
            P.op("act", lambda e, pB_=pB_, par=par: e.activation(out=L.vb[par][:, :], in_=ps[pB_][:, :], func=AF.Copy), reads=[("ps", pB_)],
                 writes=[("vb", par)])
            P.op("act", lambda e, pC=pC, par=par: e.activation(out=L.gsr[par][:, :], in_=ps[pC][:, :], func=AF.Silu), reads=[("ps", pC)],
                 writes=[("gsr", par)])
            P.op("pool", lambda e, par=par: e.tensor_tensor(out=L.gsr[par][:, :], in0=L.gsr[par][:, :], in1=L.gnb[:, :], op=ALU.mult),
                 reads=[("gsr", par), "gnb"], writes=[("gsr", par)])
            gla = emit_gla_tile(C, T, L, B, tq, oT_d)
            emit_nsa_tile(C, T, L, B, tq, qT, qkey, oT_d, filler=gla)
            for _ in gla:
                pass


def emit_nsa_tile(C, T, L, B, tq, qT, qkey, oT_d, filler=None):
    nc, P, ps = C.nc, C.P, C.ps
    qi = 4 * B + tq
    t0 = qi * 128
    par = qi % 2
    q_rhs = qT[:, tq, :, :].rearrange("p h t -> p (h t)")
    gates = L.gates[par]
    gkey = ("gates", par)
    sm = L.small
    LA = 2

    def fill():
        if filler is not None:
            next(filler, None)

    def next_pt():
        i = L.cnt["pt"] % len(L.pt)
        L.cnt["pt"] += 1
        return i

    def pv(bank, first, pti, vtile, vkeys, ncol=65):
        for h in range(4):
            P.op("pe", lambda e, h=h: e.matmul(ps[bank][:, h * ncol:(h + 1) * ncol], lhsT=L.pt[pti][:, h * 128:(h + 1) * 128], rhs=vtile,
                                               start=(first and h == 0), stop=True, skip_group_check=True),
                 reads=[("pt", pti)] + vkeys, writes=[("ps", bank)])

    def run_pairs(pairs):
        n = len(pairs)
        for i in range(n + LA):
            if i < n:
                pairs[i][0]()
                fill()
            if i >= LA:
                pairs[i - LA][1]()

    nct = 1 if qi < 16 else 2
    bc = L.bcmp[par]
    for ct in range(nct):
        src = bass.AP(oT_d["fc"].tensor, oT_d["fc"].offset + OFFD + t0 - 2048 * ct - 15, [[LC - 16, 128], [128 * LC, 4], [1, 128]])
        P.op("sp", lambda e, ct=ct, src=src: e.dma_start(out=bc[:, ct, :].rearrange("p (h t) -> p h t", h=4), in_=src),
             reads=oT_d.get("fkeys", [("F", "c", 0), ("F", "c", 1)]), writes=[("bcmp", par, ct)], dma_key=("bcmp", par, ct))
    P.op("sp", lambda e: e.dma_start(out=T.aadd[par][:, :], in_=T.aadd_d[:, qi, :]), writes=[("aadd", par)], dma_key=("aadd", par))
    cpt = []
    for ct in range(nct):
        pi = C.next_ps()
        P.op("pe", lambda e, pi=pi, ct=ct: e.matmul(ps[pi][:, :], lhsT=T.kcT[:, ct * 128:(ct + 1) * 128], rhs=q_rhs, start=True, stop=False),
             reads=["kcT", qkey], writes=[("ps", pi)])
        P.op("pe", lambda e, pi=pi, ct=ct: e.matmul(ps[pi][:, :], lhsT=T.ident_b[:, :], rhs=bc[:, ct, :], start=False, stop=True),
             reads=["ident_b", ("bcmp", par, ct)], writes=[("ps", pi)])
        pti = next_pt()
        cpt.append(pti)
        P.op("act", lambda e, pi=pi, pti=pti: e.activation(out=L.pt[pti][:, :], in_=ps[pi][:, :], func=AF.Exp), reads=[("ps", pi)], writes=[("pt", pti)])
    for ct in range(nct):
        pti = cpt[ct]
        pv(B_OC, ct == 0, pti, T.voc[:, ct, 0:65], ["voc"])
        for h in range(4):
            P.op("pe", lambda e, h=h, ct=ct, pti=pti: e.matmul(ps[B_IMP][:, h * 64:(h + 1) * 64], lhsT=L.pt[pti][:, h * 128:(h + 1) * 128],
                                                               rhs=T.voc[:, ct, 65:129], start=(ct == 0 and h == 0), stop=True, skip_group_check=True),
                 reads=[("pt", pti), "voc"], writes=[("ps", B_IMP)])
    oc = ps[B_OC][:, 0:260].rearrange("p (h c) -> p h c", c=65)
    P.op("dve", lambda e: e.tensor_scalar(out=sm[:, 0:4], in0=oc[:, :, 64], scalar1=1e-30, scalar2=None, op0=ALU.max),
         reads=[("ps", B_OC)], writes=["sm_c"])
    P.op("dve", lambda e: e.reciprocal(out=sm[:, 0:4], in_=sm[:, 0:4]), reads=["sm_c"], writes=["sm_c"])
    P.op("dve", lambda e: e.tensor_scalar(out=L.imp[:, :], in0=ps[B_IMP][:, 0:64], scalar1=sm[:, 0:1], scalar2=None, op0=ALU.mult),
         reads=[("ps", B_IMP), "sm_c"], writes=["imp"])
    for h in range(1, 4):
        P.op("dve", lambda e, h=h: e.scalar_tensor_tensor(out=L.imp[:, :], in0=ps[B_IMP][:, h * 64:(h + 1) * 64], scalar=sm[:, h:h + 1],
                                                          in1=L.imp[:, :], op0=ALU.mult, op1=ALU.add),
             reads=[("ps", B_IMP), "sm_c", "imp"], writes=["imp"])
    P.op("dve", lambda e: e.tensor_tensor(out=L.sc[:, :], in0=L.imp[:, :], in1=T.aadd[par][:, :], op=ALU.add), reads=["imp", ("aadd", par)], writes=["sc"])
    P.op("dve", lambda e: e.max(out=L.m8[:, :], in_=L.sc[:, :]), reads=["sc"], writes=["m8"])
    P.op("dve", lambda e: e.match_replace(out=L.sc2[:, :], in_to_replace=L.m8[:, :], in_values=L.sc[:, :], imm_value=-3e6),
         reads=["sc", "m8"], writes=["sc2"])
    P.op("dve", lambda e: e.max(out=L.m8[:, :], in_=L.sc2[:, :]), reads=["sc2"], writes=["m8"])
    P.op("dve", lambda e: e.match_replace(out=L.sc2[:, :], in_to_replace=L.m8[:, :], in_values=L.sc2[:, :], imm_value=-3e6),
         reads=["sc2", "m8"], writes=["sc2"])
    P.op("dve", lambda e: e.tensor_tensor(out=L.nsel[:, :], in0=L.sc2[:, :], in1=L.sc[:, :], op=ALU.is_equal), reads=["sc2", "sc"], writes=["nsel"])
    gv = gates[:, :].rearrange("p (h b) -> p h b", b=3)
    P.op("dve", lambda e: e.tensor_tensor(out=sm[:, 4:8], in0=sm[:, 0:4], in1=gv[:, :, 0], op=ALU.mult), reads=["sm_c", gkey], writes=["sm_cc"])
    for h in range(4):
        P.op("dve", lambda e, h=h: e.tensor_scalar(out=L.oacc[:, h, :], in0=oc[:, h, 0:64], scalar1=sm[:, 4 + h:5 + h], scalar2=None, op0=ALU.mult),
             reads=[("ps", B_OC), "sm_cc"], writes=["oacc"])
    wpairs = []
    wstate = {"first": True}
    for ki in range(max(0, qi - 4), qi + 1):
        dl = qi - ki
        st = {}

        def s_fn(ki=ki, dl=dl, st=st):
            pi = C.next_ps()
            P.op("pe", lambda e: e.matmul(ps[pi][:, :], lhsT=L.kwT[:, ki * 128:(ki + 1) * 128], rhs=q_rhs, start=True, stop=False),
                 reads=[("kwT", ki // 4), qkey], writes=[("ps", pi)])
            P.op("pe", lambda e: e.matmul(ps[pi][:, :], lhsT=T.ident_b[:, :], rhs=T.bwin[:, dl, :], start=False, stop=True),
                 reads=["ident_b", "bwin"], writes=[("ps", pi)])
            pti = next_pt()
            st["pti"] = pti
            P.op("act", lambda e: e.activation(out=L.pt[pti][:, :], in_=ps[pi][:, :], func=AF.Exp), reads=[("ps", pi)], writes=[("pt", pti)])

        def pv_fn(ki=ki, st=st):
            pv(B_OW, wstate["first"], st["pti"], L.vw[:, ki, :], [("vw", ki), "vw_one"])
            wstate["first"] = False
        wpairs.append((s_fn, pv_fn))
    run_pairs(wpairs)
    ow = ps[B_OW][:, 0:260].rearrange("p (h c) -> p h c", c=65)
    P.op("dve", lambda e: e.reciprocal(out=sm[:, 12:16], in_=ow[:, :, 64]), reads=[("ps", B_OW)], writes=["sm_w"])
    P.op("dve", lambda e: e.tensor_tensor(out=sm[:, 12:16], in0=sm[:, 12:16], in1=gv[:, :, 2], op=ALU.mult), reads=["sm_w", gkey], writes=["sm_w"])
    for h in range(4):
        P.op("dve", lambda e, h=h: e.scalar_tensor_tensor(out=L.oacc[:, h, :], in0=ow[:, h, 0:64], scalar=sm[:, 12 + h:13 + h], in1=L.oacc[:, h, :],
                                                          op0=ALU.mult, op1=ALU.add),
             reads=[("ps", B_OW), "sm_w", "oacc"], writes=["oacc"])
    pi = C.next_ps()
    P.op("pe", lambda e, pi=pi: e.transpose(ps[pi][0:64, 0:128], L.nsel[:, :], T.ident_f[:, :]), reads=["nsel", "ident_f"], writes=[("ps", pi)])
    ms = L.msel[par]
    mkey = ("msel", par)
    P.op("dve", lambda e, pi=pi: e.tensor_scalar(out=ms[:, 0, :], in0=ps[pi][0:64, 0:128], scalar1=NEG, scalar2=None, op0=ALU.mult),
         reads=[("ps", pi)], writes=[mkey])
    for h in range(1, 4):
        P.op("pool", lambda e, h=h: e.tensor_copy(out=ms[:, h, :], in_=ms[:, 0, :]), reads=[mkey], writes=[mkey])
    ms_rhs = ms[:, :, :].rearrange("p h t -> p (h t)")
    sstate = {"far": True, "near": True}
    spairs = []
    for ki in range(qi + 1):
        dl = qi - ki
        near = dl <= 7
        st = {}

        def s_fn(ki=ki, dl=dl, near=near, st=st):
            pi = C.next_ps()
            P.op("pe", lambda e: e.matmul(ps[pi][:, :], lhsT=L.ksT[:, ki * 128:(ki + 1) * 128], rhs=q_rhs, start=True, stop=False),
                 reads=[("ksT", ki // 4), qkey], writes=[("ps", pi)])
            P.op("pe", lambda e: e.matmul(ps[pi][:, :], lhsT=T.exall[:, ki * 128:(ki + 1) * 128], rhs=ms_rhs, start=False, stop=(not near)),
                 reads=["exall", mkey], writes=[("ps", pi)])
            if near:
                P.op("pe", lambda e: e.matmul(ps[pi][:, :], lhsT=T.ident_b[:, :], rhs=T.bsel[:, dl, :], start=False, stop=True),
                     reads=["ident_b", "bsel"], writes=[("ps", pi)])
            pti = next_pt()
            st["pti"] = pti
            P.op("act", lambda e: e.activation(out=L.pt[pti][:, :], in_=ps[pi][:, :], func=AF.Exp), reads=[("ps", pi)], writes=[("pt", pti)])

        def pv_fn(ki=ki, near=near, st=st):
            if near:
                pv(B_ON, sstate["near"], st["pti"], L.vs[:, ki, :], [("vs", ki), "vs_one"])
                sstate["near"] = False
            else:
                pv(B_OF, sstate["far"], st["pti"], L.vs[:, ki, :], [("vs", ki), "vs_one"])
                sstate["far"] = False
        spairs.append((s_fn, pv_fn))
    run_pairs(spairs)
    on = ps[B_ON][:, 0:260].rearrange("p (h c) -> p h c", c=65)
    of = ps[B_OF][:, 0:260].rearrange("p (h c) -> p h c", c=65)
    if not sstate["far"]:
        P.op("act", lambda e: e.activation(out=L.onsb[:, :, :], in_=on, func=AF.Copy), reads=[("ps", B_ON)], writes=["onsb"])
        for h in range(4):
            P.op("dve", lambda e, h=h: e.scalar_tensor_tensor(out=L.osel[:, h, :], in0=of[:, h, :], scalar=T.efar[:, h:h + 1], in1=L.onsb[:, h, :],
                                                              op0=ALU.mult, op1=ALU.add),
                 reads=[("ps", B_OF), "efar", "onsb"], writes=["osel"])
        osel, okeys = L.osel, ["osel"]
    else:
        osel, okeys = on, [("ps", B_ON)]
    P.op("dve", lambda e: e.reciprocal(out=sm[:, 8:12], in_=osel[:, :, 64]), reads=okeys, writes=["sm_s"])
    P.op("dve", lambda e: e.tensor_tensor(out=sm[:, 8:12], in0=sm[:, 8:12], in1=gv[:, :, 1], op=ALU.mult), reads=["sm_s", gkey], writes=["sm_s"])
    for h in range(4):
        P.op("dve", lambda e, h=h: e.scalar_tensor_tensor(out=L.oacc[:, h, :], in0=osel[:, h, 0:64], scalar=sm[:, 8 + h:9 + h], in1=L.oacc[:, h, :],
                                                          op0=ALU.mult, op1=ALU.add),
             reads=okeys + ["sm_s", "oacc"], writes=["oacc"])
    pi = C.next_ps()
    for c2 in range(2):
        P.op("pe", lambda e, pi=pi, c2=c2: e.transpose(ps[pi][:, c2 * 128:(c2 + 1) * 128],
                                                       L.oacc[:, 2 * c2:2 * c2 + 2, :].rearrange("p h d -> p (h d)"), T.ident_f[:, :]),
             reads=["oacc", "ident_f"], writes=[("ps", pi)])
    oaT = L.oaT[par]
    P.op("act", lambda e, pi=pi: e.activation(out=oaT[:, :, :], in_=ps[pi][:, 0:256].rearrange("p (c t) -> p c t", c=2), func=AF.Copy),
         reads=[("ps", pi)], writes=[("oaT", par)])
    ca = oT_d.get("oa_c0", 0)
    P.op("sp", lambda e: e.dma_start(out=oT_d["oT"][:, ca:ca + 2, t0:t0 + 128], in_=oaT[:, :, :]), reads=[("oaT", par)], writes=[("oTa", par)],
         dma_key=("oTa", par))


def emit_gla_tile(C, T, L, B, tq, oT_d):
    nc, P, ps = C.nc, C.P, C.ps
    qi = 4 * B + tq
    t0 = qi * 128
    par = qi % 2
    tc0 = tq * 128
    pz = C.next_ps()
    P.op("pe", lambda e: e.matmul(ps[pz][:, 0:256], lhsT=L.alrT[:, tc0:tc0 + 128], rhs=L.w2a[:, :], start=True, stop=False),
         reads=["alrT", "w2a"], writes=[("ps", pz)])
    P.op("pe", lambda e: e.matmul(ps[pz][:, 0:256], lhsT=T.ones_f[:, :], rhs=L.ba[:, :], start=False, stop=True),
         reads=["ones_f", "ba"], writes=[("ps", pz)])
    P.op("act", lambda e: e.activation(out=L.ez[:, :], in_=ps[pz][:, 0:256], func=AF.Exp, scale=-1.0), reads=[("ps", pz)], writes=["ez"])
    P.op("act", lambda e: e.activation(out=L.sp[:, :], in_=L.ez[:, :], func=AF.Ln, bias=T.one_col[:, 0:1]), reads=["ez", "one_col"], writes=["sp"])
    yield
    pr = C.next_ps()
    P.op("pe", lambda e: e.matmul(ps[pr][:, 0:256], lhsT=T.tri_u[:, :], rhs=L.sp[:, :], start=True, stop=True), reads=["tri_u", "sp"], writes=[("ps", pr)])
    P.op("act", lambda e: e.activation(out=L.erb[:, :], in_=ps[pr][:, 0:256], func=AF.Exp), reads=[("ps", pr)], writes=["erb"])
    P.op("dve", lambda e: e.tensor_tensor(out=L.kstate[:, :], in0=L.kbtok[par][:, :], in1=L.erb[:, :], op=ALU.mult),
         reads=[("kbtok", par), "erb"], writes=["kstate"])
    vb = L.vb[par]
    for h in range(2):
        eb, enb, qdec, kint, at = L.eb[h], L.enb[h], L.qdec[h], L.kint[h], L.at[h]
        pb = C.next_ps()
        P.op("pe", lambda e, h=h, pb=pb: e.matmul(ps[pb][:, 0:128], lhsT=L.sp[:, h * 128:(h + 1) * 128], rhs=T.tri_b[:, :], start=True, stop=True),
             reads=["sp", "tri_b"], writes=[("ps", pb)])
        P.op("act", lambda e, pb=pb, eb=eb: e.activation(out=eb[:, :], in_=ps[pb][:, 0:128], func=AF.Exp), reads=[("ps", pb)], writes=[("eb", h)])
        P.op("act", lambda e, pb=pb, enb=enb: e.activation(out=enb[:, :], in_=ps[pb][:, 0:128], func=AF.Exp, scale=-1.0), reads=[("ps", pb)],
             writes=[("enb", h)])
        P.op("dve", lambda e, h=h, eb=eb, qdec=qdec: e.scalar_tensor_tensor(out=qdec[:, :], in0=L.qbT[:, h, tc0:tc0 + 128], scalar=128.0 ** -0.5,
                                                                            in1=eb[:, :], op0=ALU.mult, op1=ALU.mult),
             reads=[("qbT", h), ("eb", h)], writes=[("qdec", h)])
        P.op("dve", lambda e, h=h, enb=enb, kint=kint: e.tensor_tensor(out=kint[:, :], in0=L.kbT[:, h, tc0:tc0 + 128], in1=enb[:, :], op=ALU.mult),
             reads=[("kbT", h), ("enb", h)], writes=[("kint", h)])
        yield
        pa = C.next_ps()
        P.op("pe", lambda e, pa=pa, kint=kint, qdec=qdec: e.matmul(ps[pa][:, 0:128], lhsT=kint[:, :], rhs=qdec[:, :], start=True, stop=True),
             reads=[("kint", h), ("qdec", h)], writes=[("ps", pa)])
        P.op("dve", lambda e, pa=pa, at=at: e.tensor_tensor(out=at[:, :], in0=ps[pa][:, 0:128], in1=T.amask[:, :], op=ALU.mult),
             reads=[("ps", pa), "amask"], writes=[("at", h)])
        yield
        po = C.next_ps()
        C.held = set([po])
        vh = vb[:, h * 256:(h + 1) * 256]
        P.op("pe", lambda e, po=po, at=at, vh=vh: e.matmul(ps[po][:, 0:256], lhsT=at[:, :], rhs=vh, start=True, stop=False),
             reads=[("at", h), ("vb", par)], writes=[("ps", po)])
        skey = "state%d" % h
        for ch in range(2):
            r0 = ch * 64
            yield
            si = L.cnt["sbf"][h] % 2
            sbf = L.sbf[h][si]
            P.op("pe", lambda e, po=po, r0=r0, qdec=qdec, sbf=sbf, ch=ch: e.matmul(ps[po][r0:r0 + 64, 0:256], lhsT=qdec[:, r0:r0 + 64], rhs=sbf[:, :],
                                                                                  start=False, stop=(ch == 1), skip_group_check=True),
                 reads=[("qdec", h), ("sbf", h, si)], writes=[("ps", po)])
            pu = C.next_ps()
            P.op("pe", lambda e, pu=pu, r0=r0, h=h, vh=vh: e.matmul(ps[pu][:, 0:256], lhsT=L.kstate[r0:r0 + 64, h * 128:(h + 1) * 128],
                                                                  rhs=vb[r0:r0 + 64, h * 256:(h + 1) * 256], start=True, stop=True),
                 reads=["kstate", ("vb", par)], writes=[("ps", pu)])
            dcol = eb[:, r0 + 63:r0 + 64]
            P.op("dve", lambda e, pu=pu, h=h, dcol=dcol: e.scalar_tensor_tensor(out=L.state[:, h, :], in0=L.state[:, h, :], scalar=dcol,
                                                                                in1=ps[pu][:, 0:256], op0=ALU.mult, op1=ALU.add),
                 reads=[skey, ("eb", h), ("ps", pu)], writes=[skey])
            L.cnt["sbf"][h] += 1
            sj = L.cnt["sbf"][h] % 2
            P.op("act", lambda e, h=h, sj=sj: e.activation(out=L.sbf[h][sj][:, :], in_=L.state[:, h, :], func=AF.Copy), reads=[skey],
                 writes=[("sbf", h, sj)])
        P.op("act", lambda e, po=po, h=h: e.activation(out=L.junk[:, :], in_=ps[po][:, 0:256], func=AF.Square, accum_out=L.ssq[:, h:h + 1]),
             reads=[("ps", po)], writes=["junk", ("ssq", h)])
        P.op("act", lambda e, h=h: e.activation(out=L.ssq[:, h:h + 1], in_=L.ssq[:, h:h + 1], func=AF.Sqrt, bias=T.eps_col[:, 0:1], scale=1.0 / 256.0),
             reads=[("ssq", h), "eps_col"], writes=[("ssq", h)])
        P.op("dve", lambda e, h=h: e.reciprocal(out=L.ssq[:, h:h + 1], in_=L.ssq[:, h:h + 1]), reads=[("ssq", h)], writes=[("ssq", h)])
        P.op("dve", lambda e, h=h, po=po: e.scalar_tensor_tensor(out=L.ob[:, h * 256:(h + 1) * 256], in0=ps[po][:, 0:256], scalar=L.ssq[:, h:h + 1],
                                                                 in1=L.gsr[par][:, h * 256:(h + 1) * 256], op0=ALU.mult, op1=ALU.mult),
             reads=[("ps", po), ("ssq", h), ("gsr", par)], writes=[("ob", h)])
        C.held = set()
    yield
    pt_ = C.next_ps()
    for c4 in range(4):
        P.op("pe", lambda e, c4=c4: e.transpose(ps[pt_][:, c4 * 128:(c4 + 1) * 128], L.ob[:, c4 * 128:(c4 + 1) * 128], T.ident_f[:, :]),
             reads=[("ob", c4 // 2), "ident_f"], writes=[("ps", pt_)])
    obT = L.obT[par]
    P.op("act", lambda e: e.activation(out=obT[:, :, :], in_=ps[pt_][:, :].rearrange("p (c t) -> p c t", c=4), func=AF.Copy),
         reads=[("ps", pt_)], writes=[("obT", par)])
    cb = oT_d.get("ob_c0", 2)
    P.op("sp", lambda e: e.dma_start(out=oT_d["oT"][:, cb:cb + 4, t0:t0 + 128], in_=obT[:, :, :]), reads=[("obT", par)], writes=[("oTb", par)],
         dma_key=("oTb", par))


def build_mixer_program(nlayers=1, debug=False):
    nc = bass.Bass("TRN2", target_bir_lowering=False)
    cin = {}
    for k, shp in MIX_CONST_SHAPES.items():
        cin[k] = dram_w(nc, "c_" + k, shp)
    din = {}
    for k, shp in MIX_W_SHAPES.items():
        din[k] = dram_w(nc, k, shp)
    cin["rel"] = din["rel"]
    hT_d = dram_w(nc, "hT", [128, KD, S], BF16)
    oT = nc.dram_tensor("oT", [128, 6, S], BF16, kind="ExternalOutput").ap()
    cin["fc"] = nc.dram_tensor("fc_scr", [4, 128, LC], BF16, kind="Internal").ap()
    cin["fw"] = nc.dram_tensor("fw_scr", [4, 128, LW], BF16, kind="Internal").ap()
    od = {"oT": oT, "fc": cin["fc"]}
    with ExitStack() as stack:
        C = Ctx(nc, stack)
        C.psum_banks()
        C.gen_banks = [0, 1, 2]
        T = mixer_setup(C, cin, stack)
        L = alloc_mixer_layer_tiles(C, stack)
        emit_mixer_layer(C, T, L, din, hT_d, od)
        fin = [("oTa", 0), ("oTa", 1), ("oTb", 0), ("oTb", 1)]
        if debug:
            dl = [("ksT", L.ksT, [("ksT", b) for b in range(NBLK)]), ("kwT", L.kwT, [("kwT", b) for b in range(NBLK)]),
                  ("kcrT", L.kcrT, [("kcrT", b) for b in range(NBLK)] + [("kcrTu", b) for b in range(NBLK)]),
                  ("vs", L.vs, [("vs", q) for q in range(NQT)]), ("kcT", T.kcT, ["kcT"]), ("voc", T.voc, ["voc"]),
                  ("qT", L.qT[1], [("qT", 1)]), ("gates", L.gates[1], [("gates", 1)]), ("bsel", T.bsel, ["bsel"]), ("bwin", T.bwin, ["bwin"]),
                  ("efar", T.efar, ["efar"]), ("state", L.state, ["state0", "state1"]), ("imp", L.imp, ["imp"]), ("nsel", L.nsel, ["nsel"]),
                  ("oacc", L.oacc, ["oacc"]), ("sp", L.sp, ["sp"]), ("ob", L.ob, [("ob", 0), ("ob", 1)]), ("bcmp", L.bcmp[1], [("bcmp", 1, 0), ("bcmp", 1, 1)]),
                  ("msel", L.msel[1], [("msel", 1)]), ("kbtok", L.kbtok[1], [("kbtok", 1)]), ("vb", L.vb[1], [("vb", 1)]), ("pbk", L.pbk, ["pbk"])]
            for nm, t, keys in dl:
                shp = list(t.shape)
                o = nc.dram_tensor("dbg_" + nm, shp, t.dtype, kind="ExternalOutput").ap()
                idx = tuple(slice(None) for _ in shp)
                C.P.op("sp", lambda e, o=o, t=t, idx=idx: e.dma_start(out=o[idx], in_=t[idx]), reads=keys, writes=[("dbg", nm)], dma_key=("dbg", nm))
                fin.append(("dbg", nm))
        C.P.wait_all("sp", fin)
        C.P.emit(stack)
    return nc


def alloc_merge_tiles(C, stack):
    sb = lambda n, s, d: C.sb(n, s, d, stack)
    M = MixerTiles()
    M.wgm = sb("wgm", [128, KD, 2048], BF16)
    M.wbn = sb("wbn", [128, 4, D], BF16)
    M.wbg = sb("wbg", [128, 8, D], BF16)
    M.wo = sb("wo", [128, KD, D], BF16)
    M.oT = [sb("oTin%d" % i, [128, 12, TT], BF16) for i in range(2)]
    M.hT = sb("mhT", [128, KD, TT], BF16)
    M.yT = sb("yT", [128, KD, TT], BF16)
    M.sg = [sb("msg%d" % i, [128, TT], F32) for i in range(2)]
    M.t1 = sb("mt1", [128, TT], F32)
    M.t2 = sb("mt2", [128, TT], F32)
    return M


def emit_merge(C, xT, M, gmix_col, d, oT_d, tiles, oT_loader=None):
    P, ps = C.P, C.ps
    P.op("pool", lambda e: e.dma_start(out=M.wgm[:, :, :], in_=d["wgm"].rearrange("(k p) c -> p k c", p=128)), writes=["wgm"], dma_key="wgm")
    P.op("pool", lambda e: e.dma_start(out=M.wbn[:, :, :], in_=d["wbn"].rearrange("(k p) c -> p k c", p=128)), writes=["wbn"], dma_key="wbn")
    P.op("pool", lambda e: e.dma_start(out=M.wbg[:, :, :], in_=d["wbg"].rearrange("(k p) c -> p k c", p=128)), writes=["wbg"], dma_key="wbg")
    P.op("pool", lambda e: e.dma_start(out=M.wo[:, :, :], in_=d["wo"].rearrange("(k p) c -> p k c", p=128)), writes=["wo"], dma_key="wo")
    for it in range(NTOK // TT):
        t0 = it * TT
        ts = t0 // TS
        oT = M.oT[it % 2]
        okey = ("oTin", it % 2)
        if oT_loader is None:
            P.op("sp", lambda e, oT=oT, t0=t0: e.dma_start(out=oT[:, :, :], in_=oT_d[:, :, t0:t0 + TT]), writes=[okey], dma_key=okey)
        else:
            oT_loader(it, oT, okey)
        emit_rmsnorm_tile(C, xT, t0, gmix_col, M.hT, "mhT", tiles["ones"], tiles, [("xT", ts)], gkey="gmixp")
        for dc in range(KD):
            pg0, pg1, pa, pb = C.next_ps(), C.next_ps(), C.next_ps(), C.next_ps()
            for (pi, c0) in ((pg0, dc * 128), (pg1, 1024 + dc * 128)):
                for k in range(KD):
                    P.op("pe", lambda e, pi=pi, c0=c0, k=k: e.matmul(ps[pi][:, :], lhsT=M.wgm[:, k, c0:c0 + 128], rhs=M.hT[:, k, :],
                                                                    start=(k == 0), stop=(k == KD - 1)),
                         reads=["wgm", "mhT"], writes=[("ps", pi)])
            for k in range(4):
                P.op("pe", lambda e, k=k, pa=pa, dc=dc, oT=oT: e.matmul(ps[pa][:, :], lhsT=M.wbn[:, k, dc * 128:(dc + 1) * 128], rhs=oT[:, k, :],
                                                                       start=(k == 0), stop=(k == 3)),
                     reads=["wbn", okey], writes=[("ps", pa)])
            for k in range(8):
                P.op("pe", lambda e, k=k, pb=pb, dc=dc, oT=oT: e.matmul(ps[pb][:, :], lhsT=M.wbg[:, k, dc * 128:(dc + 1) * 128], rhs=oT[:, 4 + k, :],
                                                                       start=(k == 0), stop=(k == 7)),
                     reads=["wbg", okey], writes=[("ps", pb)])
            P.op("act", lambda e, pg0=pg0: e.activation(out=M.sg[0][:, :], in_=ps[pg0][:, :], func=AF.Sigmoid), reads=[("ps", pg0)], writes=[("msg", 0)])
            P.op("act", lambda e, pg1=pg1: e.activation(out=M.sg[1][:, :], in_=ps[pg1][:, :], func=AF.Sigmoid), reads=[("ps", pg1)], writes=[("msg", 1)])
            P.op("dve", lambda e, pa=pa: e.tensor_tensor(out=M.t1[:, :], in0=ps[pa][:, :], in1=M.sg[0][:, :], op=ALU.mult),
                 reads=[("ps", pa), ("msg", 0)], writes=["mt1"])
            P.op("dve", lambda e, pb=pb: e.tensor_tensor(out=M.t2[:, :], in0=ps[pb][:, :], in1=M.sg[1][:, :], op=ALU.mult),
                 reads=[("ps", pb), ("msg", 1)], writes=["mt2"])
            P.op("pool", lambda e, dc=dc: e.tensor_tensor(out=M.yT[:, dc, :], in0=M.t1[:, :], in1=M.t2[:, :], op=ALU.add),
                 reads=["mt1", "mt2"], writes=[("yT", dc)])
        for dc in range(KD):
            po = C.next_ps()
            for k in range(KD):
                P.op("pe", lambda e, k=k, po=po, dc=dc: e.matmul(ps[po][:, :], lhsT=M.wo[:, k, dc * 128:(dc + 1) * 128], rhs=M.yT[:, k, :],
                                                                start=(k == 0), stop=(k == KD - 1)),
                     reads=["wo", ("yT", k)], writes=[("ps", po)])
            P.op("dve", lambda e, po=po, dc=dc, t0=t0: e.tensor_tensor(out=xT[:, dc, t0:t0 + TT], in0=ps[po][:, :], in1=xT[:, dc, t0:t0 + TT], op=ALU.add),
                 reads=[("ps", po), ("xT", ts)], writes=[("xT", ts)])


def emit_h_out(C, xT, g_col, gkey, tiles, hT_out, final=False):
    P = C.P
    for it in range(NTOK // TT):
        t0 = it * TT
        ts = t0 // TS
        if final:
            ht = tiles["hfin"][it % 2]
            key = ("hfin", it % 2)
        else:
            ht = tiles["hout"][it % 2]
            key = ("hout", it % 2)
        emit_rmsnorm_tile(C, xT, t0, g_col, ht, key, tiles["ones"], tiles, [("xT", ts)], gkey=gkey)
        P.op("sp", lambda e, ht=ht, t0=t0: e.dma_start(out=hT_out[:, :, t0:t0 + TT], in_=ht[:, :, :]), reads=[key], writes=[("hst", it % 2)],
             dma_key=("hst", it % 2))


def build_tok_program(first, last):
    nc = bass.Bass("TRN2", target_bir_lowering=False)
    x_in = dram_w(nc, "xT_in", [128, KD, NTOK])
    gains = dram_w(nc, "gains", [128, 5, KD])
    dr = {}
    if not first:
        oT_in = dram_w(nc, "oT_in", [128, 12, NTOK], BF16)
        for k, shp in (("wgm", [D, 2048]), ("wbn", [512, D]), ("wbg", [1024, D]), ("wo", [D, D]), ("wg2", [D, DFF]), ("wu2", [D, DFF]), ("wd2", [DFF, D])):
            dr[k] = dram_w(nc, k, shp)
    if not last:
        for k, shp in (("wg1", [D, DFF]), ("wu1", [D, DFF]), ("wd1", [DFF, D])):
            dr[k] = dram_w(nc, k, shp)
        x_out = nc.dram_tensor("xT_out", [128, KD, NTOK], F32, kind="ExternalOutput").ap()
        h_out = nc.dram_tensor("hT_out", [128, KD, NTOK], BF16, kind="ExternalOutput").ap()
    else:
        y_out = nc.dram_tensor("y_out", [128, KD, NTOK], F32, kind="ExternalOutput").ap()
    with ExitStack() as stack:
        C = Ctx(nc, stack)
        C.psum_banks()
        P = C.P
        xT = C.sb("xT", [128, KD, NTOK], F32)
        gt = C.sb("gains", [128, 5, KD], F32)
        ones = C.sb("ones", [128, 128], BF16)
        epsc = C.sb("epsc", [128, 1], F32)
        P.op("pool", lambda e: e.memset(ones[:, :], 1.0), writes=["ones"])
        P.op("pool", lambda e: e.memset(epsc[:, :], EPS), writes=["epsc"])
        for ts in range(NTOK // TS):
            P.op("sp", lambda e, ts=ts: e.dma_start(out=xT[:, :, ts * TS:(ts + 1) * TS], in_=x_in[:, :, ts * TS:(ts + 1) * TS]),
                 writes=[("xT", ts)], dma_key=("xT", ts))
        P.op("sp", lambda e: e.dma_start(out=gt[:, :, :], in_=gains[:, :, :]), writes=["gains"], dma_key="gains")
        fin = []
        base = {"ones": ones, "epsc": epsc, "sq": C.sb("sq", [128, KD, TT], BF16), "rstd": C.sb("rstd", [128, TT], F32)}
        if not first:
            with ExitStack() as s3:
                M = alloc_merge_tiles(C, s3)
                emit_merge(C, xT, M, gt[:, 0, :], dr, oT_in, base)
                P.barrier()
        with ExitStack() as s2:
            tiles = alloc_ffn_tiles(C, s2)
            tiles.update(base)
            if not first:
                emit_ffn(C, xT, gt[:, 1, :], dr["wg2"], dr["wu2"], dr["wd2"], tiles, "ffn2")
            if not last:
                emit_ffn(C, xT, gt[:, 2, :], dr["wg1"], dr["wu1"], dr["wd1"], tiles, "ffn1")
            P.barrier()
        with ExitStack() as s4:
            tiles = dict(base)
            if not last:
                tiles["hout"] = [C.sb("hout%d" % i, [128, KD, TT], BF16, s4) for i in range(2)]
                emit_h_out(C, xT, gt[:, 3, :], "gains", tiles, h_out)
                for ts in range(NTOK // TS):
                    P.op("sp", lambda e, ts=ts: e.dma_start(out=x_out[:, :, ts * TS:(ts + 1) * TS], in_=xT[:, :, ts * TS:(ts + 1) * TS]),
                         reads=[("xT", ts)], writes=[("xst", ts)], dma_key=("xst", ts))
                    fin.append(("xst", ts))
                fin += [("hst", 0), ("hst", 1)]
            else:
                tiles["hfin"] = [C.sb("hfin%d" % i, [128, KD, TT], F32, s4) for i in range(2)]
                emit_h_out(C, xT, gt[:, 4, :], "gains", tiles, y_out, final=True)
                fin += [("hst", 0), ("hst", 1)]
            P.wait_all("sp", fin)
            P.emit(stack)
    return nc


_PROGS = {}


def _prog(name, fn):
    if name not in _PROGS:
        _PROGS[name] = fn()
    return _PROGS[name]


def _gains(inp, l, first, last):
    g = np.zeros((128, 5, KD), np.float32)
    if not first:
        g[:, 0] = gain_cols(inp["mix_norm"][l])
        g[:, 1] = gain_cols(inp["ffn2_norm"][l])
    if not last:
        ln = 0 if first else l + 1
        g[:, 2] = gain_cols(inp["ffn1_norm"][ln])
        g[:, 3] = gain_cols(inp["mix_norm"][ln])
    g[:, 4] = gain_cols(inp["final_norm"])
    return g


def kernel_unfused(**inp):
    inp = {k: np.asarray(v) for k, v in inp.items()}
    x = inp["x"]
    cores = list(range(8))
    nc = _prog("tok_first", lambda: build_tok_program(True, False))
    maps = []
    for c in cores:
        b, h = c // 2, c % 2
        maps.append({"xT_in": to_feature_major(x[b, h * NTOK:(h + 1) * NTOK]), "gains": _gains(inp, 0, True, False),
                     "wg1": inp["ffn1_w_gate"][0], "wu1": inp["ffn1_w_up"][0], "wd1": inp["ffn1_w_down"][0]})
    res = run_bass_kernel_spmd(nc, maps, core_ids=cores).results
    xT = [r["xT_out"] for r in res]
    hT = [r["hT_out"] for r in res]
    consts = {"c_" + k: v for k, v in mixer_consts().items()}
    for l in range(DEPTH):
        ncm = _prog("mixer", lambda: build_mixer_program())
        maps = []
        for c in cores:
            b, g = c // 2, c % 2
            m = dict(consts)
            m.update(mixer_weight_inputs(inp, l, g))
            m["hT"] = np.ascontiguousarray(np.concatenate([hT[2 * b], hT[2 * b + 1]], axis=2))
            maps.append(m)
        res = run_bass_kernel_spmd(ncm, maps, core_ids=cores).results
        oT = [r["oT"] for r in res]
        last = (l == DEPTH - 1)
        nct = _prog("tok_last" if last else "tok_mid", lambda: build_tok_program(False, last))
        maps = []
        for c in cores:
            b, h = c // 2, c % 2
            sl = slice(h * NTOK, (h + 1) * NTOK)
            o0, o1 = oT[2 * b], oT[2 * b + 1]
            m = {"xT_in": xT[c], "gains": _gains(inp, l, False, last),
                 "oT_in": np.ascontiguousarray(np.concatenate([o0[:, 0:2, sl], o1[:, 0:2, sl], o0[:, 2:6, sl], o1[:, 2:6, sl]], axis=1)),
                 "wgm": np.ascontiguousarray(inp["w_in"][l][:, OFF_GM:OFF_GM + 2048]),
                 "wbn": inp["w_branch_nsa"][l], "wbg": inp["w_branch_gla"][l], "wo": inp["w_out"][l],
                 "wg2": inp["ffn2_w_gate"][l], "wu2": inp["ffn2_w_up"][l], "wd2": inp["ffn2_w_down"][l]}
            if not last:
                m.update({"wg1": inp["ffn1_w_gate"][l + 1], "wu1": inp["ffn1_w_up"][l + 1], "wd1": inp["ffn1_w_down"][l + 1]})
            maps.append(m)
        res = run_bass_kernel_spmd(nct, maps, core_ids=cores).results
        if not last:
            xT = [r["xT_out"] for r in res]
            hT = [r["hT_out"] for r in res]
    out = np.zeros((NB, S, D), np.float32)
    for c in cores:
        b, h = c // 2, c % 2
        out[b, h * NTOK:(h + 1) * NTOK] = from_feature_major(res[c]["y_out"])
    return out


def kernel(**inputs):
    return kernel_fused8(**inputs)


def mixer_build_tables(C, cin, rel_d, fc, fw, efar_d, g, stack):
    P = C.P
    sb = lambda n, s, d: C.sb(n + "_g%d" % g, s, d, stack)
    relx = sb("relx", [33, 4], F32)
    ones33 = sb("ones33", [33, 128], F32)
    relb = sb("relb", [33, 4, 128], F32)
    ohb = sb("ohb", [33, 512], F32)
    oh31 = sb("oh31", [33, 128], F32)
    efar = sb("efar_t", [128, 4], F32)
    fsb = [sb("fsb%d" % i, [128, 512], BF16) for i in range(2)]
    k = lambda name: (name, g)
    P.op("sp", lambda e: e.dma_start(out=relx[0:32, :], in_=rel_d[:, :]), writes=[k("relx")], dma_key=k("relx"))
    P.op("pool", lambda e: e.memset(relx[32:33, :], NEG), writes=[k("relx32")])
    P.op("pool", lambda e: e.memset(ones33[:, :], 1.0), writes=[k("ones33")])
    P.op("sp", lambda e: e.dma_start(out=oh31[:, :], in_=cin["oh31"][:, :]), writes=[k("oh31")], dma_key=k("oh31"))
    pi = C.next_ps()
    P.op("pe", lambda e: e.matmul(C.ps[pi][:, 0:4], lhsT=oh31[:, :], rhs=relx[:, :], start=True, stop=True),
         reads=[k("oh31"), k("relx"), k("relx32")], writes=[("ps", pi)])
    P.op("act", lambda e: e.activation(out=efar[:, :], in_=C.ps[pi][:, 0:4], func=AF.Exp), reads=[("ps", pi)], writes=[k("efar_t")])
    P.op("sp", lambda e: e.dma_start(out=efar_d[:, :], in_=efar[:, :]), reads=[k("efar_t")], writes=[k("efar_d")], dma_key=k("efar_d"))
    for h in range(4):
        P.op("dve", lambda e, h=h: e.tensor_scalar(out=relb[:, h, :], in0=ones33[:, :], scalar1=relx[:, h:h + 1], scalar2=None, op0=ALU.mult),
             reads=[k("ones33"), k("relx"), k("relx32")], writes=[k("relb")])
    it = 0
    for tab, src_oh, L_, scr in (("c", cin["oh_c"], LC, fc), ("w", cin["oh_w"], LW, fw)):
        for blk in range(L_ // 512):
            P.op("sp", lambda e, blk=blk, src_oh=src_oh: e.dma_start(out=ohb[:, :], in_=src_oh[:, blk * 512:(blk + 1) * 512]),
                 writes=[k("ohb")], dma_key=k("ohb"))
            for h in range(4):
                pi = C.next_ps()
                par = it % 2
                fb = fsb[par]
                fkey = ("fsb", g, par)
                it += 1
                P.op("pe", lambda e, pi=pi, h=h: e.matmul(C.ps[pi][:, :], lhsT=relb[:, h, :], rhs=ohb[:, :], start=True, stop=True),
                     reads=[k("relb"), k("ohb")], writes=[("ps", pi)])
                P.op("act", lambda e, pi=pi, fb=fb: e.activation(out=fb[:, :], in_=C.ps[pi][:, :], func=AF.Copy),
                     reads=[("ps", pi)], writes=[fkey])
                P.op("sp", lambda e, h=h, scr=scr, fb=fb, blk=blk: e.dma_start(out=scr[h, :, blk * 512:(blk + 1) * 512], in_=fb[:, :]),
                     reads=[fkey], writes=[("F", g, tab, par)], dma_key=("Fst", g, par))


def mixer_alloc_consts(C, cin, fc, fw, efar_d, g, stack):
    P = C.P
    T = MixerTiles()
    sb = lambda n, s, d: C.sb(n, s, d, stack)
    T.ident_f = sb("ident_f", [128, 128], F32)
    T.ident_b = sb("ident_b", [128, 128], BF16)
    T.exall = sb("exall", [64, S], BF16)
    T.tri_b = sb("tri_b", [128, 128], F32)
    T.tri_u = sb("tri_u", [128, 128], F32)
    T.amask = sb("amask", [128, 128], F32)
    T.aadd = [sb("aadd%d" % i, [128, 64], F32) for i in range(2)]
    T.aadd_d = cin["aadd"]
    T.one_col = sb("one_col", [128, 1], F32)
    T.eps_col = sb("eps_col", [128, 1], F32)
    T.tiny_col = sb("tiny_col", [128, 1], F32)
    T.ones_f = sb("ones_f", [1, 128], F32)
    T.efar = sb("efar", [128, 4], F32)
    T.bsel = sb("bsel", [128, 8, 512], BF16)
    T.bwin = sb("bwin", [128, 5, 512], BF16)
    T.voc = sb("voc", [128, 2, 129], BF16)
    T.kcT = sb("kcT", [64, 256], BF16)
    P.op("pool", lambda e: e.memset(T.ident_f[:, :], 0.0), writes=["ident_f"])
    P.op("pool", lambda e: e.affine_select(out=T.ident_f[:, :], in_=T.ident_f[:, :], pattern=[[-1, 128]], compare_op=ALU.not_equal,
                                           fill=1.0, base=0, channel_multiplier=1), reads=["ident_f"], writes=["ident_f"])
    P.op("pool", lambda e: e.tensor_copy(out=T.ident_b[:, :], in_=T.ident_f[:, :]), reads=["ident_f"], writes=["ident_b"])
    P.op("pool", lambda e: e.memset(T.one_col[:, :], 1.0), writes=["one_col"])
    P.op("pool", lambda e: e.memset(T.eps_col[:, :], EPS), writes=["eps_col"])
    P.op("pool", lambda e: e.memset(T.tiny_col[:, :], 1e-30), writes=["tiny_col"])
    P.op("pool", lambda e: e.memset(T.ones_f[:, :], 1.0), writes=["ones_f"])
    P.op("pool", lambda e: e.memset(T.voc[:, :, :], 0.0), writes=["voc"])
    P.op("pool", lambda e: e.memset(T.kcT[:, :], 0.0), writes=["kcT"])
    P.op("pool", lambda e: e.memset(T.voc[:, :, 64:65], 1.0), reads=["voc"], writes=["voc"])
    P.op("pool", lambda e: e.memset(T.voc[0:1, 0, 64:65], 0.0), reads=["voc"], writes=["voc"])
    P.op("pool", lambda e: e.dma_start(out=T.voc[:, :, 65:129], in_=cin["ov"][:, :, :]), reads=["voc"], writes=["voc"], dma_key="voc_ov")
    P.op("pool", lambda e: e.dma_start(out=T.exall[:, :], in_=cin["exall"][:, :]), writes=["exall"], dma_key="exall")
    for nm in ("tri_b", "tri_u", "amask"):
        t = getattr(T, nm)
        P.op("sp", lambda e, t=t, nm=nm: e.dma_start(out=t[:, :], in_=cin[nm][:, :]), writes=[nm], dma_key=nm)
    P.op("sp", lambda e: e.dma_start(out=T.efar[:, :], in_=efar_d[:, :]), reads=[("efar_d", g)], writes=["efar"], dma_key="efar")
    fk = lambda tab: [("F", g, tab, 0), ("F", g, tab, 1)]
    for dl in range(8):
        src = bass.AP(fc.tensor, fc.offset + OFFD + 128 * dl, [[LC - 1, 128], [128 * LC, 4], [1, 128]])
        P.op("sp", lambda e, dl=dl, src=src: e.dma_start(out=T.bsel[:, dl, :].rearrange("p (h t) -> p h t", h=4), in_=src),
             reads=fk("c"), writes=["bsel"], dma_key="bsel")
    for dl in range(5):
        src = bass.AP(fw.tensor, fw.offset + OFFW + 128 * dl, [[LW - 1, 128], [128 * LW, 4], [1, 128]])
        P.op("sp", lambda e, dl=dl, src=src: e.dma_start(out=T.bwin[:, dl, :].rearrange("p (h t) -> p h t", h=4), in_=src),
             reads=fk("w"), writes=["bwin"], dma_key="bwin")
    return T


def emit_tok_phase(C, first, last, base, gt, gi, dr, x_src, x_dst, oT_src, h_dst, y_dst, oT_loader=None, merge_extra=None):
    P = C.P
    with ExitStack() as sx:
        xT = C.sb("xT", [128, KD, NTOK], F32, sx)
        for ts in range(NTOK // TS):
            P.op("sp", lambda e, ts=ts: e.dma_start(out=xT[:, :, ts * TS:(ts + 1) * TS], in_=x_src[:, :, ts * TS:(ts + 1) * TS]),
                 writes=[("xT", ts)], dma_key=("xT", ts))
        if not first:
            with ExitStack() as s3:
                M = alloc_merge_tiles(C, s3)
                ldr = oT_loader(C, s3) if oT_loader is not None else None
                emit_merge(C, xT, M, gt[:, gi["mixp"], :], dr, oT_src, base, oT_loader=ldr)
                P.barrier()
        with ExitStack() as s2:
            tiles = alloc_ffn_tiles(C, s2)
            tiles.update(base)
            if not first:
                emit_ffn(C, xT, gt[:, gi["ffn2"], :], dr["wg2"], dr["wu2"], dr["wd2"], tiles, "ffn2")
            if not last:
                emit_ffn(C, xT, gt[:, gi["ffn1"], :], dr["wg1"], dr["wu1"], dr["wd1"], tiles, "ffn1")
            P.barrier()
        with ExitStack() as s4:
            tiles = dict(base)
            if not last:
                tiles["hout"] = [C.sb("hout%d" % i, [128, KD, TT], BF16, s4) for i in range(2)]
                emit_h_out(C, xT, gt[:, gi["mixn"], :], "gains", tiles, h_dst)
                for ts in range(NTOK // TS):
                    P.op("sp", lambda e, ts=ts: e.dma_start(out=x_dst[:, :, ts * TS:(ts + 1) * TS], in_=xT[:, :, ts * TS:(ts + 1) * TS]),
                         reads=[("xT", ts)], writes=[("xst", ts)], dma_key=("xst", ts))
            else:
                tiles["hfin"] = [C.sb("hfin%d" % i, [128, KD, TT], F32, s4) for i in range(2)]
                emit_h_out(C, xT, gt[:, gi["final"], :], "gains", tiles, y_dst, final=True)
            P.barrier()


FUSED_NCORES = 4


def build_fused_program(depth=DEPTH):
    nc = bass.Bass("TRN2", target_bir_lowering=False)
    x_in = dram_w(nc, "xT_in", [128, KD, S])
    gains = dram_w(nc, "gains", [128, 3 * depth + 1, KD])
    cin = {k: dram_w(nc, "c_" + k, shp) for k, shp in MIX_CONST_SHAPES.items()}
    rel_d = [dram_w(nc, "rel_g%d" % g, [32, 4]) for g in range(2)]
    LW_ = []
    for l in range(depth):
        d = {}
        for k, shp in (("wg1", [D, DFF]), ("wu1", [D, DFF]), ("wd1", [DFF, D]), ("wgm", [D, 2048]), ("wbn", [512, D]), ("wbg", [1024, D]),
                       ("wo", [D, D]), ("wg2", [D, DFF]), ("wu2", [D, DFF]), ("wd2", [DFF, D])):
            d[k] = dram_w(nc, "%s_l%d" % (k, l), shp)
        d["mix"] = []
        for g in range(2):
            dm = {}
            for k, shp in MIX_W_SHAPES.items():
                if k == "rel":
                    continue
                dm[k] = dram_w(nc, "%s_l%d_g%d" % (k, l, g), shp)
            d["mix"].append(dm)
        LW_.append(d)
    y_out = nc.dram_tensor("y_out", [128, KD, S], F32, kind="ExternalOutput").ap()
    xs = nc.dram_tensor("xs_scr", [128, KD, S], F32, kind="Internal").ap()
    hTs = nc.dram_tensor("hT_scr", [128, KD, S], BF16, kind="Internal").ap()
    oTs = nc.dram_tensor("oT_scr", [128, 12, S], BF16, kind="Internal").ap()
    fc = [nc.dram_tensor("fc_scr%d" % g, [4, 128, LC], BF16, kind="Internal").ap() for g in range(2)]
    fw = [nc.dram_tensor("fw_scr%d" % g, [4, 128, LW], BF16, kind="Internal").ap() for g in range(2)]
    efd = [nc.dram_tensor("efar_scr%d" % g, [128, 4], F32, kind="Internal").ap() for g in range(2)]
    with ExitStack() as stack:
        C = Ctx(nc, stack)
        C.psum_banks()
        P = C.P
        gt = C.sb("gains", [128, 3 * depth + 1, KD], F32)
        ones = C.sb("ones", [128, 128], BF16)
        epsc = C.sb("epsc", [128, 1], F32)
        base = {"ones": ones, "epsc": epsc, "sq": C.sb("sq", [128, KD, TT], BF16), "rstd": C.sb("rstd", [128, TT], F32)}
        P.op("pool", lambda e: e.memset(ones[:, :], 1.0), writes=["ones"])
        P.op("pool", lambda e: e.memset(epsc[:, :], EPS), writes=["epsc"])
        P.op("sp", lambda e: e.dma_start(out=gt[:, :, :], in_=gains[:, :, :]), writes=["gains"], dma_key="gains")
        C.gen_banks = [0, 1, 2]
        with ExitStack() as st:
            for g in range(2):
                mixer_build_tables(C, cin, rel_d[g], fc[g], fw[g], efd[g], g, st)
            P.barrier()
        C.gen_banks = list(range(8))
        for h in range(2):
            sl = slice(h * NTOK, (h + 1) * NTOK)
            gi = {"ffn1": 0, "mixn": 1}
            emit_tok_phase(C, True, False, base, gt, gi, {"wg1": LW_[0]["wg1"], "wu1": LW_[0]["wu1"], "wd1": LW_[0]["wd1"]},
                           x_in[:, :, sl], xs[:, :, sl], None, hTs[:, :, sl], None)
        for l in range(depth):
            C.gen_banks = [0, 1, 2]
            for g in range(2):
                with ExitStack() as sm:
                    T = mixer_alloc_consts(C, cin, fc[g], fw[g], efd[g], g, sm)
                    L = alloc_mixer_layer_tiles(C, sm)
                    od = {"oT": oTs, "fc": fc[g], "fkeys": [("F", g, "c", 0), ("F", g, "c", 1)], "oa_c0": 2 * g, "ob_c0": 4 + 4 * g}
                    emit_mixer_layer(C, T, L, LW_[l]["mix"][g], hTs, od)
                    P.barrier()
            C.gen_banks = list(range(8))
            last = (l == depth - 1)
            for h in range(2):
                sl = slice(h * NTOK, (h + 1) * NTOK)
                gi = {"mixp": 3 * l + 1, "ffn2": 3 * l + 2, "ffn1": 3 * (l + 1), "mixn": 3 * (l + 1) + 1, "final": 3 * depth}
                dr = {k: LW_[l][k] for k in ("wgm", "wbn", "wbg", "wo", "wg2", "wu2", "wd2")}
                if not last:
                    dr.update({k: LW_[l + 1][k] for k in ("wg1", "wu1", "wd1")})
                emit_tok_phase(C, False, last, base, gt, gi, dr, xs[:, :, sl], xs[:, :, sl], oTs[:, :, sl], hTs[:, :, sl], y_out[:, :, sl])
        P.wait_all("sp", [("hst", 0), ("hst", 1)])
        global _last_prog
        _last_prog = P
        P.emit(stack)
    return nc


def fused_inputs(inp, b, depth=DEPTH):
    m = {"xT_in": to_feature_major(inp["x"][b])}
    g = np.zeros((128, 3 * depth + 1, KD), np.float32)
    for l in range(depth):
        g[:, 3 * l] = gain_cols(inp["ffn1_norm"][l])
        g[:, 3 * l + 1] = gain_cols(inp["mix_norm"][l])
        g[:, 3 * l + 2] = gain_cols(inp["ffn2_norm"][l])
    g[:, 3 * depth] = gain_cols(inp["final_norm"])
    m["gains"] = g
    for k, v in mixer_consts().items():
        m["c_" + k] = v
    for gg in range(2):
        m["rel_g%d" % gg] = np.ascontiguousarray(inp["rel_table"][:, gg * 4:(gg + 1) * 4])
    for l in range(depth):
        m["wg1_l%d" % l] = inp["ffn1_w_gate"][l]
        m["wu1_l%d" % l] = inp["ffn1_w_up"][l]
        m["wd1_l%d" % l] = inp["ffn1_w_down"][l]
        m["wg2_l%d" % l] = inp["ffn2_w_gate"][l]
        m["wu2_l%d" % l] = inp["ffn2_w_up"][l]
        m["wd2_l%d" % l] = inp["ffn2_w_down"][l]
        m["wgm_l%d" % l] = np.ascontiguousarray(inp["w_in"][l][:, OFF_GM:OFF_GM + 2048])
        m["wbn_l%d" % l] = inp["w_branch_nsa"][l]
        m["wbg_l%d" % l] = inp["w_branch_gla"][l]
        m["wo_l%d" % l] = inp["w_out"][l]
        for gg in range(2):
            for k, v in mixer_weight_inputs(inp, l, gg).items():
                if k == "rel":
                    continue
                m["%s_l%d_g%d" % (k, l, gg)] = v
    return m


def kernel_fused(**inp):
    inp = {k: np.asarray(v) for k, v in inp.items()}
    nc = _prog("fused", lambda: build_fused_program())
    maps = [fused_inputs(inp, b) for b in range(NB)]
    res = run_bass_kernel_spmd(nc, maps, core_ids=list(range(NB))).results
    out = np.zeros((NB, S, D), np.float32)
    for b in range(NB):
        out[b] = from_feature_major(res[b]["y_out"])
    return out


PAIRS = [[0, 1], [2, 3], [4, 5], [6, 7]]


def build_fused8_program(depth=DEPTH):
    nc = bass.Bass("TRN2", target_bir_lowering=False)
    x_in = dram_w(nc, "xT_in", [128, KD, NTOK])
    gains = dram_w(nc, "gains", [128, 3 * depth + 1, KD])
    selc = dram_w(nc, "selc", [128, 2])
    cin = {k: dram_w(nc, "c_" + k, shp) for k, shp in MIX_CONST_SHAPES.items()}
    rel_d = dram_w(nc, "rel", [32, 4])
    LW_ = []
    for l in range(depth):
        d = {}
        for k, shp in (("wg1", [D, DFF]), ("wu1", [D, DFF]), ("wd1", [DFF, D]), ("wgm", [D, 2048]), ("wbn", [512, D]), ("wbg", [1024, D]),
                       ("wo", [D, D]), ("wg2", [D, DFF]), ("wu2", [D, DFF]), ("wd2", [DFF, D])):
            d[k] = dram_w(nc, "%s_l%d" % (k, l), shp)
        dm = {}
        for k, shp in MIX_W_SHAPES.items():
            if k == "rel":
                continue
            dm[k] = dram_w(nc, "%s_l%d" % (k, l), shp)
        d["mix"] = dm
        LW_.append(d)
    y_out = nc.dram_tensor("y_out", [128, KD, NTOK], F32, kind="ExternalOutput").ap()
    xs = nc.dram_tensor("xs_scr", [128, KD, NTOK], F32).ap()
    HC, OC_ = 2, 4
    h_src_t = [nc.dram_tensor("h_src%d" % c, [128 * KD, 1024], BF16) for c in range(HC)]
    h_all_t = [[nc.dram_tensor("h_all%d_%d" % (i, c), [2 * 128 * KD, 1024], BF16) for c in range(HC)] for i in range(2)]
    o_src_t = [nc.dram_tensor("o_src%d" % c, [128 * 6, 1024], BF16) for c in range(OC_)]
    o_all_t = [[nc.dram_tensor("o_all%d_%d" % (i, c), [2 * 128 * 6, 1024], BF16) for c in range(OC_)] for i in range(2)]
    h_src = [t.ap().rearrange("(p k) t -> p k t", k=KD) for t in h_src_t]
    h_all = [[t.ap().rearrange("(r p k) t -> r p k t", r=2, k=KD) for t in row] for row in h_all_t]
    o_src = [t.ap().rearrange("(p c) t -> p c t", c=6) for t in o_src_t]
    o_all = [[t.ap().rearrange("(r p c) t -> r p c t", r=2, c=6) for t in row] for row in o_all_t]

    class ChunkedDst:
        def __init__(self, aps):
            self.aps = aps

        def __getitem__(self, idx):
            p, c, t = idx
            ch = t.start // 1024
            assert (t.stop - 1) // 1024 == ch
            return self.aps[ch][p, c, t.start - ch * 1024:t.stop - ch * 1024]
    h_dst = ChunkedDst(h_src)
    o_dst = ChunkedDst(o_src)
    fc = nc.dram_tensor("fc_scr", [4, 128, LC], BF16).ap()
    fw = nc.dram_tensor("fw_scr", [4, 128, LW], BF16).ap()
    efd = nc.dram_tensor("efar_scr", [128, 4], F32).ap()
    with ExitStack() as stack:
        C = Ctx(nc, stack)
        C.psum_banks()
        P = C.P
        gt = C.sb("gains", [128, 3 * depth + 1, KD], F32)
        selt = C.sb("selc", [128, 2], F32)
        ones = C.sb("ones", [128, 128], BF16)
        epsc = C.sb("epsc", [128, 1], F32)
        base = {"ones": ones, "epsc": epsc, "sq": C.sb("sq", [128, KD, TT], BF16), "rstd": C.sb("rstd", [128, TT], F32)}
        P.op("pool", lambda e: e.memset(ones[:, :], 1.0), writes=["ones"])
        P.op("pool", lambda e: e.memset(epsc[:, :], EPS), writes=["epsc"])
        P.op("sp", lambda e: e.dma_start(out=gt[:, :, :], in_=gains[:, :, :]), writes=["gains"], dma_key="gains")
        P.op("sp", lambda e: e.dma_start(out=selt[:, :], in_=selc[:, :]), writes=["selc"], dma_key="selc")
        C.gen_banks = [0, 1, 2]
        with ExitStack() as st:
            mixer_build_tables(C, cin, rel_d, fc, fw, efd, 0, st)
            P.barrier()
        C.gen_banks = list(range(8))
        gi = {"ffn1": 0, "mixn": 1}
        emit_tok_phase(C, True, False, base, gt, gi, {"wg1": LW_[0]["wg1"], "wu1": LW_[0]["wu1"], "wd1": LW_[0]["wd1"]},
                       x_in, xs, None, h_dst, None)
        for l in range(depth):
            par = l % 2
            for c in range(HC):
                P.op("pool", lambda e, par=par, c=c: e.collective_compute("AllGather", ALU.bypass, replica_groups=PAIRS, ins=[h_src_t[c].ap().opt()],
                                                                          outs=[h_all_t[par][c].ap().opt()]),
                     writes=[("h_all", par, c)], dma_key="cc", dma_inc=1)
                P.wait_all("pool", [("h_all", par, c)])
            P.barrier()
            C.gen_banks = [0, 1, 2]
            with ExitStack() as sm:
                T = mixer_alloc_consts(C, cin, fc, fw, efd, 0, sm)
                L = alloc_mixer_layer_tiles(C, sm)
                od = {"oT": o_dst, "fc": fc, "fkeys": [("F", 0, "c", 0), ("F", 0, "c", 1)], "oa_c0": 0, "ob_c0": 2}
                hsrc = lambda B, par=par: h_all[par][(B % 4) // 2][B // 4, :, :, ((B % 4) % 2) * BLK:((B % 4) % 2 + 1) * BLK]
                emit_mixer_layer(C, T, L, LW_[l]["mix"], hsrc, od)
                P.barrier()
            for c in range(OC_):
                P.op("pool", lambda e, par=par, c=c: e.collective_compute("AllGather", ALU.bypass, replica_groups=PAIRS, ins=[o_src_t[c].ap().opt()],
                                                                          outs=[o_all_t[par][c].ap().opt()]),
                     writes=[("o_all", par, c)], dma_key="cc", dma_inc=1)
                P.wait_all("pool", [("o_all", par, c)])
            P.barrier()
            C.gen_banks = list(range(8))
            last = (l == depth - 1)
            gi = {"mixp": 3 * l + 1, "ffn2": 3 * l + 2, "ffn1": 3 * (l + 1), "mixn": 3 * (l + 1) + 1, "final": 3 * depth}
            dr = {k: LW_[l][k] for k in ("wgm", "wbn", "wbg", "wo", "wg2", "wu2", "wd2")}
            if not last:
                dr.update({k: LW_[l + 1][k] for k in ("wg1", "wu1", "wd1")})

            def make_loader(C_, st_, par=par):
                bt = C_.sb("oTalt", [128, 12, TT], BF16, st_)

                def loader(it, oT, okey):
                    for hh, dst in ((0, oT), (1, bt)):
                        tg = hh * NTOK + it * TT
                        oc_ = o_all[par][tg // 1024]
                        t0 = tg % 1024
                        for r in range(2):
                            P.op("sp", lambda e, dst=dst, r=r, t0=t0, oc_=oc_: e.dma_start(out=dst[:, 2 * r:2 * r + 2, :], in_=oc_[r, :, 0:2, t0:t0 + TT]),
                                 writes=[okey if hh == 0 else "oTalt"], dma_key=(okey if hh == 0 else "oTalt"))
                            P.op("sp", lambda e, dst=dst, r=r, t0=t0, oc_=oc_: e.dma_start(out=dst[:, 4 + 4 * r:8 + 4 * r, :], in_=oc_[r, :, 2:6, t0:t0 + TT]),
                                 writes=[okey if hh == 0 else "oTalt"], dma_key=(okey if hh == 0 else "oTalt"))
                    P.op("pool", lambda e: e.tensor_scalar(out=bt[:, :, :], in0=bt[:, :, :], scalar1=selt[:, 1:2], scalar2=None, op0=ALU.mult),
                         reads=["oTalt", "selc"], writes=["oTalt"])
                    P.op("dve", lambda e, oT=oT: e.scalar_tensor_tensor(out=oT[:, :, :], in0=oT[:, :, :], scalar=selt[:, 0:1], in1=bt[:, :, :],
                                                                         op0=ALU.mult, op1=ALU.add),
                         reads=[okey, "oTalt", "selc"], writes=[okey])
                return loader
            emit_tok_phase(C, False, last, base, gt, gi, dr, xs, xs, None, h_dst, y_out, oT_loader=make_loader)
        P.wait_all("sp", [("hst", 0), ("hst", 1)])
        global _last_prog
        _last_prog = P
        P.emit(stack)
    return nc


def fused8_inputs(inp, c, depth=DEPTH):
    b, h = c // 2, c % 2
    m = {"xT_in": to_feature_major(inp["x"][b, h * NTOK:(h + 1) * NTOK])}
    g = np.zeros((128, 3 * depth + 1, KD), np.float32)
    for l in range(depth):
        g[:, 3 * l] = gain_cols(inp["ffn1_norm"][l])
        g[:, 3 * l + 1] = gain_cols(inp["mix_norm"][l])
        g[:, 3 * l + 2] = gain_cols(inp["ffn2_norm"][l])
    g[:, 3 * depth] = gain_cols(inp["final_norm"])
    m["gains"] = g
    sel = np.zeros((128, 2), np.float32)
    sel[:, h] = 1.0
    m["selc"] = sel
    for k, v in mixer_consts().items():
        m["c_" + k] = v
    for l in range(depth):
        m["wg1_l%d" % l] = inp["ffn1_w_gate"][l]
        m["wu1_l%d" % l] = inp["ffn1_w_up"][l]
        m["wd1_l%d" % l] = inp["ffn1_w_down"][l]
        m["wg2_l%d" % l] = inp["ffn2_w_gate"][l]
        m["wu2_l%d" % l] = inp["ffn2_w_up"][l]
        m["wd2_l%d" % l] = inp["ffn2_w_down"][l]
        m["wgm_l%d" % l] = np.ascontiguousarray(inp["w_in"][l][:, OFF_GM:OFF_GM + 2048])
        m["wbn_l%d" % l] = inp["w_branch_nsa"][l]
        m["wbg_l%d" % l] = inp["w_branch_gla"][l]
        m["wo_l%d" % l] = inp["w_out"][l]
        for k, v in mixer_weight_inputs(inp, l, h).items():
            if k == "rel":
                m["rel"] = v
            else:
                m["%s_l%d" % (k, l)] = v
    return m


def kernel_fused8(**inp):
    inp = {k: np.asarray(v) for k, v in inp.items()}
    nc = _prog("fused8", lambda: build_fused8_program())
    maps = [fused8_inputs(inp, c) for c in range(8)]
    res = run_bass_kernel_spmd(nc, maps, core_ids=list(range(8))).results
    out = np.zeros((NB, S, D), np.float32)
    for c in range(8):
        b, h = c // 2, c % 2
        out[b, h * NTOK:(h + 1) * NTOK] = from_feature_major(res[c]["y_out"])
    return out
```

```python
import numpy as np
from contextlib import ExitStack
import concourse.bass as bass
import concourse.mybir as mybir
from concourse.bass_utils import run_bass_kernel_spmd

F32 = mybir.dt.float32
BF16 = mybir.dt.bfloat16
AF = mybir.ActivationFunctionType
ALU = mybir.AluOpType
AX = mybir.AxisListType

D = 1024
DFF = 2816
S = 4096
NB = 4
DEPTH = 4
NTOK = 2048
KD = D // 128
KF = DFF // 128
EPS = 1e-6
D_IN = 6440
OFF_QA = 0
OFF_KVA = 512
OFF_GA = 512 + 768
OFF_QB = OFF_GA + 24
OFF_KB = OFF_QB + 512
OFF_VB = OFF_KB + 512
OFF_ALR = OFF_VB + 1024
OFF_RB = OFF_ALR + 16
OFF_GM = OFF_RB + 1024
assert OFF_GM + 2048 == D_IN

ENGS = ("pe", "act", "dve", "pool", "sp")


class Op:
    __slots__ = ("eng", "fn", "waits", "pos", "marked", "dma_key", "dma_cnt", "dma_inc")

    def __init__(self, eng, fn):
        self.eng = eng
        self.fn = fn
        self.waits = []
        self.pos = None
        self.marked = False
        self.dma_key = None
        self.dma_cnt = 0
        self.dma_inc = 16


class Prog:
    def __init__(self, nc):
        self.nc = nc
        self.ops = {e: [] for e in ENGS}
        self.last_writer = {}
        self.readers = {}
        self.waited = {e: {} for e in ENGS}
        self.dma_counts = {}
        self.same_engine_sync = {"act": True, "dve": True, "pool": True, "pe": False, "sp": False}

    def _add_wait(self, op, prod):
        e = op.eng
        if prod.dma_key is not None:
            k = ("dma", prod.dma_key)
            if self.waited[e].get(k, 0) >= prod.dma_cnt:
                return
            self.waited[e][k] = prod.dma_cnt
            op.waits.append(("dma", prod.dma_key, prod.dma_cnt))
        else:
            if prod.eng == e and not self.same_engine_sync[e]:
                return
            k = ("eng", prod.eng)
            if self.waited[e].get(k, -1) >= prod.pos:
                return
            self.waited[e][k] = prod.pos
            prod.marked = True
            op.waits.append(("eng", prod.eng, prod.pos))

    def op(self, eng, fn, reads=(), writes=(), dma_key=None, dma_inc=16):
        o = Op(eng, fn)
        o.pos = len(self.ops[eng])
        if dma_key is not None:
            o.dma_key = dma_key
            o.dma_inc = dma_inc
            self.dma_counts[dma_key] = self.dma_counts.get(dma_key, 0) + dma_inc
            o.dma_cnt = self.dma_counts[dma_key]
        for k in reads:
            w = self.last_writer.get(k)
            if w is not None:
                self._add_wait(o, w)
        for k in writes:
            w = self.last_writer.get(k)
            if w is not None:
                self._add_wait(o, w)
            for r in self.readers.get(k, ()):
                self._add_wait(o, r)
        for k in reads:
            self.readers.setdefault(k, []).append(o)
        for k in writes:
            self.last_writer[k] = o
            self.readers[k] = []
        self.ops[eng].append(o)
        return o

    def wait_all(self, eng, keys):
        o = Op(eng, None)
        o.pos = len(self.ops[eng])
        for k in keys:
            w = self.last_writer.get(k)
            if w is not None:
                self._add_wait(o, w)
        self.ops[eng].append(o)
        return o

    def barrier(self):
        lasts = {e: (self.ops[e][-1] if self.ops[e] else None) for e in ENGS}
        last_comp = {}
        for e in ENGS:
            for o in reversed(self.ops[e]):
                if o.fn is not None and o.dma_key is None:
                    last_comp[e] = o
                    break
        dma_now = dict(self.dma_counts)
        for e in ENGS:
            o = Op(e, None)
            o.pos = len(self.ops[e])
            for pe_, lo in last_comp.items():
                if pe_ == e and e in ("pe", "sp"):
                    continue
                k = ("eng", pe_)
                if self.waited[e].get(k, -1) >= lo.pos:
                    continue
                self.waited[e][k] = lo.pos
                lo.marked = True
                o.waits.append(("eng", pe_, lo.pos))
            for dk, c in dma_now.items():
                k = ("dma", dk)
                if self.waited[e].get(k, 0) >= c:
                    continue
                self.waited[e][k] = c
                o.waits.append(("dma", dk, c))
            self.ops[e].append(o)

    def emit(self, stack):
        nc = self.nc
        sems = {e: stack.enter_context(nc.semaphore("s_" + e)) for e in ENGS}
        dsems = {k: stack.enter_context(nc.semaphore("d_" + str(i))) for i, k in enumerate(self.dma_counts)}
        cnt = {}
        for e in ENGS:
            c = 0
            for o in self.ops[e]:
                if o.marked:
                    c += 1
                cnt[(e, o.pos)] = c
        block = stack.enter_context(nc.Block())

        def run(e, engobj):
            for o in self.ops[e]:
                for w in o.waits:
                    if w[0] == "dma":
                        engobj.wait_ge(dsems[w[1]], w[2])
                    else:
                        engobj.wait_ge(sems[w[1]], cnt[(w[1], w[2])])
                if o.fn is None:
                    continue
                ins = o.fn(engobj)
                if o.dma_key is not None:
                    if o.dma_inc == 1:
                        ins.then_inc(dsems[o.dma_key])
                    else:
                        ins.then_inc(dsems[o.dma_key], o.dma_inc)
                elif o.marked:
                    ins.then_inc(sems[e], 1)

        @block.tensor
        def _(eng):
            run("pe", eng)

        @block.scalar
        def _(eng):
            run("act", eng)

        @block.vector
        def _(eng):
            run("dve", eng)

        @block.gpsimd
        def _(eng):
            run("pool", eng)

        @block.sync
        def _(eng):
            run("sp", eng)


class Ctx:
    def __init__(self, nc, stack):
        self.nc = nc
        self.stack = stack
        self.P = Prog(nc)
        self.ps_i = 0
        self.uid = 0

    def sb(self, name, shape, dt, stack=None):
        self.uid += 1
        return (stack or self.stack).enter_context(self.nc.sbuf_tensor("sb%d_%s" % (self.uid, name), list(shape), dt))

    def psum_banks(self):
        self.ps = [self.stack.enter_context(self.nc.psum_tensor("ps%d" % i, [128, 512], F32)) for i in range(8)]

    def next_ps(self):
        banks = getattr(self, "gen_banks", list(range(8)))
        held = getattr(self, "held", ())
        while True:
            i = banks[self.ps_i % len(banks)]
            self.ps_i += 1
            if i not in held:
                return i


def dram_w(nc, name, shape, dt=F32):
    return nc.dram_tensor(name, list(shape), dt, kind="ExternalInput").ap()


TS = 1024
TT = 512


def emit_rmsnorm_tile(C, xT, t0, g_col, hT, hkey, ones_bf, tmp, xkeys, gkey="gains"):
    P = C.P
    sq, rstd, eps_col = tmp["sq"], tmp["rstd"], tmp["epsc"]
    P.op("act", lambda e: e.activation(out=sq[:, :, :], in_=xT[:, :, t0:t0 + TT], func=AF.Square),
         reads=xkeys, writes=["sq"])
    pi = C.next_ps()
    ps = C.ps[pi]
    for k in range(KD):
        P.op("pe", lambda e, k=k: e.matmul(ps[:, :], lhsT=ones_bf[:, :], rhs=sq[:, k, :], start=(k == 0), stop=(k == KD - 1)),
             reads=["sq", "ones"], writes=[("ps", pi)])
    P.op("act", lambda e: e.activation(out=rstd[:, :], in_=ps[:, :], func=AF.Sqrt, bias=eps_col[:, 0:1], scale=1.0 / D),
         reads=[("ps", pi), "epsc"], writes=["rstd"])
    P.op("dve", lambda e: e.reciprocal(out=rstd[:, :], in_=rstd[:, :]),
         reads=["rstd"], writes=["rstd"])
    for k in range(KD):
        P.op("dve", lambda e, k=k: e.scalar_tensor_tensor(out=hT[:, k, :], in0=xT[:, k, t0:t0 + TT], scalar=g_col[:, k:k + 1],
                                                          in1=rstd[:, :], op0=ALU.mult, op1=ALU.mult),
             reads=xkeys + ["rstd", "gains"], writes=[hkey])


def emit_ffn(C, xT, g_col, wg, wu, wd, tiles, lname):
    P = C.P
    nc = C.nc
    hT, act = tiles["hT"], tiles["act"]
    wgu, wdn = tiles["wgu"], tiles["wdn"]
    ones_bf = tiles["ones"]
    sgs = tiles["sg"]
    wg_v = wg.rearrange("(k p) f -> p k f", p=128)
    wu_v = wu.rearrange("(k p) f -> p k f", p=128)
    wd_v = wd.rearrange("(f p) d -> p f d", p=128)
    NG = KF // 2
    st = tiles["state"]
    for ts in range(NTOK // TS):
        xk = [("xT", ts)]
        for tt in range(TS // TT):
            emit_rmsnorm_tile(C, xT, ts * TS + tt * TT, g_col, hT[tt], ("hT", tt), ones_bf, tiles, xk)
        for g in range(NG):
            slot = st["gu"] % 2
            st["gu"] += 1
            w = wgu[slot]
            P.op("pool", lambda e, w=w, g=g: e.dma_start(out=w[:, 0, :, :], in_=wg_v[:, :, g * 256:(g + 1) * 256]),
                 writes=[("wgu", slot)], dma_key=("wgu", slot))
            P.op("pool", lambda e, w=w, g=g: e.dma_start(out=w[:, 1, :, :], in_=wu_v[:, :, g * 256:(g + 1) * 256]),
                 writes=[("wgu", slot)], dma_key=("wgu", slot))
            for fi in range(2):
                f = g * 2 + fi
                for tt in range(TS // TT):
                    pg, pu = C.next_ps(), C.next_ps()
                    for k in range(KD):
                        P.op("pe", lambda e, k=k, pg=pg, w=w, fi=fi, tt=tt: e.matmul(
                            C.ps[pg][:, :], lhsT=w[:, 0, k, fi * 128:(fi + 1) * 128], rhs=hT[tt][:, k, :],
                            start=(k == 0), stop=(k == KD - 1)),
                            reads=[("wgu", slot), ("hT", tt)], writes=[("ps", pg)])
                    for k in range(KD):
                        P.op("pe", lambda e, k=k, pu=pu, w=w, fi=fi, tt=tt: e.matmul(
                            C.ps[pu][:, :], lhsT=w[:, 1, k, fi * 128:(fi + 1) * 128], rhs=hT[tt][:, k, :],
                            start=(k == 0), stop=(k == KD - 1)),
                            reads=[("wgu", slot), ("hT", tt)], writes=[("ps", pu)])
                    si = st["sg"] % len(sgs)
                    st["sg"] += 1
                    sg = sgs[si]
                    P.op("act", lambda e, sg=sg, pg=pg: e.activation(out=sg[:, :], in_=C.ps[pg][:, :], func=AF.Silu),
                         reads=[("ps", pg)], writes=[("sg", si)])
                    P.op("dve", lambda e, sg=sg, pu=pu, f=f, tt=tt: e.tensor_tensor(
                        out=act[:, f, tt * TT:(tt + 1) * TT], in0=C.ps[pu][:, :], in1=sg[:, :], op=ALU.mult),
                        reads=[("ps", pu), ("sg", si)], writes=[("act", f, tt)])
        for dg in range(4):
            slot = st["dn"] % 2
            st["dn"] += 1
            w = wdn[slot]
            P.op("pool", lambda e, w=w, dg=dg: e.dma_start(out=w[:, :, :], in_=wd_v[:, :, dg * 256:(dg + 1) * 256]),
                 writes=[("wdn", slot)], dma_key=("wdn", slot))
            for di in range(2):
                dch = dg * 2 + di
                for tt in range(TS // TT):
                    pd = C.next_ps()
                    for f in range(KF):
                        P.op("pe", lambda e, f=f, pd=pd, w=w, di=di, tt=tt: e.matmul(
                            C.ps[pd][:, :], lhsT=w[:, f, di * 128:(di + 1) * 128], rhs=act[:, f, tt * TT:(tt + 1) * TT],
                            start=(f == 0), stop=(f == KF - 1)),
                            reads=[("wdn", slot), ("act", f, tt)], writes=[("ps", pd)])
                    t0 = ts * TS + tt * TT
                    P.op("dve", lambda e, pd=pd, dch=dch, t0=t0: e.scalar_tensor_tensor(
                        out=xT[:, dch, t0:t0 + TT], in0=C.ps[pd][:, :], scalar=0.5, in1=xT[:, dch, t0:t0 + TT],
                        op0=ALU.mult, op1=ALU.add),
                        reads=[("ps", pd), ("xT", ts)], writes=[("xT", ts)])


def alloc_ffn_tiles(C, stack):
    t = {}
    t["hT"] = [C.sb("hT%d" % i, [128, KD, TT], BF16, stack) for i in range(TS // TT)]
    t["act"] = C.sb("act", [128, KF, TS], BF16, stack)
    t["wgu"] = [C.sb("wgu%d" % i, [128, 2, KD, 256], BF16, stack) for i in range(2)]
    t["wdn"] = [C.sb("wdn%d" % i, [128, KF, 256], BF16, stack) for i in range(2)]
    t["sg"] = [C.sb("sg%d" % i, [128, TT], BF16, stack) for i in range(3)]
    t["state"] = {"gu": 0, "dn": 0, "sg": 0}
    return t


def build_ffn_test():
    nc = bass.Bass("TRN2", target_bir_lowering=False)
    x_in = dram_w(nc, "xT_in", [128, KD, NTOK])
    gain = dram_w(nc, "gain", [128, KD])
    wg = dram_w(nc, "wg", [D, DFF])
    wu = dram_w(nc, "wu", [D, DFF])
    wd = dram_w(nc, "wd", [DFF, D])
    y = nc.dram_tensor("xT_out", [128, KD, NTOK], F32, kind="ExternalOutput").ap()
    with ExitStack() as stack:
        C = Ctx(nc, stack)
        C.psum_banks()
        xT = C.sb("xT", [128, KD, NTOK], F32)
        g_col = C.sb("g_col", [128, KD], F32)
        ones = C.sb("ones", [128, 128], BF16)
        P = C.P
        P.op("pool", lambda e: e.memset(ones[:, :], 1.0), writes=["ones"])
        for ts in range(NTOK // TS):
            P.op("sp", lambda e, ts=ts: e.dma_start(out=xT[:, :, ts * TS:(ts + 1) * TS], in_=x_in[:, :, ts * TS:(ts + 1) * TS]),
                 writes=[("xT", ts)], dma_key=("xT", ts))
        P.op("sp", lambda e: e.dma_start(out=g_col[:, :], in_=gain[:, :]), writes=["gains"], dma_key="gains")
        tiles = alloc_ffn_tiles(C, stack)
        tiles["sq"] = C.sb("sq", [128, KD, TT], BF16)
        tiles["rstd"] = C.sb("rstd", [128, TT], F32)
        tiles["ones"] = ones
        epsc = C.sb("epsc", [128, 1], F32)
        P.op("pool", lambda e: e.memset(epsc[:, :], EPS), writes=["epsc"])
        tiles["epsc"] = epsc
        emit_ffn(C, xT, g_col, wg, wu, wd, tiles, "ffn1")
        for ts in range(NTOK // TS):
            P.op("sp", lambda e, ts=ts: e.dma_start(out=y[:, :, ts * TS:(ts + 1) * TS], in_=xT[:, :, ts * TS:(ts + 1) * TS]),
                 reads=[("xT", ts)], writes=[("yout", ts)], dma_key=("yout", ts))
        P.wait_all("sp", [("yout", ts) for ts in range(NTOK // TS)])
        P.emit(stack)
    return nc


def to_feature_major(x2d):
    n = x2d.shape[0]
    return np.ascontiguousarray(x2d.T.reshape(KD, 128, n).transpose(1, 0, 2))


def from_feature_major(xT):
    n = xT.shape[2]
    return np.ascontiguousarray(xT.transpose(1, 0, 2).reshape(D, n).T)


def gain_cols(g):
    return np.ascontiguousarray(g.reshape(KD, 128).T)


QT128 = 128
NQT = S // 128
BLK = 512
NBLK = S // BLK
NEG = -30000.0
LC = 6656
OFFD = 2064
LW = 1024
OFFW = 128
CQ, CKC, CVC, CKS, CKW, CALR, CQB, CKBT = 0, 256, 384, 512, 576, 640, 656, 912
CFM_END = 1168
CVS, CVW, CGA, CKB, CVB, CRB = 1168, 1232, 1296, 1308, 1564, 2076
NCOLS = 2588


def rel_bucket_np(d):
    n = np.maximum(d, 0)
    nf = np.maximum(n, 1).astype(np.float32)
    log_b = 16 + (np.log(nf / np.float32(16)) / np.float32(np.log(1024 / 16)) * np.float32(16)).astype(np.int32)
    return np.where(n < 16, n, np.minimum(log_b, 31))


_CONSTS = {}


def mixer_consts():
    if _CONSTS:
        return _CONSTS
    c = {}
    dd = np.arange(LC) - OFFD
    b = rel_bucket_np(dd)
    oh = np.zeros((33, LC), np.float32)
    for i in range(LC):
        if dd[i] >= 0:
            oh[b[i], i] = 1.0
        else:
            oh[32, i] = 1.0
    c["oh_c"] = oh
    dd = np.arange(LW) - OFFW
    b = rel_bucket_np(dd)
    oh = np.zeros((33, LW), np.float32)
    for i in range(LW):
        if 0 <= dd[i] < 512:
            oh[b[i], i] = 1.0
        else:
            oh[32, i] = 1.0
    c["oh_w"] = oh
    o31 = np.zeros((33, 128), np.float32)
    o31[31, :] = 1.0
    c["oh31"] = o31
    ov = np.zeros((256, 64), np.float32)
    for p in range(1, 256):
        cc = p - 1
        for j in range(64):
            if (16 * cc < 64 * j + 64) and (16 * cc + 32 > 64 * j):
                ov[p, j] = 1.0
    c["ov"] = np.ascontiguousarray(ov.reshape(2, 128, 64).transpose(1, 0, 2))
    ex = np.zeros((64, S), np.float32)
    for s_ in range(S):
        ex[s_ // 64, s_] = 1.0
    c["exall"] = ex
    jj = np.arange(128)
    same = (jj[:, None] // 64) == (jj[None, :] // 64)
    c["tri_b"] = (np.where(same & (jj[:, None] <= jj[None, :]), -1.0 / 16.0, 0.0)).astype(np.float32)
    c["tri_u"] = (np.where(same & (jj[:, None] > jj[None, :]), -1.0 / 16.0, 0.0)).astype(np.float32)
    c["amask"] = (same & (jj[:, None] <= jj[None, :])).astype(np.float32)
    t = np.arange(S)
    jcur = t // 64
    js = np.arange(64)
    forced = (js[None, :] == 0) | (js[None, :] == jcur[:, None]) | (js[None, :] == jcur[:, None] - 1)
    valid = js[None, :] <= jcur[:, None]
    aadd = np.where(forced, 1e6 + 1000.0 * js[None, :], np.where(valid, 0.0, -1e6 - 1000.0 * js[None, :])).astype(np.float32)
    c["aadd"] = np.ascontiguousarray(aadd.reshape(NQT, 128, 64).transpose(1, 0, 2))
    _CONSTS.update(c)
    return c


MIX_CONST_SHAPES = {"oh_c": [33, LC], "oh_w": [33, LW], "oh31": [33, 128], "ov": [128, 2, 64], "exall": [64, S],
                    "tri_b": [128, 128], "tri_u": [128, 128], "amask": [128, 128], "aadd": [128, NQT, 64]}
MIX_W_SHAPES = {"wmix": [D, NCOLS], "w2a": [16, 256], "ba": [1, 256], "gnorm": [1, 512], "posk": [128, 16], "posv": [128, 16],
                "w1k": [128, 16, 256], "w1v": [128, 16, 256], "w2k": [128, 2, 64], "w2v": [128, 2, 64], "rel": [32, 4]}


def mixer_weight_inputs(inp, l, g):
    w = inp["w_in"][l]
    cols = []
    cols.append(w[:, OFF_QA + g * 256: OFF_QA + (g + 1) * 256])
    for i in (0, 0, 1, 1, 2, 4):
        cols.append(w[:, OFF_KVA + i * 128 + g * 64: OFF_KVA + i * 128 + (g + 1) * 64])
    cols.append(w[:, OFF_ALR:OFF_ALR + 16])
    cols.append(w[:, OFF_QB + g * 256: OFF_QB + (g + 1) * 256])
    cols.append(w[:, OFF_KB + g * 256: OFF_KB + (g + 1) * 256])
    for i in (3, 5):
        cols.append(w[:, OFF_KVA + i * 128 + g * 64: OFF_KVA + i * 128 + (g + 1) * 64])
    cols.append(w[:, OFF_GA + g * 12: OFF_GA + (g + 1) * 12])
    cols.append(w[:, OFF_KB + g * 256: OFF_KB + (g + 1) * 256])
    cols.append(w[:, OFF_VB + g * 512: OFF_VB + (g + 1) * 512])
    cols.append(w[:, OFF_RB + g * 512: OFF_RB + (g + 1) * 512])
    wmix = np.ascontiguousarray(np.concatenate(cols, axis=1))
    assert wmix.shape == (D, NCOLS)
    r = {"wmix": wmix}
    r["w2a"] = np.ascontiguousarray(inp["gla_a_w2"][l][:, g * 256:(g + 1) * 256])
    r["ba"] = np.ascontiguousarray(inp["gla_a_b"][l][None, g * 256:(g + 1) * 256])
    r["gnorm"] = np.ascontiguousarray(inp["gla_out_norm"][l][None, g * 512:(g + 1) * 512])
    r["posk"] = np.ascontiguousarray(inp["cmp_pos_k"][l].reshape(2, 16, 64).transpose(0, 2, 1).reshape(128, 16))
    r["posv"] = np.ascontiguousarray(inp["cmp_pos_v"][l].reshape(2, 16, 64).transpose(0, 2, 1).reshape(128, 16))
    r["w1k"] = np.ascontiguousarray(inp["cmp_k_w1"][l].reshape(2, 16, 64, 256).transpose(0, 2, 1, 3).reshape(128, 16, 256))
    r["w1v"] = np.ascontiguousarray(inp["cmp_v_w1"][l].reshape(2, 16, 64, 256).transpose(0, 2, 1, 3).reshape(128, 16, 256))
    r["w2k"] = np.ascontiguousarray(inp["cmp_k_w2"][l].reshape(2, 128, 64).transpose(1, 0, 2))
    r["w2v"] = np.ascontiguousarray(inp["cmp_v_w2"][l].reshape(2, 128, 64).transpose(1, 0, 2))
    r["rel"] = np.ascontiguousarray(inp["rel_table"][:, g * 4:(g + 1) * 4])
    return r


class MixerTiles:
    pass


def mixer_setup(C, cin, stack):
    nc, P = C.nc, C.P
    T = MixerTiles()
    sb = lambda n, s, d: C.sb(n, s, d, stack)
    T.ident_f = sb("ident_f", [128, 128], F32)
    T.ident_b = sb("ident_b", [128, 128], BF16)
    T.exall_d = cin["exall"]
    T.tri_b = sb("tri_b", [128, 128], F32)
    T.tri_u = sb("tri_u", [128, 128], F32)
    T.amask = sb("amask", [128, 128], F32)
    T.aadd = [sb("aadd%d" % i, [128, 64], F32) for i in range(2)]
    T.aadd_d = cin["aadd"]
    T.one_col = sb("one_col", [128, 1], F32)
    T.eps_col = sb("eps_col", [128, 1], F32)
    T.tiny_col = sb("tiny_col", [128, 1], F32)
    T.ones_f = sb("ones_f", [1, 128], F32)
    T.efar = sb("efar", [128, 4], F32)
    T.bsel = sb("bsel", [128, 8, 512], BF16)
    T.bwin = sb("bwin", [128, 5, 512], BF16)
    T.voc = sb("voc", [128, 2, 129], BF16)
    T.kcT = sb("kcT", [128, 256], BF16)
    P.op("pool", lambda e: e.memset(T.ident_f[:, :], 0.0), writes=["ident_f"])
    P.op("pool", lambda e: e.affine_select(out=T.ident_f[:, :], in_=T.ident_f[:, :], pattern=[[-1, 128]], compare_op=ALU.not_equal,
                                           fill=1.0, base=0, channel_multiplier=1), reads=["ident_f"], writes=["ident_f"])
    P.op("pool", lambda e: e.tensor_copy(out=T.ident_b[:, :], in_=T.ident_f[:, :]), reads=["ident_f"], writes=["ident_b"])
    P.op("pool", lambda e: e.memset(T.one_col[:, :], 1.0), writes=["one_col"])
    P.op("pool", lambda e: e.memset(T.eps_col[:, :], EPS), writes=["eps_col"])
    P.op("pool", lambda e: e.memset(T.tiny_col[:, :], 1e-30), writes=["tiny_col"])
    P.op("pool", lambda e: e.memset(T.ones_f[:, :], 1.0), writes=["ones_f"])
    P.op("pool", lambda e: e.memset(T.voc[:, :, :], 0.0), writes=["voc"])
    P.op("pool", lambda e: e.memset(T.kcT[:, :], 0.0), writes=["kcT"])
    P.op("pool", lambda e: e.memset(T.voc[:, :, 64:65], 1.0), reads=["voc"], writes=["voc"])
    P.op("pool", lambda e: e.memset(T.voc[0:1, 0, 64:65], 0.0), reads=["voc"], writes=["voc"])
    P.op("pool", lambda e: e.dma_start(out=T.voc[:, :, 65:129], in_=cin["ov"][:, :, :]), reads=["voc"], writes=["voc"], dma_key="voc_ov")
    for nm in ("tri_b", "tri_u", "amask"):
        t = getattr(T, nm)
        P.op("sp", lambda e, t=t, nm=nm: e.dma_start(out=t[:, :], in_=cin[nm][:, :]), writes=[nm], dma_key=nm)
    relx = sb("relx", [33, 4], F32)
    ones33 = sb("ones33", [33, 128], F32)
    relb = sb("relb", [33, 4, 128], F32)
    ohb = [sb("ohb%d" % i, [33, 512], F32) for i in range(1)]
    oh31 = sb("oh31", [33, 128], F32)
    fsb = [sb("fsb%d" % i, [128, 512], BF16) for i in range(2)]
    P.op("sp", lambda e: e.dma_start(out=relx[0:32, :], in_=cin["rel"][:, :]), writes=["relx"], dma_key="relx")
    P.op("pool", lambda e: e.memset(relx[32:33, :], NEG), writes=["relx32"])
    P.op("pool", lambda e: e.memset(ones33[:, :], 1.0), writes=["ones33"])
    P.op("sp", lambda e: e.dma_start(out=oh31[:, :], in_=cin["oh31"][:, :]), writes=["oh31"], dma_key="oh31")
    pi = C.next_ps()
    P.op("pe", lambda e: e.matmul(C.ps[pi][:, 0:4], lhsT=oh31[:, :], rhs=relx[:, :], start=True, stop=True),
         reads=["oh31", "relx", "relx32"], writes=[("ps", pi)])
    P.op("act", lambda e: e.activation(out=T.efar[:, :], in_=C.ps[pi][:, 0:4], func=AF.Exp), reads=[("ps", pi)], writes=["efar"])
    for h in range(4):
        P.op("dve", lambda e, h=h: e.tensor_scalar(out=relb[:, h, :], in0=ones33[:, :], scalar1=relx[:, h:h + 1], scalar2=None, op0=ALU.mult),
             reads=["ones33", "relx", "relx32"], writes=["relb"])
    it = 0
    for tab, src_oh, L_, scr in (("c", cin["oh_c"], LC, cin["fc"]), ("w", cin["oh_w"], LW, cin["fw"])):
        for blk in range(L_ // 512):
            ob = ohb[0]
            okey = ("ohb", 0)
            P.op("sp", lambda e, ob=ob, blk=blk, src_oh=src_oh: e.dma_start(out=ob[:, :], in_=src_oh[:, blk * 512:(blk + 1) * 512]),
                 writes=[okey], dma_key=okey)
            for h in range(4):
                pi = C.next_ps()
                fb = fsb[it % 2]
                fkey = ("fsb", it % 2)
                par = it % 2
                it += 1
                P.op("pe", lambda e, pi=pi, ob=ob, h=h: e.matmul(C.ps[pi][:, :], lhsT=relb[:, h, :], rhs=ob[:, :], start=True, stop=True),
                     reads=["relb", okey], writes=[("ps", pi)])
                P.op("act", lambda e, pi=pi, fb=fb: e.activation(out=fb[:, :], in_=C.ps[pi][:, :], func=AF.Copy),
                     reads=[("ps", pi)], writes=[fkey])
                P.op("sp", lambda e, h=h, scr=scr, fb=fb, blk=blk: e.dma_start(out=scr[h, :, blk * 512:(blk + 1) * 512], in_=fb[:, :]),
                     reads=[fkey], writes=[("F", tab, par)], dma_key=("Fst", par))
    for dl in range(8):
        src = bass.AP(cin["fc"].tensor, OFFD + 128 * dl, [[LC - 1, 128], [128 * LC, 4], [1, 128]])
        P.op("sp", lambda e, dl=dl, src=src: e.dma_start(out=T.bsel[:, dl, :].rearrange("p (h t) -> p h t", h=4), in_=src),
             reads=[("F", "c", 0), ("F", "c", 1)], writes=["bsel"], dma_key="bsel")
    for dl in range(5):
        src = bass.AP(cin["fw"].tensor, OFFW + 128 * dl, [[LW - 1, 128], [128 * LW, 4], [1, 128]])
        P.op("sp", lambda e, dl=dl, src=src: e.dma_start(out=T.bwin[:, dl, :].rearrange("p (h t) -> p h t", h=4), in_=src),
             reads=[("F", "w", 0), ("F", "w", 1)], writes=["bwin"], dma_key="bwin")
    return T


def alloc_mixer_layer_tiles(C, stack):
    sb = lambda n, s, d: C.sb(n, s, d, stack)
    L = MixerTiles()
    L.wmix = sb("wmix", [128, KD, NCOLS], BF16)
    L.w2a = sb("w2a", [16, 256], F32)
    L.ba = sb("ba", [1, 256], F32)
    L.gnb = sb("gnb", [128, 512], F32)
    L.posk = sb("posk", [128, 16], BF16)
    L.posv = sb("posv", [128, 16], BF16)
    L.w1k = sb("w1k", [128, 16, 256], BF16)
    L.w1v = sb("w1v", [128, 16, 256], BF16)
    L.w2k = sb("w2k", [128, 2, 64], BF16)
    L.w2v = sb("w2v", [128, 2, 64], BF16)
    L.pbk = sb("pbk", [128, 4], F32)
    L.pbv = sb("pbv", [128, 4], F32)
    L.htmp = sb("htmp", [128, 32], F32)
    L.hTb = [sb("hTb%d" % i, [128, KD, BLK], BF16) for i in range(2)]
    L.ksT = sb("ksT", [128, S], BF16)
    L.kwT = sb("kwT", [128, S], BF16)
    L.kcrT = sb("kcrT", [128, 16 + S], BF16)
    L.vcrT = sb("vcrT", [128, 16 + S], BF16)
    L.vs = sb("vs", [128, NQT, 65], BF16)
    L.vw = sb("vw", [128, NQT, 65], BF16)
    L.qT = [sb("qT%d" % i, [128, 4, 4, 128], BF16) for i in range(2)]
    L.alrT = sb("alrT", [16, BLK], F32)
    L.qbT = sb("qbT", [128, 2, BLK], BF16)
    L.kbT = sb("kbT", [128, 2, BLK], BF16)
    L.hidk = sb("hidk", [128, 2, 32], BF16)
    L.hidv = sb("hidv", [128, 2, 32], BF16)
    L.vstage = [sb("vstage%d" % i, [32, 64], BF16) for i in range(2)]
    L.bcmp = [sb("bcmp%d" % i, [128, 2, 512], BF16) for i in range(2)]
    L.pt = [sb("pt%d" % i, [128, 512], BF16) for i in range(5)]
    L.gates = [sb("gates%d" % i, [128, 12], F32) for i in range(2)]
    L.kbtok = [sb("kbtok%d" % i, [128, 256], BF16) for i in range(2)]
    L.vb = [sb("vb%d" % i, [128, 512], BF16) for i in range(2)]
    L.gsr = [sb("gsr%d" % i, [128, 512], F32) for i in range(2)]
    L.ez = sb("ez", [128, 256], F32)
    L.sp = sb("sp", [128, 256], F32)
    L.eb = [sb("eb%d" % i, [128, 128], F32) for i in range(2)]
    L.enb = [sb("enb%d" % i, [128, 128], F32) for i in range(2)]
    L.erb = sb("erb", [128, 256], F32)
    L.kstate = sb("kstate", [128, 256], BF16)
    L.qdec = [sb("qdec%d" % i, [128, 128], BF16) for i in range(2)]
    L.kint = [sb("kint%d" % i, [128, 128], BF16) for i in range(2)]
    L.at = [sb("at%d" % i, [128, 128], BF16) for i in range(2)]
    L.state = sb("state", [128, 2, 256], F32)
    L.sbf = [[sb("sbf%d_%d" % (h, i), [128, 256], BF16) for i in range(2)] for h in range(2)]
    L.junk = sb("junk", [128, 256], F32)
    L.ssq = sb("ssq", [128, 2], F32)
    L.ob = sb("ob", [128, 512], F32)
    L.obT = [sb("obT%d" % i, [128, 4, 128], BF16) for i in range(2)]
    L.oacc = sb("oacc", [128, 4, 64], F32)
    L.oaT = [sb("oaT%d" % i, [128, 2, 128], BF16) for i in range(2)]
    L.onsb = sb("onsb", [128, 4, 65], F32)
    L.osel = sb("osel", [128, 4, 65], F32)
    L.small = sb("small", [128, 64], F32)
    L.imp = sb("imp", [128, 64], F32)
    L.sc = sb("sc", [128, 64], F32)
    L.sc2 = sb("sc2", [128, 64], F32)
    L.m8 = sb("m8", [128, 8], F32)
    L.nsel = sb("nsel", [128, 128], F32)
    L.cnt = {"pt": 0, "sbf": [0, 0]}
    return L


B_OF, B_ON, B_OW, B_OC, B_IMP = 3, 4, 5, 3, 4
MIX_GEN_BANKS = [0, 1, 2, 6, 7]


def emit_mixer_layer(C, T, L, din, hT_d, oT_d, lidx=0, dbg=None, pre_compute=None):
    nc, P = C.nc, C.P
    C.gen_banks = MIX_GEN_BANKS
    ps = C.ps
    lk = lambda name: (name, lidx)
    P.op("pool", lambda e: e.dma_start(out=L.wmix[:, :, :], in_=din["wmix"].rearrange("(k p) c -> p k c", p=128)),
         writes=["wmix"], dma_key="wmix")
    for nm, t in (("w1k", L.w1k), ("w1v", L.w1v)):
        P.op("pool", lambda e, nm=nm, t=t: e.dma_start(out=t[:, :, :], in_=din[nm][:, :, :]), writes=[nm], dma_key=nm)
    for nm, t in (("w2k", L.w2k), ("w2v", L.w2v)):
        P.op("pool", lambda e, nm=nm, t=t: e.dma_start(out=t[:, :, :], in_=din[nm][:, :, :]), writes=[nm], dma_key=nm)
    for nm, t in (("posk", L.posk), ("posv", L.posv)):
        P.op("pool", lambda e, nm=nm, t=t: e.dma_start(out=t[:, :], in_=din[nm][:, :]), writes=[nm], dma_key=nm)
    P.op("sp", lambda e: e.dma_start(out=L.w2a[:, :], in_=din["w2a"][:, :]), writes=["w2a"], dma_key="w2a")
    P.op("sp", lambda e: e.dma_start(out=L.ba[:, :], in_=din["ba"][:, :]), writes=["ba"], dma_key="ba")
    P.op("sp", lambda e: e.dma_start(out=L.gnb[:, :], in_=bass.AP(din["gnorm"].tensor, 0, [[0, 128], [1, 512]])), writes=["gnb"], dma_key="gnb")
    P.op("pool", lambda e: e.memset(L.state[:, :, :], 0.0), writes=["state0", "state1"])
    for h in range(2):
        P.op("pool", lambda e, h=h: e.memset(L.sbf[h][0][:, :], 0.0), writes=[("sbf", h, 0)])
    P.op("pool", lambda e: e.memset(L.kcrT[0:64, 0:16], 0.0), writes=["kcr_pad"])
    P.op("pool", lambda e: e.memset(L.vcrT[0:64, 0:16], 0.0), writes=["vcr_pad"])
    P.op("pool", lambda e: e.dma_start(out=L.ksT[64:128, :], in_=T.exall_d[:, :]), writes=["exall"], dma_key="exall")
    P.op("pool", lambda e: e.memset(L.kwT[64:128, :], 0.0), writes=["kw_zero"])
    for i in range(2):
        P.op("pool", lambda e, i=i: e.memset(L.qT[i][64:128, :, :, :], 0.0), writes=[("qT", i)])
    P.op("pool", lambda e: e.memset(L.nsel[:, 0:64], 0.0), writes=["nsel_pad"])
    P.op("pool", lambda e: e.memset(L.vs[:, :, 64:65], 1.0), writes=["vs_one"])
    P.op("pool", lambda e: e.memset(L.vw[:, :, 64:65], 1.0), writes=["vw_one"])
    for (w1, pos, pb, nm) in ((L.w1k, L.posk, L.pbk, "k"), (L.w1v, L.posv, L.pbv, "v")):
        pi = C.next_ps()
        for hc in range(2):
            for l in range(16):
                P.op("pe", lambda e, w1=w1, pos=pos, hc=hc, l=l, pi=pi: e.matmul(
                    ps[pi][:, hc:hc + 1], lhsT=w1[:, l, hc * 128:(hc + 1) * 128], rhs=pos[:, l:l + 1],
                    start=(l == 0 and hc == 0), stop=(l == 15), skip_group_check=True),
                    reads=["w1" + nm, "pos" + nm], writes=[("ps", pi)])
        P.op("dve", lambda e, pb=pb, pi=pi: e.tensor_copy(out=pb[:, 0:2], in_=ps[pi][:, 0:2]), reads=[("ps", pi)], writes=["pb" + nm])
        P.op("dve", lambda e, pb=pb, pi=pi: e.tensor_scalar(out=pb[:, 2:4], in0=ps[pi][:, 0:2], scalar1=-1.0, scalar2=None, op0=ALU.mult),
             reads=[("ps", pi), "pb" + nm], writes=["pb" + nm])

    def proj_fm(dst_fn, col0, ncol, hb, hkey, wkeys, evac):
        pi = C.next_ps()
        for k in range(KD):
            P.op("pe", lambda e, k=k, pi=pi: e.matmul(ps[pi][0:ncol, :], lhsT=L.wmix[:, k, col0:col0 + ncol], rhs=hb[:, k, :],
                                                      start=(k == 0), stop=(k == KD - 1)),
                 reads=["wmix", hkey], writes=[("ps", pi)])
        evac(pi)

    if pre_compute is not None:
        pre_compute()
    for B in range(NBLK):
        T0 = B * BLK
        hb = L.hTb[B % 2]
        hkey = ("hTb", B % 2)
        qT = L.qT[B % 2]
        qkey = ("qT", B % 2)
        h_src = hT_d(B) if callable(hT_d) else hT_d[:, :, T0:T0 + BLK]
        P.op("sp", lambda e, hb=hb, h_src=h_src: e.dma_start(out=hb[:, :, :], in_=h_src), writes=[hkey], dma_key=hkey)
        for h in range(4):
            def ev(pi, h=h, qT=qT):
                P.op("dve", lambda e: e.tensor_scalar(out=qT[0:64, :, h, :], in0=ps[pi][0:64, :].rearrange("p (q t) -> p q t", q=4),
                                                      scalar1=0.125, scalar2=None, op0=ALU.mult),
                     reads=[("ps", pi)], writes=[qkey])
            proj_fm(None, CQ + 64 * h, 64, hb, hkey, None, ev)
        for (col, dst, key) in ((CKC, L.kcrT, "kcrT"), (CVC, L.vcrT, "vcrT")):
            def ev(pi, dst=dst, key=key, T0=T0):
                P.op("act", lambda e: e.activation(out=dst[0:64, 16 + T0: 16 + T0 + BLK], in_=ps[pi][0:64, :], func=AF.Copy),
                     reads=[("ps", pi)], writes=[(key, B)])
                P.op("dve", lambda e: e.tensor_copy(out=dst[64:128, T0: T0 + BLK], in_=ps[pi][64:128, :]),
                     reads=[("ps", pi)], writes=[(key + "u", B)])
            proj_fm(None, col, 128, hb, hkey, None, ev)
        for (col, dst, key) in ((CKS, L.ksT, "ksT"), (CKW, L.kwT, "kwT")):
            def ev(pi, dst=dst, key=key, T0=T0):
                P.op("act", lambda e: e.activation(out=dst[0:64, T0: T0 + BLK], in_=ps[pi][0:64, :], func=AF.Copy),
                     reads=[("ps", pi)], writes=[(key, B)])
            proj_fm(None, col, 64, hb, hkey, None, ev)

        def ev(pi):
            P.op("dve", lambda e: e.tensor_copy(out=L.alrT[:, :], in_=ps[pi][0:16, :]), reads=[("ps", pi)], writes=["alrT"])
        proj_fm(None, CALR, 16, hb, hkey, None, ev)
        for h in range(2):
            def ev(pi, h=h):
                P.op("act", lambda e: e.activation(out=L.qbT[:, h, :], in_=ps[pi][:, :], func=AF.Copy), reads=[("ps", pi)], writes=[("qbT", h)])
            proj_fm(None, CQB + 128 * h, 128, hb, hkey, None, ev)

            def ev2(pi, h=h):
                P.op("dve", lambda e: e.tensor_copy(out=L.kbT[:, h, :], in_=ps[pi][:, :]), reads=[("ps", pi)], writes=[("kbT", h)])
            proj_fm(None, CKBT + 128 * h, 128, hb, hkey, None, ev2)
        p0 = 32 * B
        for (w1, w2, raw, rkey, pb, hid, nm) in ((L.w1k, L.w2k, L.kcrT, "kcrT", L.pbk, L.hidk, "k"), (L.w1v, L.w2v, L.vcrT, "vcrT", L.pbv, L.hidv, "v")):
            rk = [(rkey, B), (rkey + "u", B), rkey[0:3] + "_pad"] + ([(rkey, B - 1), (rkey + "u", B - 1)] if B > 0 else [])
            for hc in range(2):
                pi = C.next_ps()
                for l in range(16):
                    rhs = raw[:, 16 * p0 + l: 16 * p0 + l + 16 * 31 + 1: 16]
                    P.op("pe", lambda e, pi=pi, l=l, hc=hc, rhs=rhs, w1=w1: e.matmul(
                        ps[pi][:, 0:32], lhsT=w1[:, l, hc * 128:(hc + 1) * 128], rhs=rhs, start=(l == 0), stop=(l == 15)),
                        reads=rk + ["w1" + nm], writes=[("ps", pi)])
                P.op("act", lambda e, pi=pi, hc=hc, pb=pb: e.activation(out=L.htmp[:, :], in_=ps[pi][:, 0:32], func=AF.Exp, scale=-1.0,
                                                                        bias=pb[:, 2 + hc:3 + hc]),
                     reads=[("ps", pi), "pb" + nm], writes=["htmp"])
                P.op("dve", lambda e: e.tensor_scalar(out=L.htmp[:, :], in0=L.htmp[:, :], scalar1=1.0, scalar2=None, op0=ALU.add), reads=["htmp"], writes=["htmp"])
                P.op("dve", lambda e: e.reciprocal(out=L.htmp[:, :], in_=L.htmp[:, :]), reads=["htmp"], writes=["htmp"])
                P.op("dve", lambda e, pi=pi, hc=hc, hid=hid, pb=pb: e.scalar_tensor_tensor(out=hid[:, hc, :], in0=ps[pi][:, 0:32], scalar=pb[:, hc:hc + 1],
                                                                                            in1=L.htmp[:, :], op0=ALU.add, op1=ALU.mult),
                     reads=[("ps", pi), "pb" + nm, "htmp"], writes=[("hid" + nm, hc)])
            pi = C.next_ps()
            if nm == "k":
                for hc in range(2):
                    P.op("pe", lambda e, pi=pi, hc=hc: e.matmul(ps[pi][0:64, 0:32], lhsT=L.w2k[:, hc, :], rhs=L.hidk[:, hc, :],
                                                                start=(hc == 0), stop=(hc == 1)),
                         reads=[("hidk", hc), "w2k"], writes=[("ps", pi)])
                P.op("dve", lambda e, pi=pi, p0=p0: e.tensor_copy(out=T.kcT[0:64, p0:p0 + 32], in_=ps[pi][0:64, 0:32]), reads=[("ps", pi)], writes=["kcT"])
            else:
                ct, r0 = p0 // 128, p0 % 128
                vst = L.vstage[B % 2]
                for hc in range(2):
                    P.op("pe", lambda e, pi=pi, hc=hc: e.matmul(ps[pi][0:32, 0:64], lhsT=L.hidv[:, hc, :], rhs=L.w2v[:, hc, :],
                                                                start=(hc == 0), stop=(hc == 1)),
                         reads=[("hidv", hc), "w2v"], writes=[("ps", pi)])
                P.op("dve", lambda e, pi=pi, vst=vst: e.tensor_copy(out=vst[:, :], in_=ps[pi][0:32, 0:64]),
                     reads=[("ps", pi)], writes=[("vstage", B % 2)])
                P.op("sp", lambda e, ct=ct, r0=r0, vst=vst: e.dma_start(out=T.voc[r0:r0 + 32, ct, 0:64], in_=vst[:, :]),
                     reads=[("vstage", B % 2)], writes=["voc"], dma_key=("vocst", B % 2))
                if B == 0:
                    P.op("pool", lambda e: e.memset(T.voc[0:1, 0, 0:64], 0.0), reads=["voc"], writes=["voc"])
        for tq in range(4):
            qi = 4 * B + tq
            t0 = qi * 128
            par = qi % 2
            pA, pB_, pC = C.next_ps(), C.next_ps(), C.next_ps()
            for (pi, col, ncol) in ((pA, CVS, CVB - CVS), (pB_, CVB, 512), (pC, CRB, 512)):
                for k in range(KD):
                    P.op("pe", lambda e, k=k, pi=pi, col=col, ncol=ncol, hb=hb, tq=tq: e.matmul(
                        ps[pi][:, 0:ncol], lhsT=hb[:, k, tq * 128:(tq + 1) * 128], rhs=L.wmix[:, k, col:col + ncol],
                        start=(k == 0), stop=(k == KD - 1)),
                        reads=["wmix", hkey], writes=[("ps", pi)])
            P.op("dve", lambda e, pA=pA, qi=qi: e.tensor_copy(out=L.vs[:, qi, 0:64], in_=ps[pA][:, 0:64]), reads=[("ps", pA)], writes=[("vs", qi)])
            P.op("dve", lambda e, pA=pA, qi=qi: e.tensor_copy(out=L.vw[:, qi, 0:64], in_=ps[pA][:, 64:128]), reads=[("ps", pA)], writes=[("vw", qi)])
            P.op("act", lambda e, pA=pA, par=par: e.activation(out=L.gates[par][:, :], in_=ps[pA][:, 128:140], func=AF.Exp, scale=-1.0),
                 reads=[("ps", pA)], writes=[("gates", par)])
            P.op("dve", lambda e, par=par: e.tensor_scalar(out=L.gates[par][:, :], in0=L.gates[par][:, :], scalar1=1.0, scalar2=None, op0=ALU.add),
                 reads=[("gates", par)], writes=[("gates", par)])
            P.op("dve", lambda e, par=par: e.reciprocal(out=L.gates[par][:, :], in_=L.gates[par][:, :]), reads=[("gates", par)], writes=[("gates", par)])
            P.op("dve", lambda e, pA=pA, par=par: e.tensor_copy(out=L.kbtok[par][:, :], in_=ps[pA][:, 140:396]), reads=[("ps", pA)],
                 writes=[("kbtok", par)])
            P.op("act", lambda e, pB_=pB_, par=par: e.activation(out=L.vb[par][:, :], in_=ps[pB_][:, :], func=AF.Copy), reads=[("ps", pB_)],
                 writes=[("vb", par)])
            P.op("act", lambda e, pC=pC, par=par: e.activation(out=L.gsr[par][:, :], in_=ps[pC][:, :], func=AF.Exp, scale=-1.0), reads=[("ps", pC)],
                 writes=[("gsr", par)])
            P.op("dve", lambda e, par=par: e.tensor_scalar(out=L.gsr[par][:, :], in0=L.gsr[par][:, :], scalar1=1.0, scalar2=None, op0=ALU.add),
                 reads=[("gsr", par)], writes=[("gsr", par)])
            P.op("dve", lambda e, par=par: e.reciprocal(out=L.gsr[par][:, :], in_=L.gsr[par][:, :]), reads=[("gsr", par)], writes=[("gsr", par)])
            P.op("dve", lambda e, pC=pC, par=par: e.tensor_tensor(out=L.gsr[par][:, :], in0=ps[pC][:, :], in1=L.gsr[par][:, :], op=ALU.mult),
                 reads=[("ps", pC), ("gsr", par)], writes=[("gsr", par)])
            P.op("pool", lambda e, par=par: e.tensor_tensor(out=L.gsr[par][:, :], in0=L.gsr[par][:, :], in1=L.gnb[:, :], op=ALU.mult),
                 reads=[("gsr", par), "gnb"], writes=[("gsr", par)])
            gla = emit_gla_tile(C, T, L, B, tq, oT_d)
            emit_nsa_tile(C, T, L, B, tq, qT, qkey, oT_d, filler=gla)
            for _ in gla:
                pass


def emit_nsa_tile(C, T, L, B, tq, qT, qkey, oT_d, filler=None):
    nc, P, ps = C.nc, C.P, C.ps
    qi = 4 * B + tq
    t0 = qi * 128
    par = qi % 2
    q_rhs = qT[:, tq, :, :].rearrange("p h t -> p (h t)")
    gates = L.gates[par]
    gkey = ("gates", par)
    sm = L.small
    LA = 2

    def fill():
        if filler is not None:
            next(filler, None)

    def next_pt():
        i = L.cnt["pt"] % len(L.pt)
        L.cnt["pt"] += 1
        return i

    def pv(bank, first, pti, vtile, vkeys, ncol=65):
        for h in range(4):
            P.op("pe", lambda e, h=h: e.matmul(ps[bank][:, h * ncol:(h + 1) * ncol], lhsT=L.pt[pti][:, h * 128:(h + 1) * 128], rhs=vtile,
                                               start=(first and h == 0), stop=True, skip_group_check=True),
                 reads=[("pt", pti)] + vkeys, writes=[("ps", bank)])

    def run_pairs(pairs):
        n = len(pairs)
        for i in range(n + LA):
            if i < n:
                pairs[i][0]()
                fill()
            if i >= LA:
                pairs[i - LA][1]()

    nct = 1 if qi < 16 else 2
    bc = L.bcmp[par]
    for ct in range(nct):
        src = bass.AP(oT_d["fc"].tensor, oT_d["fc"].offset + OFFD + t0 - 2048 * ct - 15, [[LC - 16, 128], [128 * LC, 4], [1, 128]])
        P.op("sp", lambda e, ct=ct, src=src: e.dma_start(out=bc[:, ct, :].rearrange("p (h t) -> p h t", h=4), in_=src),
             reads=oT_d.get("fkeys", [("F", "c", 0), ("F", "c", 1)]), writes=[("bcmp", par, ct)], dma_key=("bcmp", par, ct))
    P.op("sp", lambda e: e.dma_start(out=T.aadd[par][:, :], in_=T.aadd_d[:, qi, :]), writes=[("aadd", par)], dma_key=("aadd", par))
    cpt = []
    for ct in range(nct):
        pi = C.next_ps()
        P.op("pe", lambda e, pi=pi, ct=ct: e.matmul(ps[pi][:, :], lhsT=T.kcT[:, ct * 128:(ct + 1) * 128], rhs=q_rhs, start=True, stop=False),
             reads=["kcT", qkey], writes=[("ps", pi)])
        P.op("pe", lambda e, pi=pi, ct=ct: e.matmul(ps[pi][:, :], lhsT=T.ident_b[:, :], rhs=bc[:, ct, :], start=False, stop=True),
             reads=["ident_b", ("bcmp", par, ct)], writes=[("ps", pi)])
        pti = next_pt()
        cpt.append(pti)
        P.op("act", lambda e, pi=pi, pti=pti: e.activation(out=L.pt[pti][:, :], in_=ps[pi][:, :], func=AF.Exp), reads=[("ps", pi)], writes=[("pt", pti)])
    for ct in range(nct):
        pti = cpt[ct]
        pv(B_OC, ct == 0, pti, T.voc[:, ct, 0:65], ["voc"])
        for h in range(4):
            P.op("pe", lambda e, h=h, ct=ct, pti=pti: e.matmul(ps[B_IMP][:, h * 64:(h + 1) * 64], lhsT=L.pt[pti][:, h * 128:(h + 1) * 128],
                                                               rhs=T.voc[:, ct, 65:129], start=(ct == 0 and h == 0), stop=True, skip_group_check=True),
                 reads=[("pt", pti), "voc"], writes=[("ps", B_IMP)])
    oc = ps[B_OC][:, 0:260].rearrange("p (h c) -> p h c", c=65)
    P.op("dve", lambda e: e.tensor_scalar(out=sm[:, 0:4], in0=oc[:, :, 64], scalar1=1e-30, scalar2=None, op0=ALU.max),
         reads=[("ps", B_OC)], writes=["sm_c"])
    P.op("dve", lambda e: e.reciprocal(out=sm[:, 0:4], in_=sm[:, 0:4]), reads=["sm_c"], writes=["sm_c"])
    P.op("dve", lambda e: e.tensor_scalar(out=L.imp[:, :], in0=ps[B_IMP][:, 0:64], scalar1=sm[:, 0:1], scalar2=None, op0=ALU.mult),
         reads=[("ps", B_IMP), "sm_c"], writes=["imp"])
    for h in range(1, 4):
        P.op("dve", lambda e, h=h: e.scalar_tensor_tensor(out=L.imp[:, :], in0=ps[B_IMP][:, h * 64:(h + 1) * 64], scalar=sm[:, h:h + 1],
                                                          in1=L.imp[:, :], op0=ALU.mult, op1=ALU.add),
             reads=[("ps", B_IMP), "sm_c", "imp"], writes=["imp"])
    P.op("dve", lambda e: e.tensor_tensor(out=L.sc[:, :], in0=L.imp[:, :], in1=T.aadd[par][:, :], op=ALU.add), reads=["imp", ("aadd", par)], writes=["sc"])
    P.op("dve", lambda e: e.max(out=L.m8[:, :], in_=L.sc[:, :]), reads=["sc"], writes=["m8"])
    P.op("dve", lambda e: e.match_replace(out=L.sc2[:, :], in_to_replace=L.m8[:, :], in_values=L.sc[:, :], imm_value=-3e6),
         reads=["sc", "m8"], writes=["sc2"])
    P.op("dve", lambda e: e.max(out=L.m8[:, :], in_=L.sc2[:, :]), reads=["sc2"], writes=["m8"])
    P.op("dve", lambda e: e.match_replace(out=L.sc2[:, :], in_to_replace=L.m8[:, :], in_values=L.sc2[:, :], imm_value=-3e6),
         reads=["sc2", "m8"], writes=["sc2"])
    P.op("dve", lambda e: e.tensor_tensor(out=L.nsel[:, 64:128], in0=L.sc2[:, :], in1=L.sc[:, :], op=ALU.is_equal), reads=["sc2", "sc"], writes=["nsel"])
    gv = gates[:, :].rearrange("p (h b) -> p h b", b=3)
    P.op("dve", lambda e: e.tensor_tensor(out=sm[:, 4:8], in0=sm[:, 0:4], in1=gv[:, :, 0], op=ALU.mult), reads=["sm_c", gkey], writes=["sm_cc"])
    for h in range(4):
        P.op("dve", lambda e, h=h: e.tensor_scalar(out=L.oacc[:, h, :], in0=oc[:, h, 0:64], scalar1=sm[:, 4 + h:5 + h], scalar2=None, op0=ALU.mult),
             reads=[("ps", B_OC), "sm_cc"], writes=["oacc"])
    wpairs = []
    wstate = {"first": True}
    for ki in range(max(0, qi - 4), qi + 1):
        dl = qi - ki
        st = {}

        def s_fn(ki=ki, dl=dl, st=st):
            pi = C.next_ps()
            P.op("pe", lambda e: e.matmul(ps[pi][:, :], lhsT=L.kwT[:, ki * 128:(ki + 1) * 128], rhs=q_rhs, start=True, stop=False),
                 reads=[("kwT", ki // 4), "kw_zero", qkey], writes=[("ps", pi)])
            P.op("pe", lambda e: e.matmul(ps[pi][:, :], lhsT=T.ident_b[:, :], rhs=T.bwin[:, dl, :], start=False, stop=True),
                 reads=["ident_b", "bwin"], writes=[("ps", pi)])
            pti = next_pt()
            st["pti"] = pti
            P.op("act", lambda e: e.activation(out=L.pt[pti][:, :], in_=ps[pi][:, :], func=AF.Exp), reads=[("ps", pi)], writes=[("pt", pti)])

        def pv_fn(ki=ki, st=st):
            pv(B_OW, wstate["first"], st["pti"], L.vw[:, ki, :], [("vw", ki), "vw_one"])
            wstate["first"] = False
        wpairs.append((s_fn, pv_fn))
    run_pairs(wpairs)
    ow = ps[B_OW][:, 0:260].rearrange("p (h c) -> p h c", c=65)
    P.op("dve", lambda e: e.reciprocal(out=sm[:, 12:16], in_=ow[:, :, 64]), reads=[("ps", B_OW)], writes=["sm_w"])
    P.op("dve", lambda e: e.tensor_tensor(out=sm[:, 12:16], in0=sm[:, 12:16], in1=gv[:, :, 2], op=ALU.mult), reads=["sm_w", gkey], writes=["sm_w"])
    for h in range(4):
        P.op("dve", lambda e, h=h: e.scalar_tensor_tensor(out=L.oacc[:, h, :], in0=ow[:, h, 0:64], scalar=sm[:, 12 + h:13 + h], in1=L.oacc[:, h, :],
                                                          op0=ALU.mult, op1=ALU.add),
             reads=[("ps", B_OW), "sm_w", "oacc"], writes=["oacc"])
    pi = C.next_ps()
    P.op("pe", lambda e, pi=pi: e.transpose(ps[pi][:, 0:128], L.nsel[:, :], T.ident_f[:, :]), reads=["nsel", "nsel_pad", "ident_f"], writes=[("ps", pi)])
    P.op("dve", lambda e, pi=pi: e.tensor_scalar(out=qT[64:128, tq, 0, :], in0=ps[pi][64:128, 0:128], scalar1=NEG, scalar2=None, op0=ALU.mult),
         reads=[("ps", pi)], writes=[qkey])
    for h in range(1, 4):
        P.op("pool", lambda e, h=h: e.tensor_copy(out=qT[64:128, tq, h, :], in_=qT[64:128, tq, 0, :]), reads=[qkey], writes=[qkey])
    sstate = {"far": True, "near": True}
    spairs = []
    for ki in range(qi + 1):
        dl = qi - ki
        near = dl <= 7
        st = {}

        def s_fn(ki=ki, dl=dl, near=near, st=st):
            pi = C.next_ps()
            P.op("pe", lambda e: e.matmul(ps[pi][:, :], lhsT=L.ksT[:, ki * 128:(ki + 1) * 128], rhs=q_rhs, start=True, stop=(not near)),
                 reads=[("ksT", ki // 4), "exall", qkey], writes=[("ps", pi)])
            if near:
                P.op("pe", lambda e: e.matmul(ps[pi][:, :], lhsT=T.ident_b[:, :], rhs=T.bsel[:, dl, :], start=False, stop=True),
                     reads=["ident_b", "bsel"], writes=[("ps", pi)])
            pti = next_pt()
            st["pti"] = pti
            P.op("act", lambda e: e.activation(out=L.pt[pti][:, :], in_=ps[pi][:, :], func=AF.Exp), reads=[("ps", pi)], writes=[("pt", pti)])

        def pv_fn(ki=ki, near=near, st=st):
            if near:
                pv(B_ON, sstate["near"], st["pti"], L.vs[:, ki, :], [("vs", ki), "vs_one"])
                sstate["near"] = False
            else:
                pv(B_OF, sstate["far"], st["pti"], L.vs[:, ki, :], [("vs", ki), "vs_one"])
                sstate["far"] = False
        spairs.append((s_fn, pv_fn))
    run_pairs(spairs)
    on = ps[B_ON][:, 0:260].rearrange("p (h c) -> p h c", c=65)
    of = ps[B_OF][:, 0:260].rearrange("p (h c) -> p h c", c=65)
    if not sstate["far"]:
        P.op("act", lambda e: e.activation(out=L.onsb[:, :, :], in_=on, func=AF.Copy), reads=[("ps", B_ON)], writes=["onsb"])
        for h in range(4):
            P.op("dve", lambda e, h=h: e.scalar_tensor_tensor(out=L.osel[:, h, :], in0=of[:, h, :], scalar=T.efar[:, h:h + 1], in1=L.onsb[:, h, :],
                                                              op0=ALU.mult, op1=ALU.add),
                 reads=[("ps", B_OF), "efar", "onsb"], writes=["osel"])
        osel, okeys = L.osel, ["osel"]
    else:
        osel, okeys = on, [("ps", B_ON)]
    P.op("dve", lambda e: e.reciprocal(out=sm[:, 8:12], in_=osel[:, :, 64]), reads=okeys, writes=["sm_s"])
    P.op("dve", lambda e: e.tensor_tensor(out=sm[:, 8:12], in0=sm[:, 8:12], in1=gv[:, :, 1], op=ALU.mult), reads=["sm_s", gkey], writes=["sm_s"])
    for h in range(4):
        P.op("dve", lambda e, h=h: e.scalar_tensor_tensor(out=L.oacc[:, h, :], in0=osel[:, h, 0:64], scalar=sm[:, 8 + h:9 + h], in1=L.oacc[:, h, :],
                                                          op0=ALU.mult, op1=ALU.add),
             reads=okeys + ["sm_s", "oacc"], writes=["oacc"])
    pi = C.next_ps()
    for c2 in range(2):
        P.op("pe", lambda e, pi=pi, c2=c2: e.transpose(ps[pi][:, c2 * 128:(c2 + 1) * 128],
                                                       L.oacc[:, 2 * c2:2 * c2 + 2, :].rearrange("p h d -> p (h d)"), T.ident_f[:, :]),
             reads=["oacc", "ident_f"], writes=[("ps", pi)])
    oaT = L.oaT[par]
    P.op("act", lambda e, pi=pi: e.activation(out=oaT[:, :, :], in_=ps[pi][:, 0:256].rearrange("p (c t) -> p c t", c=2), func=AF.Copy),
         reads=[("ps", pi)], writes=[("oaT", par)])
    ca = oT_d.get("oa_c0", 0)
    P.op("sp", lambda e: e.dma_start(out=oT_d["oT"][:, ca:ca + 2, t0:t0 + 128], in_=oaT[:, :, :]), reads=[("oaT", par)], writes=[("oTa", par)],
         dma_key=("oTa", par))


def emit_gla_tile(C, T, L, B, tq, oT_d):
    nc, P, ps = C.nc, C.P, C.ps
    qi = 4 * B + tq
    t0 = qi * 128
    par = qi % 2
    tc0 = tq * 128
    pz = C.next_ps()
    P.op("pe", lambda e: e.matmul(ps[pz][:, 0:256], lhsT=L.alrT[:, tc0:tc0 + 128], rhs=L.w2a[:, :], start=True, stop=False),
         reads=["alrT", "w2a"], writes=[("ps", pz)])
    P.op("pe", lambda e: e.matmul(ps[pz][:, 0:256], lhsT=T.ones_f[:, :], rhs=L.ba[:, :], start=False, stop=True),
         reads=["ones_f", "ba"], writes=[("ps", pz)])
    P.op("act", lambda e: e.activation(out=L.ez[:, :], in_=ps[pz][:, 0:256], func=AF.Exp, scale=-1.0), reads=[("ps", pz)], writes=["ez"])
    P.op("act", lambda e: e.activation(out=L.sp[:, :], in_=L.ez[:, :], func=AF.Ln, bias=T.one_col[:, 0:1]), reads=["ez", "one_col"], writes=["sp"])
    yield
    pr = C.next_ps()
    P.op("pe", lambda e: e.matmul(ps[pr][:, 0:256], lhsT=T.tri_u[:, :], rhs=L.sp[:, :], start=True, stop=True), reads=["tri_u", "sp"], writes=[("ps", pr)])
    P.op("act", lambda e: e.activation(out=L.erb[:, :], in_=ps[pr][:, 0:256], func=AF.Exp), reads=[("ps", pr)], writes=["erb"])
    P.op("dve", lambda e: e.tensor_tensor(out=L.kstate[:, :], in0=L.kbtok[par][:, :], in1=L.erb[:, :], op=ALU.mult),
         reads=[("kbtok", par), "erb"], writes=["kstate"])
    vb = L.vb[par]
    for h in range(2):
        eb, enb, qdec, kint, at = L.eb[h], L.enb[h], L.qdec[h], L.kint[h], L.at[h]
        pb = C.next_ps()
        P.op("pe", lambda e, h=h, pb=pb: e.matmul(ps[pb][:, 0:128], lhsT=L.sp[:, h * 128:(h + 1) * 128], rhs=T.tri_b[:, :], start=True, stop=True),
             reads=["sp", "tri_b"], writes=[("ps", pb)])
        P.op("act", lambda e, pb=pb, eb=eb: e.activation(out=eb[:, :], in_=ps[pb][:, 0:128], func=AF.Exp), reads=[("ps", pb)], writes=[("eb", h)])
        P.op("act", lambda e, pb=pb, enb=enb: e.activation(out=enb[:, :], in_=ps[pb][:, 0:128], func=AF.Exp, scale=-1.0), reads=[("ps", pb)],
             writes=[("enb", h)])
        P.op("dve", lambda e, h=h, eb=eb, qdec=qdec: e.scalar_tensor_tensor(out=qdec[:, :], in0=L.qbT[:, h, tc0:tc0 + 128], scalar=128.0 ** -0.5,
                                                                            in1=eb[:, :], op0=ALU.mult, op1=ALU.mult),
             reads=[("qbT", h), ("eb", h)], writes=[("qdec", h)])
        P.op("dve", lambda e, h=h, enb=enb, kint=kint: e.tensor_tensor(out=kint[:, :], in0=L.kbT[:, h, tc0:tc0 + 128], in1=enb[:, :], op=ALU.mult),
             reads=[("kbT", h), ("enb", h)], writes=[("kint", h)])
        yield
        pa = C.next_ps()
        P.op("pe", lambda e, pa=pa, kint=kint, qdec=qdec: e.matmul(ps[pa][:, 0:128], lhsT=kint[:, :], rhs=qdec[:, :], start=True, stop=True),
             reads=[("kint", h), ("qdec", h)], writes=[("ps", pa)])
        P.op("dve", lambda e, pa=pa, at=at: e.tensor_tensor(out=at[:, :], in0=ps[pa][:, 0:128], in1=T.amask[:, :], op=ALU.mult),
             reads=[("ps", pa), "amask"], writes=[("at", h)])
        yield
        po = C.next_ps()
        C.held = set([po])
        vh = vb[:, h * 256:(h + 1) * 256]
        P.op("pe", lambda e, po=po, at=at, vh=vh: e.matmul(ps[po][:, 0:256], lhsT=at[:, :], rhs=vh, start=True, stop=False),
             reads=[("at", h), ("vb", par)], writes=[("ps", po)])
        skey = "state%d" % h
        for ch in range(2):
            r0 = ch * 64
            yield
            si = L.cnt["sbf"][h] % 2
            sbf = L.sbf[h][si]
            P.op("pe", lambda e, po=po, r0=r0, qdec=qdec, sbf=sbf, ch=ch: e.matmul(ps[po][r0:r0 + 64, 0:256], lhsT=qdec[:, r0:r0 + 64], rhs=sbf[:, :],
                                                                                  start=False, stop=(ch == 1), skip_group_check=True),
                 reads=[("qdec", h), ("sbf", h, si)], writes=[("ps", po)])
            pu = C.next_ps()
            P.op("pe", lambda e, pu=pu, r0=r0, h=h, vh=vh: e.matmul(ps[pu][:, 0:256], lhsT=L.kstate[r0:r0 + 64, h * 128:(h + 1) * 128],
                                                                  rhs=vb[r0:r0 + 64, h * 256:(h + 1) * 256], start=True, stop=True),
                 reads=["kstate", ("vb", par)], writes=[("ps", pu)])
            dcol = eb[:, r0 + 63:r0 + 64]
            P.op("dve", lambda e, pu=pu, h=h, dcol=dcol: e.scalar_tensor_tensor(out=L.state[:, h, :], in0=L.state[:, h, :], scalar=dcol,
                                                                                in1=ps[pu][:, 0:256], op0=ALU.mult, op1=ALU.add),
                 reads=[skey, ("eb", h), ("ps", pu)], writes=[skey])
            L.cnt["sbf"][h] += 1
            sj = L.cnt["sbf"][h] % 2
            P.op("act", lambda e, h=h, sj=sj: e.activation(out=L.sbf[h][sj][:, :], in_=L.state[:, h, :], func=AF.Copy), reads=[skey],
                 writes=[("sbf", h, sj)])
        P.op("act", lambda e, po=po, h=h: e.activation(out=L.junk[:, :], in_=ps[po][:, 0:256], func=AF.Square, accum_out=L.ssq[:, h:h + 1]),
             reads=[("ps", po)], writes=["junk", ("ssq", h)])
        P.op("act", lambda e, h=h: e.activation(out=L.ssq[:, h:h + 1], in_=L.ssq[:, h:h + 1], func=AF.Ln, bias=T.eps_col[:, 0:1], scale=1.0 / 256.0),
             reads=[("ssq", h), "eps_col"], writes=[("ssq", h)])
        P.op("act", lambda e, h=h: e.activation(out=L.ssq[:, h:h + 1], in_=L.ssq[:, h:h + 1], func=AF.Exp, scale=-0.5), reads=[("ssq", h)], writes=[("ssq", h)])
        P.op("dve", lambda e, h=h, po=po: e.scalar_tensor_tensor(out=L.ob[:, h * 256:(h + 1) * 256], in0=ps[po][:, 0:256], scalar=L.ssq[:, h:h + 1],
                                                                 in1=L.gsr[par][:, h * 256:(h + 1) * 256], op0=ALU.mult, op1=ALU.mult),
             reads=[("ps", po), ("ssq", h), ("gsr", par)], writes=[("ob", h)])
        C.held = set()
    yield
    pt_ = C.next_ps()
    for c4 in range(4):
        P.op("pe", lambda e, c4=c4: e.transpose(ps[pt_][:, c4 * 128:(c4 + 1) * 128], L.ob[:, c4 * 128:(c4 + 1) * 128], T.ident_f[:, :]),
             reads=[("ob", c4 // 2), "ident_f"], writes=[("ps", pt_)])
    obT = L.obT[par]
    P.op("act", lambda e: e.activation(out=obT[:, :, :], in_=ps[pt_][:, :].rearrange("p (c t) -> p c t", c=4), func=AF.Copy),
         reads=[("ps", pt_)], writes=[("obT", par)])
    cb = oT_d.get("ob_c0", 2)
    P.op("sp", lambda e: e.dma_start(out=oT_d["oT"][:, cb:cb + 4, t0:t0 + 128], in_=obT[:, :, :]), reads=[("obT", par)], writes=[("oTb", par)],
         dma_key=("oTb", par))


def build_mixer_program(nlayers=1, debug=False):
    nc = bass.Bass("TRN2", target_bir_lowering=False)
    cin = {}
    for k, shp in MIX_CONST_SHAPES.items():
        cin[k] = dram_w(nc, "c_" + k, shp)
    din = {}
    for k, shp in MIX_W_SHAPES.items():
        din[k] = dram_w(nc, k, shp)
    cin["rel"] = din["rel"]
    hT_d = dram_w(nc, "hT", [128, KD, S], BF16)
    oT = nc.dram_tensor("oT", [128, 6, S], BF16, kind="ExternalOutput").ap()
    cin["fc"] = nc.dram_tensor("fc_scr", [4, 128, LC], BF16, kind="Internal").ap()
    cin["fw"] = nc.dram_tensor("fw_scr", [4, 128, LW], BF16, kind="Internal").ap()
    od = {"oT": oT, "fc": cin["fc"]}
    with ExitStack() as stack:
        C = Ctx(nc, stack)
        C.psum_banks()
        C.gen_banks = [0, 1, 2]
        T = mixer_setup(C, cin, stack)
        L = alloc_mixer_layer_tiles(C, stack)
        emit_mixer_layer(C, T, L, din, hT_d, od)
        fin = [("oTa", 0), ("oTa", 1), ("oTb", 0), ("oTb", 1)]
        if debug:
            dl = [("ksT", L.ksT, [("ksT", b) for b in range(NBLK)]), ("kwT", L.kwT, [("kwT", b) for b in range(NBLK)]),
                  ("kcrT", L.kcrT, [("kcrT", b) for b in range(NBLK)] + [("kcrTu", b) for b in range(NBLK)]),
                  ("vs", L.vs, [("vs", q) for q in range(NQT)]), ("kcT", T.kcT, ["kcT"]), ("voc", T.voc, ["voc"]),
                  ("qT", L.qT[1], [("qT", 1)]), ("gates", L.gates[1], [("gates", 1)]), ("bsel", T.bsel, ["bsel"]), ("bwin", T.bwin, ["bwin"]),
                  ("efar", T.efar, ["efar"]), ("state", L.state, ["state0", "state1"]), ("imp", L.imp, ["imp"]), ("nsel", L.nsel, ["nsel"]),
                  ("oacc", L.oacc, ["oacc"]), ("sp", L.sp, ["sp"]), ("ob", L.ob, [("ob", 0), ("ob", 1)]), ("bcmp", L.bcmp[1], [("bcmp", 1, 0), ("bcmp", 1, 1)]),
                  ("kbtok", L.kbtok[1], [("kbtok", 1)]), ("vb", L.vb[1], [("vb", 1)]), ("pbk", L.pbk, ["pbk"])]
            for nm, t, keys in dl:
                shp = list(t.shape)
                o = nc.dram_tensor("dbg_" + nm, shp, t.dtype, kind="ExternalOutput").ap()
                idx = tuple(slice(None) for _ in shp)
                C.P.op("sp", lambda e, o=o, t=t, idx=idx: e.dma_start(out=o[idx], in_=t[idx]), reads=keys, writes=[("dbg", nm)], dma_key=("dbg", nm))
                fin.append(("dbg", nm))
        C.P.wait_all("sp", fin)
        C.P.emit(stack)
    return nc


def alloc_merge_tiles(C, stack):
    sb = lambda n, s, d: C.sb(n, s, d, stack)
    M = MixerTiles()
    M.wgm = sb("wgm", [128, KD, 2048], BF16)
    M.wbn = sb("wbn", [128, 4, D], BF16)
    M.wbg = sb("wbg", [128, 8, D], BF16)
    M.wo = sb("wo", [128, KD, D], BF16)
    M.oT = [sb("oTin%d" % i, [128, 12, TT], BF16) for i in range(2)]
    M.hT = sb("mhT", [128, KD, TT], BF16)
    M.yT = sb("yT", [128, KD, TT], BF16)
    M.sg = [sb("msg%d" % i, [128, TT], F32) for i in range(2)]
    M.t1 = sb("mt1", [128, TT], F32)
    M.t2 = sb("mt2", [128, TT], F32)
    return M


def emit_merge(C, xT, M, gmix_col, d, oT_d, tiles, oT_loader=None, pre_compute=None):
    P, ps = C.P, C.ps
    P.op("pool", lambda e: e.dma_start(out=M.wgm[:, :, :], in_=d["wgm"].rearrange("(k p) c -> p k c", p=128)), writes=["wgm"], dma_key="wgm")
    P.op("pool", lambda e: e.dma_start(out=M.wbn[:, :, :], in_=d["wbn"].rearrange("(k p) c -> p k c", p=128)), writes=["wbn"], dma_key="wbn")
    P.op("pool", lambda e: e.dma_start(out=M.wbg[:, :, :], in_=d["wbg"].rearrange("(k p) c -> p k c", p=128)), writes=["wbg"], dma_key="wbg")
    P.op("pool", lambda e: e.dma_start(out=M.wo[:, :, :], in_=d["wo"].rearrange("(k p) c -> p k c", p=128)), writes=["wo"], dma_key="wo")
    if pre_compute is not None:
        pre_compute()
    for it in range(NTOK // TT):
        t0 = it * TT
        ts = t0 // TS
        oT = M.oT[it % 2]
        okey = ("oTin", it % 2)
        if oT_loader is None:
            P.op("sp", lambda e, oT=oT, t0=t0: e.dma_start(out=oT[:, :, :], in_=oT_d[:, :, t0:t0 + TT]), writes=[okey], dma_key=okey)
        else:
            oT_loader(it, oT, okey)
        emit_rmsnorm_tile(C, xT, t0, gmix_col, M.hT, "mhT", tiles["ones"], tiles, [("xT", ts)], gkey="gmixp")
        for dc in range(KD):
            pg0, pg1, pa, pb = C.next_ps(), C.next_ps(), C.next_ps(), C.next_ps()
            for (pi, c0) in ((pg0, dc * 128), (pg1, 1024 + dc * 128)):
                for k in range(KD):
                    P.op("pe", lambda e, pi=pi, c0=c0, k=k: e.matmul(ps[pi][:, :], lhsT=M.wgm[:, k, c0:c0 + 128], rhs=M.hT[:, k, :],
                                                                    start=(k == 0), stop=(k == KD - 1)),
                         reads=["wgm", "mhT"], writes=[("ps", pi)])
            for k in range(4):
                P.op("pe", lambda e, k=k, pa=pa, dc=dc, oT=oT: e.matmul(ps[pa][:, :], lhsT=M.wbn[:, k, dc * 128:(dc + 1) * 128], rhs=oT[:, k, :],
                                                                       start=(k == 0), stop=(k == 3)),
                     reads=["wbn", okey], writes=[("ps", pa)])
            for k in range(8):
                P.op("pe", lambda e, k=k, pb=pb, dc=dc, oT=oT: e.matmul(ps[pb][:, :], lhsT=M.wbg[:, k, dc * 128:(dc + 1) * 128], rhs=oT[:, 4 + k, :],
                                                                       start=(k == 0), stop=(k == 7)),
                     reads=["wbg", okey], writes=[("ps", pb)])
            P.op("act", lambda e, pg0=pg0: e.activation(out=M.sg[0][:, :], in_=ps[pg0][:, :], func=AF.Sigmoid), reads=[("ps", pg0)], writes=[("msg", 0)])
            P.op("act", lambda e, pg1=pg1: e.activation(out=M.sg[1][:, :], in_=ps[pg1][:, :], func=AF.Sigmoid), reads=[("ps", pg1)], writes=[("msg", 1)])
            P.op("dve", lambda e, pa=pa: e.tensor_tensor(out=M.t1[:, :], in0=ps[pa][:, :], in1=M.sg[0][:, :], op=ALU.mult),
                 reads=[("ps", pa), ("msg", 0)], writes=["mt1"])
            P.op("dve", lambda e, pb=pb: e.tensor_tensor(out=M.t2[:, :], in0=ps[pb][:, :], in1=M.sg[1][:, :], op=ALU.mult),
                 reads=[("ps", pb), ("msg", 1)], writes=["mt2"])
            P.op("pool", lambda e, dc=dc: e.tensor_tensor(out=M.yT[:, dc, :], in0=M.t1[:, :], in1=M.t2[:, :], op=ALU.add),
                 reads=["mt1", "mt2"], writes=[("yT", dc)])
        for dc in range(KD):
            po = C.next_ps()
            for k in range(KD):
                P.op("pe", lambda e, k=k, po=po, dc=dc: e.matmul(ps[po][:, :], lhsT=M.wo[:, k, dc * 128:(dc + 1) * 128], rhs=M.yT[:, k, :],
                                                                start=(k == 0), stop=(k == KD - 1)),
                     reads=["wo", ("yT", k)], writes=[("ps", po)])
            P.op("dve", lambda e, po=po, dc=dc, t0=t0: e.tensor_tensor(out=xT[:, dc, t0:t0 + TT], in0=ps[po][:, :], in1=xT[:, dc, t0:t0 + TT], op=ALU.add),
                 reads=[("ps", po), ("xT", ts)], writes=[("xT", ts)])


def emit_h_out(C, xT, g_col, gkey, tiles, hT_out, final=False):
    P = C.P
    for it in range(NTOK // TT):
        t0 = it * TT
        ts = t0 // TS
        if final:
            ht = tiles["hfin"][it % 2]
            key = ("hfin", it % 2)
        else:
            ht = tiles["hout"][it % 2]
            key = ("hout", it % 2)
        emit_rmsnorm_tile(C, xT, t0, g_col, ht, key, tiles["ones"], tiles, [("xT", ts)], gkey=gkey)
        P.op("sp", lambda e, ht=ht, t0=t0: e.dma_start(out=hT_out[:, :, t0:t0 + TT], in_=ht[:, :, :]), reads=[key], writes=[("hst", it % 2)],
             dma_key=("hst", it % 2))


def build_tok_program(first, last):
    nc = bass.Bass("TRN2", target_bir_lowering=False)
    x_in = dram_w(nc, "xT_in", [128, KD, NTOK])
    gains = dram_w(nc, "gains", [128, 5, KD])
    dr = {}
    if not first:
        oT_in = dram_w(nc, "oT_in", [128, 12, NTOK], BF16)
        for k, shp in (("wgm", [D, 2048]), ("wbn", [512, D]), ("wbg", [1024, D]), ("wo", [D, D]), ("wg2", [D, DFF]), ("wu2", [D, DFF]), ("wd2", [DFF, D])):
            dr[k] = dram_w(nc, k, shp)
    if not last:
        for k, shp in (("wg1", [D, DFF]), ("wu1", [D, DFF]), ("wd1", [DFF, D])):
            dr[k] = dram_w(nc, k, shp)
        x_out = nc.dram_tensor("xT_out", [128, KD, NTOK], F32, kind="ExternalOutput").ap()
        h_out = nc.dram_tensor("hT_out", [128, KD, NTOK], BF16, kind="ExternalOutput").ap()
    else:
        y_out = nc.dram_tensor("y_out", [128, KD, NTOK], F32, kind="ExternalOutput").ap()
    with ExitStack() as stack:
        C = Ctx(nc, stack)
        C.psum_banks()
        P = C.P
        xT = C.sb("xT", [128, KD, NTOK], F32)
        gt = C.sb("gains", [128, 5, KD], F32)
        ones = C.sb("ones", [128, 128], BF16)
        epsc = C.sb("epsc", [128, 1], F32)
        P.op("pool", lambda e: e.memset(ones[:, :], 1.0), writes=["ones"])
        P.op("pool", lambda e: e.memset(epsc[:, :], EPS), writes=["epsc"])
        for ts in range(NTOK // TS):
            P.op("sp", lambda e, ts=ts: e.dma_start(out=xT[:, :, ts * TS:(ts + 1) * TS], in_=x_in[:, :, ts * TS:(ts + 1) * TS]),
                 writes=[("xT", ts)], dma_key=("xT", ts))
        P.op("sp", lambda e: e.dma_start(out=gt[:, :, :], in_=gains[:, :, :]), writes=["gains"], dma_key="gains")
        fin = []
        base = {"ones": ones, "epsc": epsc, "sq": C.sb("sq", [128, KD, TT], BF16), "rstd": C.sb("rstd", [128, TT], F32)}
        if not first:
            with ExitStack() as s3:
                M = alloc_merge_tiles(C, s3)
                emit_merge(C, xT, M, gt[:, 0, :], dr, oT_in, base)
                P.barrier()
        with ExitStack() as s2:
            tiles = alloc_ffn_tiles(C, s2)
            tiles.update(base)
            if not first:
                emit_ffn(C, xT, gt[:, 1, :], dr["wg2"], dr["wu2"], dr["wd2"], tiles, "ffn2")
            if not last:
                emit_ffn(C, xT, gt[:, 2, :], dr["wg1"], dr["wu1"], dr["wd1"], tiles, "ffn1")
            P.barrier()
        with ExitStack() as s4:
            tiles = dict(base)
            if not last:
                tiles["hout"] = [C.sb("hout%d" % i, [128, KD, TT], BF16, s4) for i in range(2)]
                emit_h_out(C, xT, gt[:, 3, :], "gains", tiles, h_out)
                for ts in range(NTOK // TS):
                    P.op("sp", lambda e, ts=ts: e.dma_start(out=x_out[:, :, ts * TS:(ts + 1) * TS], in_=xT[:, :, ts * TS:(ts + 1) * TS]),
                         reads=[("xT", ts)], writes=[("xst", ts)], dma_key=("xst", ts))
                    fin.append(("xst", ts))
                fin += [("hst", 0), ("hst", 1)]
            else:
                tiles["hfin"] = [C.sb("hfin%d" % i, [128, KD, TT], F32, s4) for i in range(2)]
                emit_h_out(C, xT, gt[:, 4, :], "gains", tiles, y_out, final=True)
                fin += [("hst", 0), ("hst", 1)]
            P.wait_all("sp", fin)
            P.emit(stack)
    return nc


_PROGS = {}


def _prog(name, fn):
    if name not in _PROGS:
        _PROGS[name] = fn()
    return _PROGS[name]


def _gains(inp, l, first, last):
    g = np.zeros((128, 5, KD), np.float32)
    if not first:
        g[:, 0] = gain_cols(inp["mix_norm"][l])
        g[:, 1] = gain_cols(inp["ffn2_norm"][l])
    if not last:
        ln = 0 if first else l + 1
        g[:, 2] = gain_cols(inp["ffn1_norm"][ln])
        g[:, 3] = gain_cols(inp["mix_norm"][ln])
    g[:, 4] = gain_cols(inp["final_norm"])
    return g


def kernel_unfused(**inp):
    inp = {k: np.asarray(v) for k, v in inp.items()}
    x = inp["x"]
    cores = list(range(8))
    nc = _prog("tok_first", lambda: build_tok_program(True, False))
    maps = []
    for c in cores:
        b, h = c // 2, c % 2
        maps.append({"xT_in": to_feature_major(x[b, h * NTOK:(h + 1) * NTOK]), "gains": _gains(inp, 0, True, False),
                     "wg1": inp["ffn1_w_gate"][0], "wu1": inp["ffn1_w_up"][0], "wd1": inp["ffn1_w_down"][0]})
    res = run_bass_kernel_spmd(nc, maps, core_ids=cores).results
    xT = [r["xT_out"] for r in res]
    hT = [r["hT_out"] for r in res]
    consts = {"c_" + k: v for k, v in mixer_consts().items()}
    for l in range(DEPTH):
        ncm = _prog("mixer", lambda: build_mixer_program())
        maps = []
        for c in cores:
            b, g = c // 2, c % 2
            m = dict(consts)
            m.update(mixer_weight_inputs(inp, l, g))
            m["hT"] = np.ascontiguousarray(np.concatenate([hT[2 * b], hT[2 * b + 1]], axis=2))
            maps.append(m)
        res = run_bass_kernel_spmd(ncm, maps, core_ids=cores).results
        oT = [r["oT"] for r in res]
        last = (l == DEPTH - 1)
        nct = _prog("tok_last" if last else "tok_mid", lambda: build_tok_program(False, last))
        maps = []
        for c in cores:
            b, h = c // 2, c % 2
            sl = slice(h * NTOK, (h + 1) * NTOK)
            o0, o1 = oT[2 * b], oT[2 * b + 1]
            m = {"xT_in": xT[c], "gains": _gains(inp, l, False, last),
                 "oT_in": np.ascontiguousarray(np.concatenate([o0[:, 0:2, sl], o1[:, 0:2, sl], o0[:, 2:6, sl], o1[:, 2:6, sl]], axis=1)),
                 "wgm": np.ascontiguousarray(inp["w_in"][l][:, OFF_GM:OFF_GM + 2048]),
                 "wbn": inp["w_branch_nsa"][l], "wbg": inp["w_branch_gla"][l], "wo": inp["w_out"][l],
                 "wg2": inp["ffn2_w_gate"][l], "wu2": inp["ffn2_w_up"][l], "wd2": inp["ffn2_w_down"][l]}
            if not last:
                m.update({"wg1": inp["ffn1_w_gate"][l + 1], "wu1": inp["ffn1_w_up"][l + 1], "wd1": inp["ffn1_w_down"][l + 1]})
            maps.append(m)
        res = run_bass_kernel_spmd(nct, maps, core_ids=cores).results
        if not last:
            xT = [r["xT_out"] for r in res]
            hT = [r["hT_out"] for r in res]
    out = np.zeros((NB, S, D), np.float32)
    for c in cores:
        b, h = c // 2, c % 2
        out[b, h * NTOK:(h + 1) * NTOK] = from_feature_major(res[c]["y_out"])
    return out


def kernel(**inputs):
    return kernel_fused8(**inputs)


def mixer_build_tables(C, cin, rel_d, fc, fw, efar_d, g, stack):
    P = C.P
    sb = lambda n, s, d: C.sb(n + "_g%d" % g, s, d, stack)
    relx = sb("relx", [33, 4], F32)
    ones33 = sb("ones33", [33, 128], F32)
    relb = sb("relb", [33, 4, 128], F32)
    ohb = sb("ohb", [33, 512], F32)
    oh31 = sb("oh31", [33, 128], F32)
    efar = sb("efar_t", [128, 4], F32)
    fsb = [sb("fsb%d" % i, [128, 512], BF16) for i in range(2)]
    k = lambda name: (name, g)
    P.op("sp", lambda e: e.dma_start(out=relx[0:32, :], in_=rel_d[:, :]), writes=[k("relx")], dma_key=k("relx"))
    P.op("pool", lambda e: e.memset(relx[32:33, :], NEG), writes=[k("relx32")])
    P.op("pool", lambda e: e.memset(ones33[:, :], 1.0), writes=[k("ones33")])
    P.op("sp", lambda e: e.dma_start(out=oh31[:, :], in_=cin["oh31"][:, :]), writes=[k("oh31")], dma_key=k("oh31"))
    pi = C.next_ps()
    P.op("pe", lambda e: e.matmul(C.ps[pi][:, 0:4], lhsT=oh31[:, :], rhs=relx[:, :], start=True, stop=True),
         reads=[k("oh31"), k("relx"), k("relx32")], writes=[("ps", pi)])
    P.op("act", lambda e: e.activation(out=efar[:, :], in_=C.ps[pi][:, 0:4], func=AF.Exp), reads=[("ps", pi)], writes=[k("efar_t")])
    P.op("sp", lambda e: e.dma_start(out=efar_d[:, :], in_=efar[:, :]), reads=[k("efar_t")], writes=[k("efar_d")], dma_key=k("efar_d"))
    for h in range(4):
        P.op("dve", lambda e, h=h: e.tensor_scalar(out=relb[:, h, :], in0=ones33[:, :], scalar1=relx[:, h:h + 1], scalar2=None, op0=ALU.mult),
             reads=[k("ones33"), k("relx"), k("relx32")], writes=[k("relb")])
    it = 0
    for tab, src_oh, L_, scr in (("c", cin["oh_c"], LC, fc), ("w", cin["oh_w"], LW, fw)):
        for blk in range(L_ // 512):
            P.op("sp", lambda e, blk=blk, src_oh=src_oh: e.dma_start(out=ohb[:, :], in_=src_oh[:, blk * 512:(blk + 1) * 512]),
                 writes=[k("ohb")], dma_key=k("ohb"))
            for h in range(4):
                pi = C.next_ps()
                par = it % 2
                fb = fsb[par]
                fkey = ("fsb", g, par)
                it += 1
                P.op("pe", lambda e, pi=pi, h=h: e.matmul(C.ps[pi][:, :], lhsT=relb[:, h, :], rhs=ohb[:, :], start=True, stop=True),
                     reads=[k("relb"), k("ohb")], writes=[("ps", pi)])
                P.op("act", lambda e, pi=pi, fb=fb: e.activation(out=fb[:, :], in_=C.ps[pi][:, :], func=AF.Copy),
                     reads=[("ps", pi)], writes=[fkey])
                P.op("sp", lambda e, h=h, scr=scr, fb=fb, blk=blk: e.dma_start(out=scr[h, :, blk * 512:(blk + 1) * 512], in_=fb[:, :]),
                     reads=[fkey], writes=[("F", g, tab, par)], dma_key=("Fst", g, par))


def mixer_alloc_consts(C, cin, fc, fw, efar_d, g, stack):
    P = C.P
    T = MixerTiles()
    sb = lambda n, s, d: C.sb(n, s, d, stack)
    T.ident_f = sb("ident_f", [128, 128], F32)
    T.ident_b = sb("ident_b", [128, 128], BF16)
    T.exall_d = cin["exall"]
    T.tri_b = sb("tri_b", [128, 128], F32)
    T.tri_u = sb("tri_u", [128, 128], F32)
    T.amask = sb("amask", [128, 128], F32)
    T.aadd = [sb("aadd%d" % i, [128, 64], F32) for i in range(2)]
    T.aadd_d = cin["aadd"]
    T.one_col = sb("one_col", [128, 1], F32)
    T.eps_col = sb("eps_col", [128, 1], F32)
    T.tiny_col = sb("tiny_col", [128, 1], F32)
    T.ones_f = sb("ones_f", [1, 128], F32)
    T.efar = sb("efar", [128, 4], F32)
    T.bsel = sb("bsel", [128, 8, 512], BF16)
    T.bwin = sb("bwin", [128, 5, 512], BF16)
    T.voc = sb("voc", [128, 2, 129], BF16)
    T.kcT = sb("kcT", [128, 256], BF16)
    P.op("pool", lambda e: e.memset(T.ident_f[:, :], 0.0), writes=["ident_f"])
    P.op("pool", lambda e: e.affine_select(out=T.ident_f[:, :], in_=T.ident_f[:, :], pattern=[[-1, 128]], compare_op=ALU.not_equal,
                                           fill=1.0, base=0, channel_multiplier=1), reads=["ident_f"], writes=["ident_f"])
    P.op("pool", lambda e: e.tensor_copy(out=T.ident_b[:, :], in_=T.ident_f[:, :]), reads=["ident_f"], writes=["ident_b"])
    P.op("pool", lambda e: e.memset(T.one_col[:, :], 1.0), writes=["one_col"])
    P.op("pool", lambda e: e.memset(T.eps_col[:, :], EPS), writes=["eps_col"])
    P.op("pool", lambda e: e.memset(T.tiny_col[:, :], 1e-30), writes=["tiny_col"])
    P.op("pool", lambda e: e.memset(T.ones_f[:, :], 1.0), writes=["ones_f"])
    P.op("pool", lambda e: e.memset(T.voc[:, :, :], 0.0), writes=["voc"])
    P.op("pool", lambda e: e.memset(T.kcT[:, :], 0.0), writes=["kcT"])
    P.op("pool", lambda e: e.memset(T.voc[:, :, 64:65], 1.0), reads=["voc"], writes=["voc"])
    P.op("pool", lambda e: e.memset(T.voc[0:1, 0, 64:65], 0.0), reads=["voc"], writes=["voc"])
    P.op("pool", lambda e: e.dma_start(out=T.voc[:, :, 65:129], in_=cin["ov"][:, :, :]), reads=["voc"], writes=["voc"], dma_key="voc_ov")
    for nm in ("tri_b", "tri_u", "amask"):
        t = getattr(T, nm)
        P.op("sp", lambda e, t=t, nm=nm: e.dma_start(out=t[:, :], in_=cin[nm][:, :]), writes=[nm], dma_key=nm)
    P.op("sp", lambda e: e.dma_start(out=T.efar[:, :], in_=efar_d[:, :]), reads=[("efar_d", g)], writes=["efar"], dma_key="efar")
    fk = lambda tab: [("F", g, tab, 0), ("F", g, tab, 1)]
    for dl in range(8):
        src = bass.AP(fc.tensor, fc.offset + OFFD + 128 * dl, [[LC - 1, 128], [128 * LC, 4], [1, 128]])
        P.op("sp", lambda e, dl=dl, src=src: e.dma_start(out=T.bsel[:, dl, :].rearrange("p (h t) -> p h t", h=4), in_=src),
             reads=fk("c"), writes=["bsel"], dma_key="bsel")
    for dl in range(5):
        src = bass.AP(fw.tensor, fw.offset + OFFW + 128 * dl, [[LW - 1, 128], [128 * LW, 4], [1, 128]])
        P.op("sp", lambda e, dl=dl, src=src: e.dma_start(out=T.bwin[:, dl, :].rearrange("p (h t) -> p h t", h=4), in_=src),
             reads=fk("w"), writes=["bwin"], dma_key="bwin")
    return T


def emit_tok_phase(C, first, last, base, gt, gi, dr, x_src, x_dst, oT_src, h_dst, y_dst, oT_loader=None, pre_compute=None):
    P = C.P
    with ExitStack() as sx:
        xT = C.sb("xT", [128, KD, NTOK], F32, sx)
        for ts in range(NTOK // TS):
            P.op("sp", lambda e, ts=ts: e.dma_start(out=xT[:, :, ts * TS:(ts + 1) * TS], in_=x_src[:, :, ts * TS:(ts + 1) * TS]),
                 writes=[("xT", ts)], dma_key=("xT", ts))
        if not first:
            with ExitStack() as s3:
                M = alloc_merge_tiles(C, s3)
                ldr = oT_loader(C, s3) if oT_loader is not None else None
                emit_merge(C, xT, M, gt[:, gi["mixp"], :], dr, oT_src, base, oT_loader=ldr, pre_compute=pre_compute)
                P.barrier()
        with ExitStack() as s2:
            tiles = alloc_ffn_tiles(C, s2)
            tiles.update(base)
            if not first:
                emit_ffn(C, xT, gt[:, gi["ffn2"], :], dr["wg2"], dr["wu2"], dr["wd2"], tiles, "ffn2")
            if not last:
                emit_ffn(C, xT, gt[:, gi["ffn1"], :], dr["wg1"], dr["wu1"], dr["wd1"], tiles, "ffn1")
            P.barrier()
        with ExitStack() as s4:
            tiles = dict(base)
            if not last:
                tiles["hout"] = [C.sb("hout%d" % i, [128, KD, TT], BF16, s4) for i in range(2)]
                emit_h_out(C, xT, gt[:, gi["mixn"], :], "gains", tiles, h_dst)
                for ts in range(NTOK // TS):
                    P.op("sp", lambda e, ts=ts: e.dma_start(out=x_dst[:, :, ts * TS:(ts + 1) * TS], in_=xT[:, :, ts * TS:(ts + 1) * TS]),
                         reads=[("xT", ts)], writes=[("xst", ts)], dma_key=("xst", ts))
            else:
                tiles["hfin"] = [C.sb("hfin%d" % i, [128, KD, TT], F32, s4) for i in range(2)]
                emit_h_out(C, xT, gt[:, gi["final"], :], "gains", tiles, y_dst, final=True)
            P.barrier()


FUSED_NCORES = 4


def build_fused_program(depth=DEPTH):
    nc = bass.Bass("TRN2", target_bir_lowering=False)
    x_in = dram_w(nc, "xT_in", [128, KD, S])
    gains = dram_w(nc, "gains", [128, 3 * depth + 1, KD])
    cin = {k: dram_w(nc, "c_" + k, shp) for k, shp in MIX_CONST_SHAPES.items()}
    rel_d = [dram_w(nc, "rel_g%d" % g, [32, 4]) for g in range(2)]
    LW_ = []
    for l in range(depth):
        d = {}
        for k, shp in (("wg1", [D, DFF]), ("wu1", [D, DFF]), ("wd1", [DFF, D]), ("wgm", [D, 2048]), ("wbn", [512, D]), ("wbg", [1024, D]),
                       ("wo", [D, D]), ("wg2", [D, DFF]), ("wu2", [D, DFF]), ("wd2", [DFF, D])):
            d[k] = dram_w(nc, "%s_l%d" % (k, l), shp)
        d["mix"] = []
        for g in range(2):
            dm = {}
            for k, shp in MIX_W_SHAPES.items():
                if k == "rel":
                    continue
                dm[k] = dram_w(nc, "%s_l%d_g%d" % (k, l, g), shp)
            d["mix"].append(dm)
        LW_.append(d)
    y_out = nc.dram_tensor("y_out", [128, KD, S], F32, kind="ExternalOutput").ap()
    xs = nc.dram_tensor("xs_scr", [128, KD, S], F32, kind="Internal").ap()
    hTs = nc.dram_tensor("hT_scr", [128, KD, S], BF16, kind="Internal").ap()
    oTs = nc.dram_tensor("oT_scr", [128, 12, S], BF16, kind="Internal").ap()
    fc = [nc.dram_tensor("fc_scr%d" % g, [4, 128, LC], BF16, kind="Internal").ap() for g in range(2)]
    fw = [nc.dram_tensor("fw_scr%d" % g, [4, 128, LW], BF16, kind="Internal").ap() for g in range(2)]
    efd = [nc.dram_tensor("efar_scr%d" % g, [128, 4], F32, kind="Internal").ap() for g in range(2)]
    with ExitStack() as stack:
        C = Ctx(nc, stack)
        C.psum_banks()
        P = C.P
        gt = C.sb("gains", [128, 3 * depth + 1, KD], F32)
        ones = C.sb("ones", [128, 128], BF16)
        epsc = C.sb("epsc", [128, 1], F32)
        base = {"ones": ones, "epsc": epsc, "sq": C.sb("sq", [128, KD, TT], BF16), "rstd": C.sb("rstd", [128, TT], F32)}
        P.op("pool", lambda e: e.memset(ones[:, :], 1.0), writes=["ones"])
        P.op("pool", lambda e: e.memset(epsc[:, :], EPS), writes=["epsc"])
        P.op("sp", lambda e: e.dma_start(out=gt[:, :, :], in_=gains[:, :, :]), writes=["gains"], dma_key="gains")
        C.gen_banks = [0, 1, 2]
        with ExitStack() as st:
            for g in range(2):
                mixer_build_tables(C, cin, rel_d[g], fc[g], fw[g], efd[g], g, st)
            P.barrier()
        C.gen_banks = list(range(8))
        for h in range(2):
            sl = slice(h * NTOK, (h + 1) * NTOK)
            gi = {"ffn1": 0, "mixn": 1}
            emit_tok_phase(C, True, False, base, gt, gi, {"wg1": LW_[0]["wg1"], "wu1": LW_[0]["wu1"], "wd1": LW_[0]["wd1"]},
                           x_in[:, :, sl], xs[:, :, sl], None, hTs[:, :, sl], None)
        for l in range(depth):
            C.gen_banks = [0, 1, 2]
            for g in range(2):
                with ExitStack() as sm:
                    T = mixer_alloc_consts(C, cin, fc[g], fw[g], efd[g], g, sm)
                    L = alloc_mixer_layer_tiles(C, sm)
                    od = {"oT": oTs, "fc": fc[g], "fkeys": [("F", g, "c", 0), ("F", g, "c", 1)], "oa_c0": 2 * g, "ob_c0": 4 + 4 * g}
                    emit_mixer_layer(C, T, L, LW_[l]["mix"][g], hTs, od)
                    P.barrier()
            C.gen_banks = list(range(8))
            last = (l == depth - 1)
            for h in range(2):
                sl = slice(h * NTOK, (h + 1) * NTOK)
                gi = {"mixp": 3 * l + 1, "ffn2": 3 * l + 2, "ffn1": 3 * (l + 1), "mixn": 3 * (l + 1) + 1, "final": 3 * depth}
                dr = {k: LW_[l][k] for k in ("wgm", "wbn", "wbg", "wo", "wg2", "wu2", "wd2")}
                if not last:
                    dr.update({k: LW_[l + 1][k] for k in ("wg1", "wu1", "wd1")})
                emit_tok_phase(C, False, last, base, gt, gi, dr, xs[:, :, sl], xs[:, :, sl], oTs[:, :, sl], hTs[:, :, sl], y_out[:, :, sl])
        P.wait_all("sp", [("hst", 0), ("hst", 1)])
        global _last_prog
        _last_prog = P
        P.emit(stack)
    return nc


def fused_inputs(inp, b, depth=DEPTH):
    m = {"xT_in": to_feature_major(inp["x"][b])}
    g = np.zeros((128, 3 * depth + 1, KD), np.float32)
    for l in range(depth):
        g[:, 3 * l] = gain_cols(inp["ffn1_norm"][l])
        g[:, 3 * l + 1] = gain_cols(inp["mix_norm"][l])
        g[:, 3 * l + 2] = gain_cols(inp["ffn2_norm"][l])
    g[:, 3 * depth] = gain_cols(inp["final_norm"])
    m["gains"] = g
    for k, v in mixer_consts().items():
        m["c_" + k] = v
    for gg in range(2):
        m["rel_g%d" % gg] = np.ascontiguousarray(inp["rel_table"][:, gg * 4:(gg + 1) * 4])
    for l in range(depth):
        m["wg1_l%d" % l] = inp["ffn1_w_gate"][l]
        m["wu1_l%d" % l] = inp["ffn1_w_up"][l]
        m["wd1_l%d" % l] = inp["ffn1_w_down"][l]
        m["wg2_l%d" % l] = inp["ffn2_w_gate"][l]
        m["wu2_l%d" % l] = inp["ffn2_w_up"][l]
        m["wd2_l%d" % l] = inp["ffn2_w_down"][l]
        m["wgm_l%d" % l] = np.ascontiguousarray(inp["w_in"][l][:, OFF_GM:OFF_GM + 2048])
        m["wbn_l%d" % l] = inp["w_branch_nsa"][l]
        m["wbg_l%d" % l] = inp["w_branch_gla"][l]
        m["wo_l%d" % l] = inp["w_out"][l]
        for gg in range(2):
            for k, v in mixer_weight_inputs(inp, l, gg).items():
                if k == "rel":
                    continue
                m["%s_l%d_g%d" % (k, l, gg)] = v
    return m


def kernel_fused(**inp):
    inp = {k: np.asarray(v) for k, v in inp.items()}
    nc = _prog("fused", lambda: build_fused_program())
    maps = [fused_inputs(inp, b) for b in range(NB)]
    res = run_bass_kernel_spmd(nc, maps, core_ids=list(range(NB))).results
    out = np.zeros((NB, S, D), np.float32)
    for b in range(NB):
        out[b] = from_feature_major(res[b]["y_out"])
    return out


PAIRS = [[0, 1], [2, 3], [4, 5], [6, 7]]


def build_fused8_program(depth=DEPTH):
    nc = bass.Bass("TRN2", target_bir_lowering=False)
    x_in = dram_w(nc, "xT_in", [128, KD, NTOK])
    gains = dram_w(nc, "gains", [128, 3 * depth + 1, KD])
    selc = dram_w(nc, "selc", [128, 2])
    cin = {k: dram_w(nc, "c_" + k, shp) for k, shp in MIX_CONST_SHAPES.items()}
    rel_d = dram_w(nc, "rel", [32, 4])
    LW_ = []
    for l in range(depth):
        d = {}
        for k, shp in (("wg1", [D, DFF]), ("wu1", [D, DFF]), ("wd1", [DFF, D]), ("wgm", [D, 2048]), ("wbn", [512, D]), ("wbg", [1024, D]),
                       ("wo", [D, D]), ("wg2", [D, DFF]), ("wu2", [D, DFF]), ("wd2", [DFF, D])):
            d[k] = dram_w(nc, "%s_l%d" % (k, l), shp)
        dm = {}
        for k, shp in MIX_W_SHAPES.items():
            if k == "rel":
                continue
            dm[k] = dram_w(nc, "%s_l%d" % (k, l), shp)
        d["mix"] = dm
        LW_.append(d)
    y_out = nc.dram_tensor("y_out", [128, KD, NTOK], F32, kind="ExternalOutput").ap()
    xs = nc.dram_tensor("xs_scr", [128, KD, NTOK], F32).ap()
    HC, OC_ = 2, 4
    h_src_t = [nc.dram_tensor("h_src%d" % c, [128 * KD, 1024], BF16) for c in range(HC)]
    h_all_t = [[nc.dram_tensor("h_all%d_%d" % (i, c), [2 * 128 * KD, 1024], BF16) for c in range(HC)] for i in range(2)]
    o_src_t = [nc.dram_tensor("o_src%d" % c, [128 * 6, 1024], BF16) for c in range(OC_)]
    o_all_t = [[nc.dram_tensor("o_all%d_%d" % (i, c), [2 * 128 * 6, 1024], BF16) for c in range(OC_)] for i in range(2)]
    h_src = [t.ap().rearrange("(p k) t -> p k t", k=KD) for t in h_src_t]
    h_all = [[t.ap().rearrange("(r p k) t -> r p k t", r=2, k=KD) for t in row] for row in h_all_t]
    o_src = [t.ap().rearrange("(p c) t -> p c t", c=6) for t in o_src_t]
    o_all = [[t.ap().rearrange("(r p c) t -> r p c t", r=2, c=6) for t in row] for row in o_all_t]

    class ChunkedDst:
        def __init__(self, aps):
            self.aps = aps

        def __getitem__(self, idx):
            p, c, t = idx
            ch = t.start // 1024
            assert (t.stop - 1) // 1024 == ch
            return self.aps[ch][p, c, t.start - ch * 1024:t.stop - ch * 1024]
    h_dst = ChunkedDst(h_src)
    o_dst = ChunkedDst(o_src)
    fc = nc.dram_tensor("fc_scr", [4, 128, LC], BF16).ap()
    fw = nc.dram_tensor("fw_scr", [4, 128, LW], BF16).ap()
    efd = nc.dram_tensor("efar_scr", [128, 4], F32).ap()
    with ExitStack() as stack:
        C = Ctx(nc, stack)
        C.psum_banks()
        P = C.P
        gt = C.sb("gains", [128, 3 * depth + 1, KD], F32)
        selt = C.sb("selc", [128, 2], F32)
        ones = C.sb("ones", [128, 128], BF16)
        epsc = C.sb("epsc", [128, 1], F32)
        base = {"ones": ones, "epsc": epsc, "sq": C.sb("sq", [128, KD, TT], BF16), "rstd": C.sb("rstd", [128, TT], F32)}
        P.op("pool", lambda e: e.memset(ones[:, :], 1.0), writes=["ones"])
        P.op("pool", lambda e: e.memset(epsc[:, :], EPS), writes=["epsc"])
        P.op("sp", lambda e: e.dma_start(out=gt[:, :, :], in_=gains[:, :, :]), writes=["gains"], dma_key="gains")
        P.op("sp", lambda e: e.dma_start(out=selt[:, :], in_=selc[:, :]), writes=["selc"], dma_key="selc")
        C.gen_banks = [0, 1, 2]
        with ExitStack() as st:
            mixer_build_tables(C, cin, rel_d, fc, fw, efd, 0, st)
            P.barrier()
        C.gen_banks = list(range(8))
        gi = {"ffn1": 0, "mixn": 1}
        emit_tok_phase(C, True, False, base, gt, gi, {"wg1": LW_[0]["wg1"], "wu1": LW_[0]["wu1"], "wd1": LW_[0]["wd1"]},
                       x_in, xs, None, h_dst, None)
        for l in range(depth):
            par = l % 2
            def xchg_h(par=par):
                for c in range(HC):
                    P.op("pool", lambda e, c=c: e.collective_compute("AllGather", ALU.bypass, replica_groups=PAIRS, ins=[h_src_t[c].ap().opt()],
                                                                     outs=[h_all_t[par][c].ap().opt()]),
                         writes=[("h_all", par, c)], dma_key="cc", dma_inc=1)
                    P.wait_all("pool", [("h_all", par, c)])
                P.barrier()
            xchg_h()
            C.gen_banks = [0, 1, 2]
            with ExitStack() as sm:
                T = mixer_alloc_consts(C, cin, fc, fw, efd, 0, sm)
                L = alloc_mixer_layer_tiles(C, sm)
                od = {"oT": o_dst, "fc": fc, "fkeys": [("F", 0, "c", 0), ("F", 0, "c", 1)], "oa_c0": 0, "ob_c0": 2}
                hsrc = lambda B, par=par: h_all[par][(B % 4) // 2][B // 4, :, :, ((B % 4) % 2) * BLK:((B % 4) % 2 + 1) * BLK]
                emit_mixer_layer(C, T, L, LW_[l]["mix"], hsrc, od)
                P.barrier()

            def xchg_o(par=par):
                for c in range(OC_):
                    P.op("pool", lambda e, c=c: e.collective_compute("AllGather", ALU.bypass, replica_groups=PAIRS, ins=[o_src_t[c].ap().opt()],
                                                                     outs=[o_all_t[par][c].ap().opt()]),
                         writes=[("o_all", par, c)], dma_key="cc", dma_inc=1)
                    P.wait_all("pool", [("o_all", par, c)])
                P.barrier()
            xchg_o()
            C.gen_banks = list(range(8))
            last = (l == depth - 1)
            gi = {"mixp": 3 * l + 1, "ffn2": 3 * l + 2, "ffn1": 3 * (l + 1), "mixn": 3 * (l + 1) + 1, "final": 3 * depth}
            dr = {k: LW_[l][k] for k in ("wgm", "wbn", "wbg", "wo", "wg2", "wu2", "wd2")}
            if not last:
                dr.update({k: LW_[l + 1][k] for k in ("wg1", "wu1", "wd1")})

            def make_loader(C_, st_, par=par):
                bt = C_.sb("oTalt", [128, 12, TT], BF16, st_)

                def loader(it, oT, okey):
                    for hh, dst in ((0, oT), (1, bt)):
                        tg = hh * NTOK + it * TT
                        oc_ = o_all[par][tg // 1024]
                        t0 = tg % 1024
                        for r in range(2):
                            P.op("sp", lambda e, dst=dst, r=r, t0=t0, oc_=oc_: e.dma_start(out=dst[:, 2 * r:2 * r + 2, :], in_=oc_[r, :, 0:2, t0:t0 + TT]),
                                 writes=[okey if hh == 0 else "oTalt"], dma_key=(okey if hh == 0 else "oTalt"))
                            P.op("sp", lambda e, dst=dst, r=r, t0=t0, oc_=oc_: e.dma_start(out=dst[:, 4 + 4 * r:8 + 4 * r, :], in_=oc_[r, :, 2:6, t0:t0 + TT]),
                                 writes=[okey if hh == 0 else "oTalt"], dma_key=(okey if hh == 0 else "oTalt"))
                    P.op("pool", lambda e: e.tensor_scalar(out=bt[:, :, :], in0=bt[:, :, :], scalar1=selt[:, 1:2], scalar2=None, op0=ALU.mult),
                         reads=["oTalt", "selc"], writes=["oTalt"])
                    P.op("dve", lambda e, oT=oT: e.scalar_tensor_tensor(out=oT[:, :, :], in0=oT[:, :, :], scalar=selt[:, 0:1], in1=bt[:, :, :],
                                                                         op0=ALU.mult, op1=ALU.add),
                         reads=[okey, "oTalt", "selc"], writes=[okey])
                return loader
            emit_tok_phase(C, False, last, base, gt, gi, dr, xs, xs, None, h_dst, y_out, oT_loader=make_loader)
        P.wait_all("sp", [("hst", 0), ("hst", 1)])
        global _last_prog
        _last_prog = P
        P.emit(stack)
    return nc


def fused8_inputs(inp, c, depth=DEPTH):
    b, h = c // 2, c % 2
    m = {"xT_in": to_feature_major(inp["x"][b, h * NTOK:(h + 1) * NTOK])}
    g = np.zeros((128, 3 * depth + 1, KD), np.float32)
    for l in range(depth):
        g[:, 3 * l] = gain_cols(inp["ffn1_norm"][l])
        g[:, 3 * l + 1] = gain_cols(inp["mix_norm"][l])
        g[:, 3 * l + 2] = gain_cols(inp["ffn2_norm"][l])
    g[:, 3 * depth] = gain_cols(inp["final_norm"])
    m["gains"] = g
    sel = np.zeros((128, 2), np.float32)
    sel[:, h] = 1.0
    m["selc"] = sel
    for k, v in mixer_consts().items():
        m["c_" + k] = v
    for l in range(depth):
        m["wg1_l%d" % l] = inp["ffn1_w_gate"][l]
        m["wu1_l%d" % l] = inp["ffn1_w_up"][l]
        m["wd1_l%d" % l] = inp["ffn1_w_down"][l]
        m["wg2_l%d" % l] = inp["ffn2_w_gate"][l]
        m["wu2_l%d" % l] = inp["ffn2_w_up"][l]
        m["wd2_l%d" % l] = inp["ffn2_w_down"][l]
        m["wgm_l%d" % l] = np.ascontiguousarray(inp["w_in"][l][:, OFF_GM:OFF_GM + 2048])
        m["wbn_l%d" % l] = inp["w_branch_nsa"][l]
        m["wbg_l%d" % l] = inp["w_branch_gla"][l]
        m["wo_l%d" % l] = inp["w_out"][l]
        for k, v in mixer_weight_inputs(inp, l, h).items():
            if k == "rel":
                m["rel"] = v
            else:
                m["%s_l%d" % (k, l)] = v
    return m


def kernel_fused8(**inp):
    inp = {k: np.asarray(v) for k, v in inp.items()}
    nc = _prog("fused8", lambda: build_fused8_program())
    maps = [fused8_inputs(inp, c) for c in range(8)]
    res = run_bass_kernel_spmd(nc, maps, core_ids=list(range(8))).results
    out = np.zeros((NB, S, D), np.float32)
    for c in range(8):
        b, h = c // 2, c % 2
        out[b, h * NTOK:(h + 1) * NTOK] = from_feature_major(res[c]["y_out"])
    return out
```
